# Optimizing a Trainium2 kernel written in Bass

```python
import jax, jax.numpy as jnp
from jax import lax
import numpy as np

D_MODEL = 1024
BATCH = 1
SEQ = 16384
DEPTH = 4

N_MEM = 256
N_EVEN = (DEPTH + 1) // 2
N_ODD = DEPTH // 2
MIX_WIDTH = D_MODEL
EPS = 1e-6

MLA_HEADS = 8
MLA_NOPE = 64
MLA_ROPE = 32
MLA_V = 64
MLA_Q_RANK = 256
MLA_KV_RANK = 128
ROPE_THETA = 10000.0
Q_BLOCK = 128

GLA_HEADS = 4
GLA_DK = 64
GLA_DV = 128
GLA_GATE_RANK = 16
GLA_TAU = 16.0
GLA_CHUNK = 64

MLSTM_HEADS = 4
MLSTM_DH = MIX_WIDTH // MLSTM_HEADS
MLSTM_CONV = 4
MLSTM_CHUNK = 64

XATTN_HEADS = 4
XATTN_DH = D_MODEL // XATTN_HEADS

D_FF = 4 * D_MODEL

EVEN_SPLITS = (MLA_Q_RANK, MLA_KV_RANK, MLA_ROPE, GLA_HEADS * GLA_DK, GLA_HEADS * GLA_DK,
               GLA_HEADS * GLA_DV, GLA_GATE_RANK, GLA_HEADS * GLA_DV)
EVEN_IN = sum(EVEN_SPLITS)
EVEN_OUT = MLA_HEADS * MLA_V + GLA_HEADS * GLA_DV

kernel_name = "hybrid_mla_gla_mlstm_sandwich_trunk"


def _rmsnorm(x, g):
    xf = x.astype(jnp.float32)
    y = xf * lax.rsqrt(jnp.mean(xf * xf, axis=-1, keepdims=True) + EPS)
    return (y * g.astype(jnp.float32)).astype(x.dtype)


def _layernorm(x, g):
    xf = x.astype(jnp.float32)
    mu = jnp.mean(xf, axis=-1, keepdims=True)
    xc = xf - mu
    y = xc * lax.rsqrt(jnp.mean(xc * xc, axis=-1, keepdims=True) + EPS)
    return (y * g.astype(jnp.float32)).astype(x.dtype)


def _rope_tables(positions, dim):
    inv_freq = ROPE_THETA ** (-jnp.arange(0, dim, 2, dtype=jnp.float32) / dim)
    ang = positions.astype(jnp.float32)[..., None] * inv_freq
    return jnp.cos(ang), jnp.sin(ang)


def _apply_rope(x, cos, sin):
    x1, x2 = jnp.split(x.astype(jnp.float32), 2, axis=-1)
    return jnp.concatenate([x1 * cos - x2 * sin, x2 * cos + x1 * sin], axis=-1).astype(x.dtype)


def _to_chunks(t, L):
    B, S, H, d = t.shape
    return t.reshape(B, S // L, L, H, d).transpose(1, 0, 3, 2, 4)


def _from_chunks(t):
    nc, B, H, L, d = t.shape
    return t.transpose(1, 0, 3, 2, 4).reshape(B, nc * L, H, d)


def _mla(c_q, c_kv, k_pe, positions, g_q, w_uq, g_kv, w_ukv):
    B, S, _ = c_q.shape
    H = MLA_HEADS
    q = (_rmsnorm(c_q, g_q) @ w_uq).reshape(B, S, H, MLA_NOPE + MLA_ROPE)
    q_nope, q_pe = q[..., :MLA_NOPE], q[..., MLA_NOPE:]
    kv = (_rmsnorm(c_kv, g_kv) @ w_ukv).reshape(B, S, H, MLA_NOPE + MLA_V)
    k_nope, v = kv[..., :MLA_NOPE], kv[..., MLA_NOPE:]
    cos, sin = _rope_tables(positions, MLA_ROPE)
    q_pe = _apply_rope(q_pe, cos[:, :, None, :], sin[:, :, None, :])
    k_pe = _apply_rope(k_pe, cos, sin)
    scale = (MLA_NOPE + MLA_ROPE) ** -0.5
    nb = S // Q_BLOCK
    key_idx = jnp.arange(S)

    def to_blocks(t):
        return jnp.moveaxis(t.reshape(B, nb, Q_BLOCK, *t.shape[2:]), 1, 0)

    def attend(blk):
        qn, qp, bi = blk
        s = (jnp.einsum('bqhd,bkhd->bhqk', qn, k_nope)
             + jnp.einsum('bqhr,bkr->bhqk', qp, k_pe)).astype(jnp.float32) * scale
        q_idx = bi * Q_BLOCK + jnp.arange(Q_BLOCK)
        s = jnp.where(key_idx[None, :] <= q_idx[:, None], s, -jnp.inf)
        p = jax.nn.softmax(s, axis=-1).astype(v.dtype)
        return jnp.einsum('bhqk,bkhd->bqhd', p, v)

    o = lax.map(attend, (to_blocks(q_nope), to_blocks(q_pe), jnp.arange(nb)))
    return jnp.moveaxis(o, 0, 1).reshape(B, S, H * MLA_V)


def _gla(q, k, v, g_lr, r, w_gate, b_gate, g_norm):
    B, S, _ = q.shape
    H, DK, DV, L = GLA_HEADS, GLA_DK, GLA_DV, GLA_CHUNK
    f32 = jnp.float32
    log_a = jax.nn.log_sigmoid((g_lr @ w_gate + b_gate).astype(f32)) / GLA_TAU
    qc = _to_chunks(q.astype(f32).reshape(B, S, H, DK) * DK ** -0.5, L)
    kc = _to_chunks(k.astype(f32).reshape(B, S, H, DK), L)
    vc = _to_chunks(v.astype(f32).reshape(B, S, H, DV), L)
    gc = _to_chunks(log_a.reshape(B, S, H, DK), L)
    causal = jnp.tril(jnp.ones((L, L), dtype=bool))

    def step(state, inp):
        qi, ki, vi, gi = inp
        b = jnp.cumsum(gi, axis=2)
        b_end = b[:, :, -1:, :]
        diff = b[:, :, :, None, :] - b[:, :, None, :, :]
        decay = jnp.exp(jnp.where(causal[:, :, None], diff, -jnp.inf))
        attn = jnp.einsum('bhtd,bhsd,bhtsd->bhts', qi, ki, decay)
        o = attn @ vi + (qi * jnp.exp(b)) @ state
        state = (jnp.exp(b_end).swapaxes(-1, -2) * state
                 + (ki * jnp.exp(b_end - b)).swapaxes(-1, -2) @ vi)
        return state, o

    _, o = lax.scan(step, jnp.zeros((B, H, DK, DV), f32), (qc, kc, vc, gc))
    o = _rmsnorm(_from_chunks(o), g_norm).reshape(B, S, H * DV)
    return (o * jax.nn.silu(r.astype(f32))).astype(q.dtype)


def _mlstm_cell(q, k, v, i_pre, f_pre):
    B, S, H, DH = q.shape
    L = MLSTM_CHUNK
    f32 = jnp.float32
    qc = _to_chunks(q.astype(f32) * DH ** -0.5, L)
    kc = _to_chunks(k.astype(f32), L)
    vc = _to_chunks(v.astype(f32), L)
    ic = _to_chunks(i_pre.astype(f32)[..., None], L)[..., 0]
    fc = _to_chunks(jax.nn.log_sigmoid(f_pre.astype(f32))[..., None], L)[..., 0]
    causal = jnp.tril(jnp.ones((L, L), dtype=bool))

    def step(carry, inp):
        C, n, m = carry
        qi, ki, vi, ii, lfi = inp
        b = jnp.cumsum(lfi, axis=-1)
        log_d = jnp.where(causal, b[..., :, None] - b[..., None, :] + ii[..., None, :], -jnp.inf)
        m_inter = b + m[..., None]
        m_t = jnp.maximum(m_inter, jnp.max(log_d, axis=-1))
        w_intra = jnp.exp(log_d - m_t[..., None]) * jnp.einsum('bhtd,bhsd->bhts', qi, ki)
        w_inter = jnp.exp(m_inter - m_t)
        num = w_intra @ vi + w_inter[..., None] * (qi @ C)
        den = jnp.sum(w_intra, axis=-1) + w_inter * jnp.einsum('bhtd,bhd->bht', qi, n)
        h = num / jnp.maximum(jnp.abs(den), jnp.exp(-m_t))[..., None]
        b_end = b[..., -1]
        log_w = b_end[..., None] - b + ii
        m_new = jnp.maximum(b_end + m, jnp.max(log_w, axis=-1))
        w_s = jnp.exp(log_w - m_new[..., None])
        carry_decay = jnp.exp(b_end + m - m_new)
        C = carry_decay[..., None, None] * C + jnp.einsum('bhs,bhsd,bhse->bhde', w_s, ki, vi)
        n = carry_decay[..., None] * n + jnp.einsum('bhs,bhsd->bhd', w_s, ki)
        return (C, n, m_new), h

    init = (jnp.zeros((B, H, DH, DH), f32), jnp.zeros((B, H, DH), f32), jnp.zeros((B, H), f32))
    _, h = lax.scan(step, init, (qc, kc, vc, ic, fc))
    return _from_chunks(h).astype(q.dtype)


def _causal_conv(x, w, b):
    C = x.shape[-1]
    y = lax.conv_general_dilated(x, w[:, None, :].astype(x.dtype), window_strides=(1,),
                                 padding=[(MLSTM_CONV - 1, 0)],
                                 dimension_numbers=('NWC', 'WIO', 'NWC'),
                                 feature_group_count=C)
    return y + b


def _even_mixer(hn, positions, w_in, g_q, w_uq, g_kv, w_ukv, w_gate, b_gate, g_gla, w_out):
    proj = hn @ w_in
    offs = np.cumsum(EVEN_SPLITS)[:-1].tolist()
    c_q, c_kv, k_pe, gq, gk, gv, g_lr, r = jnp.split(proj, offs, axis=-1)
    a = _mla(c_q, c_kv, k_pe, positions, g_q, w_uq, g_kv, w_ukv)
    g = _gla(gq, gk, gv, g_lr, r, w_gate, b_gate, g_gla)
    return jnp.concatenate([a, g], axis=-1) @ w_out


def _odd_mixer(hn, w_in, conv_w, conv_b, w_q, w_k, w_v, w_gates, b_gates, g_hnorm, skip, w_out):
    B, S, _ = hn.shape
    H, DH = MLSTM_HEADS, MLSTM_DH
    x_m, z = jnp.split(hn @ w_in, 2, axis=-1)
    x_c = jax.nn.silu(_causal_conv(x_m, conv_w, conv_b))
    xch = x_c.reshape(B, S, H, DH)
    xmh = x_m.reshape(B, S, H, DH)
    q = jnp.einsum('bshd,hde->bshe', xch, w_q)
    k = jnp.einsum('bshd,hde->bshe', xch, w_k)
    v = jnp.einsum('bshd,hde->bshe', xmh, w_v)
    gates = jnp.concatenate([q, k, v], axis=-1).reshape(B, S, 3 * MIX_WIDTH) @ w_gates + b_gates
    i_pre, f_pre = gates[..., :H], gates[..., H:]
    h = _mlstm_cell(q, k, v, i_pre, f_pre)
    h = _layernorm(h, g_hnorm.reshape(H, DH)).reshape(B, S, MIX_WIDTH)
    out = (h + skip * x_c) * jax.nn.silu(z)
    return out @ w_out


def _mem_xattn(hn, mem_n, w_q, w_k, w_v, w_o):
    B, S, _ = hn.shape
    M = mem_n.shape[1]
    q = (hn @ w_q).reshape(B, S, XATTN_HEADS, XATTN_DH)
    k = (mem_n @ w_k).reshape(B, M, XATTN_HEADS, XATTN_DH)
    v = (mem_n @ w_v).reshape(B, M, XATTN_HEADS, XATTN_DH)
    s = jnp.einsum('bshd,bmhd->bhsm', q, k).astype(jnp.float32) * XATTN_DH ** -0.5
    p = jax.nn.softmax(s, axis=-1).astype(v.dtype)
    return jnp.einsum('bhsm,bmhd->bshd', p, v).reshape(B, S, D_MODEL) @ w_o


def _sq_relu_mlp(hn, w1, w2):
    return jnp.square(jax.nn.relu(hn @ w1)) @ w2


def setup_inputs(seed: int = 0) -> dict:
    key = jax.random.key(seed)
    keys = jax.random.split(key, 64)
    ctr = [0]

    def nk():
        ctr[0] += 1
        return keys[ctr[0] - 1]

    def w(shape, fan_in):
        return jax.random.normal(nk(), shape, jnp.float32) * fan_in ** -0.5

    def gain(shape):
        return 1.0 + 0.05 * jax.random.normal(nk(), shape, jnp.float32)

    def small(shape, s=0.02):
        return s * jax.random.normal(nk(), shape, jnp.float32)

    x = jax.random.normal(nk(), (BATCH, SEQ, D_MODEL), jnp.float32)
    mem = jax.random.normal(nk(), (BATCH, N_MEM, D_MODEL), jnp.float32)
    offset = jax.random.randint(nk(), (BATCH, 1), 0, 4096, dtype=jnp.int32)
    positions = (offset + jnp.arange(SEQ, dtype=jnp.int32)[None, :]).astype(jnp.int32)

    f_bias = jnp.broadcast_to(jnp.linspace(3.0, 6.0, MLSTM_HEADS, dtype=jnp.float32), (N_ODD, MLSTM_HEADS))
    od_b_gates = jnp.concatenate([small((N_ODD, MLSTM_HEADS), 0.1),
                                  f_bias + small((N_ODD, MLSTM_HEADS), 0.1)], axis=-1)
    return {
        "x": x, "mem": mem, "positions": positions,
        "g_mix_pre": gain((DEPTH, D_MODEL)), "g_mix_post": gain((DEPTH, D_MODEL)),
        "g_xattn_pre": gain((DEPTH, D_MODEL)), "g_xattn_post": gain((DEPTH, D_MODEL)),
        "g_mem": gain((DEPTH, D_MODEL)),
        "g_ffn_pre": gain((DEPTH, D_MODEL)), "g_ffn_post": gain((DEPTH, D_MODEL)),
        "ev_w_in": w((N_EVEN, D_MODEL, EVEN_IN), D_MODEL),
        "ev_g_q": gain((N_EVEN, MLA_Q_RANK)),
        "ev_w_uq": w((N_EVEN, MLA_Q_RANK, MLA_HEADS * (MLA_NOPE + MLA_ROPE)), MLA_Q_RANK),
        "ev_g_kv": gain((N_EVEN, MLA_KV_RANK)),
        "ev_w_ukv": w((N_EVEN, MLA_KV_RANK, MLA_HEADS * (MLA_NOPE + MLA_V)), MLA_KV_RANK),
        "ev_w_gate": w((N_EVEN, GLA_GATE_RANK, GLA_HEADS * GLA_DK), GLA_GATE_RANK),
        "ev_b_gate": small((N_EVEN, GLA_HEADS * GLA_DK), 0.1),
        "ev_g_gla": gain((N_EVEN, GLA_DV)),
        "ev_w_out": w((N_EVEN, EVEN_OUT, D_MODEL), EVEN_OUT),
        "od_w_in": w((N_ODD, D_MODEL, 2 * MIX_WIDTH), D_MODEL),
        "od_conv_w": w((N_ODD, MLSTM_CONV, MIX_WIDTH), MLSTM_CONV),
        "od_conv_b": small((N_ODD, MIX_WIDTH)),
        "od_w_q": w((N_ODD, MLSTM_HEADS, MLSTM_DH, MLSTM_DH), MLSTM_DH),
        "od_w_k": w((N_ODD, MLSTM_HEADS, MLSTM_DH, MLSTM_DH), MLSTM_DH),
        "od_w_v": w((N_ODD, MLSTM_HEADS, MLSTM_DH, MLSTM_DH), MLSTM_DH),
        "od_w_gates": w((N_ODD, 3 * MIX_WIDTH, 2 * MLSTM_HEADS), 3 * MIX_WIDTH),
        "od_b_gates": od_b_gates,
        "od_g_hnorm": gain((N_ODD, MIX_WIDTH)),
        "od_skip": gain((N_ODD, MIX_WIDTH)),
        "od_w_out": w((N_ODD, MIX_WIDTH, D_MODEL), MIX_WIDTH),
        "xa_w_q": w((DEPTH, D_MODEL, D_MODEL), D_MODEL),
        "xa_w_k": w((DEPTH, D_MODEL, D_MODEL), D_MODEL),
        "xa_w_v": w((DEPTH, D_MODEL, D_MODEL), D_MODEL),
        "xa_w_o": w((DEPTH, D_MODEL, D_MODEL), D_MODEL),
        "ffn_w1": w((DEPTH, D_MODEL, D_FF), D_MODEL),
        "ffn_w2": w((DEPTH, D_FF, D_MODEL), D_FF),
    }


def reference(x, mem, positions,
              g_mix_pre, g_mix_post, g_xattn_pre, g_xattn_post, g_mem, g_ffn_pre, g_ffn_post,
              ev_w_in, ev_g_q, ev_w_uq, ev_g_kv, ev_w_ukv, ev_w_gate, ev_b_gate, ev_g_gla, ev_w_out,
              od_w_in, od_conv_w, od_conv_b, od_w_q, od_w_k, od_w_v, od_w_gates, od_b_gates,
              od_g_hnorm, od_skip, od_w_out,
              xa_w_q, xa_w_k, xa_w_v, xa_w_o,
              ffn_w1, ffn_w2):
    h = x
    for layer in range(DEPTH):
        j = layer // 2
        hn = _rmsnorm(h, g_mix_pre[layer])
        if layer % 2 == 0:
            mix = _even_mixer(hn, positions, ev_w_in[j], ev_g_q[j], ev_w_uq[j], ev_g_kv[j],
                              ev_w_ukv[j], ev_w_gate[j], ev_b_gate[j], ev_g_gla[j], ev_w_out[j])
        else:
            mix = _odd_mixer(hn, od_w_in[j], od_conv_w[j], od_conv_b[j], od_w_q[j], od_w_k[j],
                             od_w_v[j], od_w_gates[j], od_b_gates[j], od_g_hnorm[j], od_skip[j],
                             od_w_out[j])
        h = h + _rmsnorm(mix, g_mix_post[layer])
        mem_n = _rmsnorm(mem, g_mem[layer])
        xa = _mem_xattn(_rmsnorm(h, g_xattn_pre[layer]), mem_n,
                        xa_w_q[layer], xa_w_k[layer], xa_w_v[layer], xa_w_o[layer])
        h = h + _rmsnorm(xa, g_xattn_post[layer])
        f = _sq_relu_mlp(_rmsnorm(h, g_ffn_pre[layer]), ffn_w1[layer], ffn_w2[layer])
        h = h + _rmsnorm(f, g_ffn_post[layer])
    return h
```

```python
import contextlib
import numpy as np
import concourse.bass as bass
import concourse.mybir as mybir
from concourse.bass_utils import run_bass_kernel_spmd

F32 = mybir.dt.float32
BF16 = mybir.dt.bfloat16
I32 = mybir.dt.int32
AF = mybir.ActivationFunctionType
ALU = mybir.AluOpType
AX = mybir.AxisListType

COMPUTE = ('pe', 'act', 'dve', 'pool')
NDSEM = 8


class _Op:
    __slots__ = ('eng', 'fn', 'dma', 'deps', 'has_dep', 'cval', 'dq', 'dslot', 'dval', 'idx')


class Prog:
    def __init__(self, nc, self_sync=True):
        self.nc = nc
        self.ops = []
        self.last_w = {}
        self.readers = {}
        self.stack = contextlib.ExitStack()
        self.self_sync = self_sync
        self.ndma = {}
        self.out_dmas = []
        self._n = 0
        self.psum_names = set()

    def sb(self, shape, dt, name=None):
        self._n += 1
        name = name or f"sb{self._n}"
        return self.stack.enter_context(self.nc.sbuf_tensor(name, list(shape), dt))

    def ps(self, shape, dt=F32, name=None):
        self._n += 1
        name = name or f"ps{self._n}"
        self.psum_names.add(name)
        return self.stack.enter_context(self.nc.psum_tensor(name, list(shape), dt))

    @staticmethod
    def _keys(xs):
        out = []
        for x in xs:
            if x is None or isinstance(x, (int, float)):
                continue
            if isinstance(x, (str, tuple)):
                out.append(x)
            else:
                out.append(x.name)
        return out

    def op(self, eng, fn, reads=(), writes=(), dma=False):
        o = _Op()
        o.eng = eng
        o.fn = fn
        o.dma = dma
        o.has_dep = False
        o.idx = len(self.ops)
        deps = set()
        pn = self.psum_names
        rk = [k[0] if (isinstance(k, tuple) and k[0] in pn) else k for k in self._keys(reads)]
        wk = [k[0] if (isinstance(k, tuple) and k[0] in pn) else k for k in self._keys(writes)]
        for k in rk:
            if k in self.last_w:
                deps.add(self.last_w[k])
            if k in pn:
                r = self.readers.get(k)
                if r:
                    for e2, v in r.items():
                        if e2 != eng and not isinstance(v, list):
                            deps.add(v)
        for k in wk:
            if k in self.last_w:
                deps.add(self.last_w[k])
            r = self.readers.get(k)
            if r:
                for v in r.values():
                    if isinstance(v, list):
                        deps.update(v)
                    else:
                        deps.add(v)
        deps.discard(o.idx)
        o.deps = deps
        if dma:
            j = self.ndma.get(eng, 0)
            self.ndma[eng] = j + 1
            o.dq = eng
            o.dslot = j % NDSEM
            o.dval = 16 * (j // NDSEM + 1)
        self.ops.append(o)
        for k in wk:
            self.last_w[k] = o.idx
            self.readers[k] = {}
        for k in rk:
            if k in wk:
                continue
            r = self.readers.setdefault(k, {})
            if dma:
                r.setdefault('dma', []).append(o.idx)
            else:
                r[eng] = o.idx
        return o

    def dma(self, q, out, in_, R=None, W=None, is_out=False, **kw):
        o = self.op(q, lambda e: e.dma_start(out=out, in_=in_, **kw),
                    reads=[in_] if R is None else R, writes=[out] if W is None else W, dma=True)
        if is_out:
            self.out_dmas.append(o.idx)
        return o

    def mm(self, out, lhsT, rhs, start=True, stop=True, R=None, W=None, **kw):
        return self.op('pe', lambda e: e.matmul(out, lhsT, rhs, start=start, stop=stop, **kw),
                       reads=[lhsT, rhs] if R is None else R, writes=[out] if W is None else W)

    def tr(self, out, in_, ident, R=None, W=None):
        return self.op('pe', lambda e: e.transpose(out, in_, ident),
                       reads=[in_, ident] if R is None else R, writes=[out] if W is None else W)

    def act(self, out, in_, func, bias=None, scale=None, accum_out=None, R=None, W=None, eng='act'):
        kw = {}
        if bias is not None:
            kw['bias'] = bias
        if scale is not None:
            kw['scale'] = scale
        if accum_out is not None:
            kw['accum_out'] = accum_out
        return self.op(eng, lambda e: e.activation(out, in_, func, **kw),
                       reads=[in_, bias, scale] if R is None else R, writes=[out, accum_out] if W is None else W)

    def v(self, eng, name, *args, reads=(), writes=(), **kw):
        return self.op(eng, lambda e: getattr(e, name)(*args, **kw), reads=reads, writes=writes)

    def emit(self):
        nc = self.nc
        ops = self.ops
        for o in ops:
            for d in o.deps:
                ops[d].has_dep = True
        for i in self.out_dmas:
            ops[i].has_dep = True
        st = self.stack
        csem = {e: st.enter_context(nc.semaphore(f"c_{e}")) for e in COMPUTE}
        dsem = {}
        for q in self.ndma:
            dsem[q] = [st.enter_context(nc.semaphore(f"d_{q}_{i}")) for i in range(NDSEM)]
        cnt = {e: 0 for e in COMPUTE}
        for o in ops:
            if not o.dma and o.has_dep:
                cnt[o.eng] += 1
                o.cval = cnt[o.eng]
            elif not o.dma:
                o.cval = None
        per_eng = {}
        for o in ops:
            per_eng.setdefault(o.eng, []).append(o)
        waited = {}
        dma_hist = {q: [] for q in self.ndma}

        def emit_engine(ename, e):
            wl = waited.setdefault(ename, {})
            for o in per_eng.get(ename, []):
                need = {}
                for d in o.deps:
                    p = ops[d]
                    if p.dma:
                        sem = dsem[p.dq][p.dslot]
                        val = p.dval
                    else:
                        if p.eng == ename and (ename == 'pe' or not self.self_sync):
                            continue
                        sem = csem[p.eng]
                        val = p.cval
                    if need.get(id(sem), (None, 0))[1] < val:
                        need[id(sem)] = (sem, val)
                if o.dma and o.dval > 16:
                    sem = dsem[o.dq][o.dslot]
                    k = id(sem)
                    if need.get(k, (None, 0))[1] < o.dval - 16:
                        need[k] = (sem, o.dval - 16)
                for k, (sem, val) in need.items():
                    if wl.get(k, 0) >= val:
                        continue
                    wl[k] = val
                    e.wait_ge(sem, val)
                ins = o.fn(e)
                if o.dma:
                    ins.then_inc(dsem[o.dq][o.dslot], 16)
                elif o.has_dep:
                    ins.then_inc(csem[o.eng], 1)
            if ename == 'sp':
                for i in self.out_dmas:
                    p = ops[i]
                    e.wait_ge(dsem[p.dq][p.dslot], p.dval)

        with nc.Block() as block:
            @block.sync
            def _(e):
                emit_engine('sp', e)

            @block.tensor
            def _(e):
                emit_engine('pe', e)

            @block.scalar
            def _(e):
                emit_engine('act', e)

            @block.vector
            def _(e):
                emit_engine('dve', e)

            @block.gpsimd
            def _(e):
                emit_engine('pool', e)
        self.stack.close()


EPS = 1e-6
NCORES = 8
TOK = 2048
NT = TOK // 128


class Ctx:
    def __init__(self):
        self.nc = bass.Bass("TRN2", target_bir_lowering=False)
        self.P = Prog(self.nc)
        self.rr = 0

    def din(self, name, shape, dt=F32):
        return self.nc.dram_tensor(name, list(shape), dt, kind="ExternalInput").ap()

    def dout(self, name, shape, dt=F32):
        return self.nc.dram_tensor(name, list(shape), dt, kind="ExternalOutput").ap()

    def ident(self):
        P = self.P
        idn = self.din("idn", [128, 128])
        self.idf = P.sb([128, 128], F32, "idf")
        self.idb = P.sb([128, 128], BF16, "idb")
        P.dma('sp', self.idf[:], idn)
        P.v('dve', 'tensor_copy', self.idb[:], self.idf[:], reads=[self.idf], writes=[self.idb])

    def small(self, name, shape, dt=F32, q='sp'):
        d = self.din(name, shape, dt)
        t = self.P.sb(shape, dt, "s_" + name)
        self.P.dma(q, t[:], d)
        return t


class WMat:
    def __init__(self, C, name, KC, N, rows=128):
        self.t = [C.P.sb([rows, N], BF16, f"{name}_{c}") for c in range(KC)]
        self.KC, self.N, self.rows = KC, N, rows

    def sl(self, c, lo, hi):
        return self.t[c][:, lo:hi]


def load_w(C, name, KC, N, g=None, rows=128, chunk=2048, stages=None, q=('sp', 'pool')):
    P = C.P
    w = C.din(name, [rows, KC * N])
    W = WMat(C, name, KC, N, rows)
    if stages is None:
        if not hasattr(C, '_stages'):
            C._stages = [P.sb([128, chunk], F32, f"wstage{i}") for i in range(3)]
        stages = C._stages
    engs = ('act', 'dve', 'pool')
    i = C.__dict__.get('_wi', 0)
    for c in range(KC):
        eng = engs[c % 3]
        for n0 in range(0, N, chunk):
            n = min(chunk, N - n0)
            st = stages[i % len(stages)]
            P.dma(q[i % len(q)], st[:rows, :n], w[:, c * N + n0:c * N + n0 + n])
            dst = W.t[c][:, n0:n0 + n]
            src = st[:rows, :n]
            if g is None:
                if eng == 'act':
                    P.act(dst, src, AF.Copy)
                else:
                    P.v(eng, 'tensor_copy', dst, src, reads=[st], writes=[W.t[c]])
            else:
                gs = g[:rows, c:c + 1]
                if eng == 'act':
                    P.act(dst, src, AF.Copy, scale=gs)
                else:
                    P.v(eng, 'tensor_scalar_mul', dst, src, gs, reads=[st, g], writes=[W.t[c]])
            i += 1
    C._wi = i
    return W


class NormScr:
    def __init__(self, C, D, tag, pt=None):
        P = C.P
        self.junk = P.sb([128, D], F32, f"junk_{tag}")
        self.ss = P.sb([128, 1], F32, f"ss_{tag}")
        self.rstd = P.sb([128, 1], F32, f"rstd_{tag}")
        self.xb = P.sb([128, D], BF16, f"xb_{tag}")
        self.pt = pt if pt is not None else P.ps([128, D // 128, 128], BF16, f"pt_{tag}")
        self.D = D


def rstd_of(C, src, D, S, ss=None, rstd=None, junk=None):
    P = C.P
    ss = S.ss if ss is None else ss
    rstd = S.rstd if rstd is None else rstd
    junk = S.junk if junk is None else junk
    P.act(junk, src, AF.Square, scale=float(D) ** -0.5, accum_out=ss)
    P.act(rstd, ss, AF.Sqrt, bias=EPS)
    P.v('dve', 'reciprocal', rstd, rstd, reads=[rstd], writes=[rstd])


def norm_T(C, src, D, S, dst, copy_eng='dve'):
    P = C.P
    rstd_of(C, src, D, S, S.ss[:], S.rstd[:], S.junk[:, :D])
    P.act(S.xb[:, :D], src, AF.Copy, scale=S.rstd[:])
    for c in range(D // 128):
        P.tr(S.pt[:, c, :], S.xb[:, c * 128:(c + 1) * 128], C.idb[:])
    if copy_eng == 'act':
        P.act(dst, S.pt[:, :D // 128, :], AF.Copy)
    else:
        P.v('dve', 'tensor_copy', dst, S.pt[:, :D // 128, :], reads=[S.pt], writes=[dst])


def postnorm_res(C, py, ht, gpo, S, tmp):
    P = C.P
    rstd_of(C, py, 1024, S, S.ss[:], S.rstd[:], S.junk[:, :1024])
    P.v('dve', 'scalar_tensor_tensor', tmp[:], py, S.rstd[:], gpo[:], ALU.mult, ALU.mult,
        reads=[py, S.rstd, gpo], writes=[tmp])
    P.v('pool', 'tensor_add', ht, ht, tmp[:], reads=[ht, tmp], writes=[ht])


def build_ffn():
    C = Ctx()
    P = C.P
    h = C.din("h", [TOK, 1024])
    ho = C.dout("ho", [TOK, 1024])
    C.ident()
    gpre = C.small("gpre", [128, 8])
    gpo = C.small("gpo", [128, 1024], q='pool')
    C._stages = [P.sb([128, 1024], F32, f"wstage{i}") for i in range(2)]
    W1 = load_w(C, "w1", 8, 4096, g=gpre, chunk=1024)
    W2 = load_w(C, "w2", 32, 1024, chunk=1024)
    S = NormScr(C, 1024, "a")
    hr = [P.sb([128, 1024], F32, f"hr{j}") for j in range(3)]
    xT = P.sb([128, 8, 512], BF16, "xT")
    h1T = [P.sb([128, 512], BF16, f"h1T{f}") for f in range(32)]
    rl = [P.sb([128, 512], BF16, f"rl{i}") for i in range(3)]
    p1 = [P.ps([128, 512], F32, f"p1_{i}") for i in range(3)]
    pys = [P.ps([128, 1024], F32, f"py{i}") for i in range(2)]
    n1 = 0
    nh = 0
    for g in range(TOK // 512):
        for j in range(4):
            r0 = g * 512 + j * 128
            ht = hr[nh % 3]
            nh += 1
            P.dma('sp', ht[:], h[r0:r0 + 128, :])
            norm_T(C, ht[:], 1024, S, xT[:, :, j * 128:(j + 1) * 128])
        for f in range(32):
            ps = p1[n1 % 3]
            r = rl[n1 % 3]
            n1 += 1
            for k in range(8):
                P.mm(ps[:], W1.sl(k, f * 128, (f + 1) * 128), xT[:, k, :], start=(k == 0), stop=(k == 7))
            P.act(r[:], ps[:], AF.Relu)
            P.v('pool', 'tensor_mul', h1T[f][:], r[:], r[:], reads=[r], writes=[h1T[f]])
        for j in range(4):
            r0 = g * 512 + j * 128
            ht = hr[nh % 3]
            nh += 1
            P.dma('pool', ht[:], h[r0:r0 + 128, :])
            py = pys[j % 2]
            for n in range(2):
                for k in range(32):
                    P.mm(py[:, n * 512:(n + 1) * 512], h1T[k][:, j * 128:(j + 1) * 128],
                         W2.sl(k, n * 512, (n + 1) * 512), start=(k == 0), stop=(k == 31))
            postnorm_res(C, py[:], ht[:], gpo, S, S.junk)
            P.dma('sp', ho[r0:r0 + 128, :], ht[:], W=[("ho", r0)], is_out=True)
    P.emit()
    return C.nc


def build_xa():
    C = Ctx()
    P = C.P
    h = C.din("h", [TOK, 1024])
    mem = C.din("mem", [256, 1024])
    ho = C.dout("ho", [TOK, 1024])
    C.ident()
    gpre = C.small("gpre", [128, 8])
    gmem = C.small("gmem", [128, 8])
    gpo = C.small("gpo", [128, 1024], q='pool')
    C._stages = [P.sb([128, 1024], F32, f"wstage{i}") for i in range(3)]
    Wq = load_w(C, "wq", 8, 1024, g=gpre, chunk=1024)
    Wk = load_w(C, "wk", 8, 1024, g=gmem, chunk=1024)
    Wv = load_w(C, "wv", 8, 1024, g=gmem, chunk=1024)
    Wo = load_w(C, "wo", 8, 1024, chunk=1024)
    S = NormScr(C, 1024, "a")
    hr = [P.sb([128, 1024], F32, f"hr{j}") for j in range(3)]
    pa = [P.ps([128, 512], F32, f"pa{i}") for i in range(2)]
    po = [P.ps([128, 512], F32, f"po{i}") for i in range(2)]
    py = P.ps([128, 1024], F32, "py")
    memT = P.sb([128, 8, 256], BF16, "memT")
    for m in range(2):
        mt = hr[m]
        P.dma('sp', mt[:], mem[m * 128:(m + 1) * 128, :])
        norm_T(C, mt[:], 1024, S, memT[:, :, m * 128:(m + 1) * 128])
    kT = [P.sb([128, 256], BF16, f"kT{oc}") for oc in range(8)]
    for oc in range(8):
        ps = pa[oc % 2]
        for k in range(8):
            P.mm(ps[:, :256], Wk.sl(k, oc * 128, (oc + 1) * 128), memT[:, k, :], start=(k == 0), stop=(k == 7))
        P.v('dve', 'tensor_copy', kT[oc][:], ps[:, :256], reads=[ps], writes=[kT[oc]])
    V1 = [P.sb([128, 4, 257], BF16, f"V1_{m}") for m in range(2)]
    for m in range(2):
        P.v('pool', 'memset', V1[m][:], 1.0, writes=[V1[m]])
        for n in range(2):
            ps = pa[n % 2]
            for k in range(8):
                P.mm(ps[:], memT[:, k, m * 128:(m + 1) * 128], Wv.sl(k, n * 512, (n + 1) * 512),
                     start=(k == 0), stop=(k == 7))
            P.v('dve', 'tensor_copy', V1[m][:, 2 * n:2 * n + 2, 0:256],
                ps[:].rearrange("p (h d) -> p h d", h=2), reads=[ps], writes=[V1[m]])
    xT = P.sb([128, 8, 512], BF16, "xT")
    qT = [P.sb([128, 512], BF16, f"qT{oc}") for oc in range(8)]
    PT = [[P.sb([128, 512], BF16, f"PT{hh}_{m}") for m in range(2)] for hh in range(4)]
    oa = P.sb([128, 1024], BF16, "oa")
    oaT = P.sb([128, 8, 128], BF16, "oaT")
    rinv = P.sb([128, 4], F32, "rinv")
    nh = 0
    na = 0
    no = 0
    for g in range(TOK // 512):
        for j in range(4):
            r0 = g * 512 + j * 128
            ht = hr[nh % 3]
            nh += 1
            P.dma('sp', ht[:], h[r0:r0 + 128, :])
            norm_T(C, ht[:], 1024, S, xT[:, :, j * 128:(j + 1) * 128])
        for oc in range(8):
            ps = pa[na % 2]
            na += 1
            for k in range(8):
                P.mm(ps[:], Wq.sl(k, oc * 128, (oc + 1) * 128), xT[:, k, :], start=(k == 0), stop=(k == 7))
            if oc % 2 == 0:
                P.act(qT[oc][:], ps[:], AF.Copy)
            else:
                P.v('dve', 'tensor_copy', qT[oc][:], ps[:], reads=[ps], writes=[qT[oc]])
        for hh in range(4):
            for m in range(2):
                ps = pa[na % 2]
                na += 1
                for dc in range(2):
                    P.mm(ps[:], kT[2 * hh + dc][:, m * 128:(m + 1) * 128], qT[2 * hh + dc][:],
                         start=(dc == 0), stop=(dc == 1))
                P.act(PT[hh][m][:], ps[:], AF.Exp, scale=1.0 / 16.0)
        for j in range(4):
            r0 = g * 512 + j * 128
            ht = hr[nh % 3]
            nh += 1
            P.dma('pool', ht[:], h[r0:r0 + 128, :])
            for hh in range(4):
                ps = po[no % 2]
                no += 1
                for m in range(2):
                    P.mm(ps[:, :257], PT[hh][m][:, j * 128:(j + 1) * 128], V1[m][:, hh, :],
                         start=(m == 0), stop=(m == 1))
                P.v('dve', 'reciprocal', rinv[:, hh:hh + 1], ps[:, 256:257], reads=[ps], writes=[rinv])
                P.act(oa[:, hh * 256:(hh + 1) * 256], ps[:, 0:256], AF.Copy, scale=rinv[:, hh:hh + 1])
            for c in range(8):
                P.tr(S.pt[:, c, :], oa[:, c * 128:(c + 1) * 128], C.idb[:])
            P.v('dve', 'tensor_copy', oaT[:], S.pt[:], reads=[S.pt], writes=[oaT])
            for n in range(2):
                for k in range(8):
                    P.mm(py[:, n * 512:(n + 1) * 512], oaT[:, k, :], Wo.sl(k, n * 512, (n + 1) * 512),
                         start=(k == 0), stop=(k == 7))
            postnorm_res(C, py[:], ht[:], gpo, S, S.junk)
            P.dma('sp', ho[r0:r0 + 128, :], ht[:], W=[("ho", r0)], is_out=True)
    P.emit()
    return C.nc


TWO_PI = 6.283185307179586
SKIP = set()


def rope_tables(C, pos_i, invf):
    P = C.P
    posf = P.sb([128, NT], F32, "posf")
    P.v('dve', 'tensor_copy', posf[:], pos_i[:], reads=[pos_i], writes=[posf])
    ang = P.sb([128, NT, 128], F32, "ang")
    for j in range(NT):
        P.v('dve', 'tensor_scalar_mul', ang[:, j, :], invf[:], posf[:, j:j + 1], reads=[invf, posf], writes=[ang])
    tabs = []
    for nm, off in (("sin", 0.0), ("cos", 0.25)):
        t = P.sb([128, NT, 128], F32, "t_" + nm)
        ti = P.sb([128, NT, 128], I32, "ti_" + nm)
        P.v('dve', 'tensor_scalar', t[:], ang[:], 1.0 / TWO_PI, off, ALU.mult, ALU.add, reads=[ang], writes=[t])
        P.v('dve', 'tensor_copy', ti[:], t[:], reads=[t], writes=[ti])
        tf = P.sb([128, NT, 128], F32, "tf_" + nm)
        P.v('dve', 'tensor_copy', tf[:], ti[:], reads=[ti], writes=[tf])
        P.v('dve', 'tensor_sub', t[:], t[:], tf[:], reads=[t, tf], writes=[t])
        P.act(tf[:], t[:], AF.Sin, scale=6.28318)
        tabs.append(tf)
    return tabs[0], tabs[1]


def rope_apply(C, x1, x2, cs, sn, o1, o2, tmps, shape):
    P = C.P
    t1, t2, t3, t4 = tmps
    P.v('dve', 'tensor_mul', t1, x1, cs, reads=[x1, cs], writes=[t1])
    P.v('dve', 'tensor_mul', t2, x2, sn, reads=[x2, sn], writes=[t2])
    P.v('dve', 'tensor_mul', t3, x2, cs, reads=[x2, cs], writes=[t3])
    P.v('dve', 'tensor_mul', t4, x1, sn, reads=[x1, sn], writes=[t4])
    P.v('pool', 'tensor_sub', o1, t1, t2, reads=[t1, t2], writes=[o1])
    P.v('pool', 'tensor_add', o2, t3, t4, reads=[t3, t4], writes=[o2])


def build_a_even():
    C = Ctx()
    P = C.P
    h = C.din("h", [TOK, 1024])
    q_o = C.dout("q", [TOK, 768], BF16)
    kv_o = C.dout("kv", [TOK, 1024], BF16)
    kpe_o = C.dout("kpe", [TOK, 32], BF16)
    g_o = C.dout("gqkv", [TOK, 1024], BF16)
    la_o = C.dout("la", [TOK, 256])
    r_o = C.dout("r", [TOK, 512])
    C.ident()
    gpre = C.small("gpre", [128, 8])
    gq = C.small("gq", [128, 2])
    gkv = C.small("gkv", [128, 1])
    wga = C.small("wga", [17, 256])
    pos_i = C.small("pos", [128, NT], I32)
    invf = C.small("invf", [128, 128])
    C._stages = [P.sb([128, 1968], F32, f"wstage{i}") for i in range(2)]
    Win = load_w(C, "w_in", 8, 1968, g=gpre, chunk=1968)
    Wuq = load_w(C, "w_uq", 2, 768, g=gq, chunk=768)
    Wukv = load_w(C, "w_ukv", 1, 1024, g=gkv, chunk=1024)
    sin_t, cos_t = rope_tables(C, pos_i, invf)
    S = NormScr(C, 1024, "a")
    hr = [P.sb([128, 1024], F32, f"hr{j}") for j in range(2)]
    xTt = [P.sb([128, 8, 128], BF16, f"xTt{j}") for j in range(2)]
    pp = [P.ps([128, 512], F32, f"pp{i}") for i in range(4)]
    pg = P.ps([128, 512], F32, "pg")
    pjs = [P.sb([128, 1968], F32, f"pj{i}") for i in range(2)]
    cqT = P.sb([128, 2, 128], BF16, "cqT")
    ckvT = P.sb([128, 128], BF16, "ckvT")
    aug = P.sb([32, 128], F32, "aug")
    P.v('dve', 'memset', aug[:], 1.0, writes=[aug])
    qos = [P.sb([128, 8, 96], BF16, f"qo{i}") for i in range(2)]
    kvos = [P.sb([128, 1024], BF16, f"kvo{i}") for i in range(2)]
    kpos = [P.sb([128, 32], BF16, f"kpo{i}") for i in range(2)]
    gbs = [P.sb([128, 1024], BF16, f"gb{i}") for i in range(2)]
    las = [P.sb([128, 256], F32, f"la{i}") for i in range(2)]
    ex = P.sb([128, 256], F32, "ex")
    tq = [P.sb([128, 4, 16], F32, f"tq{i}") for i in range(4)]
    tk = [P.sb([128, 16], F32, f"tk{i}") for i in range(4)]
    for j in range(NT):
        r0 = j * 128
        ht = hr[j % 2]
        xT = xTt[j % 2]
        pj = pjs[j % 2]
        P.dma('sp', ht[:], h[r0:r0 + 128, :])
        norm_T(C, ht[:], 1024, S, xT[:])
        for n in range(4):
            w = min(512, 1968 - n * 512)
            for k in range(8):
                P.mm(pp[n][:, :w], xT[:, k, :], Win.sl(k, n * 512, n * 512 + w), start=(k == 0), stop=(k == 7))
        for n in range(4):
            w = min(512, 1968 - n * 512)
            if n % 2 == 0:
                P.act(pj[:, n * 512:n * 512 + w], pp[n][:, :w], AF.Copy)
            else:
                P.v('dve', 'tensor_copy', pj[:, n * 512:n * 512 + w], pp[n][:, :w], reads=[pp[n]], writes=[pj])
        P.dma('pool', r_o[r0:r0 + 128, :], pj[:, 1456:1968], W=[("r_o", j)], is_out=True)
        gb = gbs[j % 2]
        P.v('pool', 'tensor_copy', gb[:], pj[:, 416:1440], reads=[pj], writes=[gb])
        P.dma('pool', g_o[r0:r0 + 128, :], gb[:], W=[("g_o", j)], is_out=True)
        rstd_of(C, pj[:, 0:256], 256, S, S.ss[:], S.rstd[:], S.junk[:, :256])
        P.act(S.xb[:, :256], pj[:, 0:256], AF.Copy, scale=S.rstd[:])
        for c in range(2):
            P.tr(S.pt[:, c, :], S.xb[:, c * 128:(c + 1) * 128], C.idb[:])
        P.v('dve', 'tensor_copy', cqT[:], S.pt[:, 0:2, :], reads=[S.pt], writes=[cqT])
        for hf in range(2):
            for kc in range(2):
                P.mm(pp[hf][:, :384], cqT[:, kc, :], Wuq.sl(kc, hf * 384, (hf + 1) * 384),
                     start=(kc == 0), stop=(kc == 1))
        qo = qos[j % 2]
        cs4 = cos_t[:, j, 0:64].rearrange("p (h f) -> p h f", h=4)
        sn4 = sin_t[:, j, 0:64].rearrange("p (h f) -> p h f", h=4)
        for hf in range(2):
            pq = pp[hf][:, :384].rearrange("p (h d) -> p h d", h=4)
            P.act(qo[:, 4 * hf:4 * hf + 4, 0:64], pq[:, :, 0:64], AF.Copy)
            rope_apply(C, pq[:, :, 64:80], pq[:, :, 80:96], cs4, sn4,
                       qo[:, 4 * hf:4 * hf + 4, 64:80], qo[:, 4 * hf:4 * hf + 4, 80:96],
                       [t[:] for t in tq], None)
        P.dma('sp', q_o[r0:r0 + 128, :], qo[:].rearrange("p h d -> p (h d)"), W=[("q_o", j)], is_out=True)
        rstd_of(C, pj[:, 256:384], 128, S, S.ss[:], S.rstd[:], S.junk[:, :128])
        P.act(S.xb[:, :128], pj[:, 256:384], AF.Copy, scale=S.rstd[:])
        P.tr(S.pt[:, 0, :], S.xb[:, 0:128], C.idb[:])
        P.v('dve', 'tensor_copy', ckvT[:], S.pt[:, 0, :], reads=[S.pt], writes=[ckvT])
        for n in range(2):
            P.mm(pp[2 + n][:], ckvT[:], Wukv.sl(0, n * 512, (n + 1) * 512), start=True, stop=True)
        kvo = kvos[j % 2]
        P.act(kvo[:, 0:512], pp[2][:], AF.Copy)
        P.v('dve', 'tensor_copy', kvo[:, 512:1024], pp[3][:], reads=[pp[3]], writes=[kvo])
        P.dma('sp', kv_o[r0:r0 + 128, :], kvo[:], W=[("kv_o", j)], is_out=True)
        kpo = kpos[j % 2]
        rope_apply(C, pj[:, 384:400], pj[:, 400:416], cos_t[:, j, 0:16], sin_t[:, j, 0:16],
                   kpo[:, 0:16], kpo[:, 16:32], [t[:] for t in tk], None)
        P.dma('sp', kpe_o[r0:r0 + 128, :], kpo[:], W=[("kpe_o", j)], is_out=True)
        if 'gate' in SKIP:
            continue
        P.tr(pg[0:16, 256:384], pj[:, 1440:1456], C.idf[:], W=[("pg", "t")])
        P.v('dve', 'tensor_copy', aug[0:16, :], pg[0:16, 256:384], reads=[("pg", "t")], writes=[aug])
        P.mm(pg[:, 0:256], aug[0:17, :], wga[0:17, :], start=True, stop=True, W=[("pg", "m")])
        P.act(ex[:], pg[:, 0:256], AF.Exp, scale=-1.0, R=[("pg", "m")])
        P.act(ex[:], ex[:], AF.Ln, bias=1.0)
        la = las[j % 2]
        P.v('dve', 'tensor_scalar_mul', la[:], ex[:], -1.0 / 16.0, reads=[ex], writes=[la])
        P.dma('sp', la_o[r0:r0 + 128, :], la[:], W=[("la_o", j)], is_out=True)
    P.emit()
    return C.nc


SEQ = 16384
NCH = SEQ // 128


def build_m_even():
    C = Ctx()
    P = C.P
    qT_d = C.din("qT", [96, SEQ], BF16)
    kT_d = C.din("kT", [96, SEQ], BF16)
    v_d = C.din("v", [128, NCH * 64], BF16)
    tri_d = C.din("trib", [128, 128], BF16)
    o_d = C.dout("o", [128, NCH * 64])
    gqT_d = C.din("gqT", [64, SEQ], BF16)
    gkT_d = C.din("gkT", [64, SEQ], BF16)
    gk_d = C.din("gk", [128, NCH * 64], BF16)
    gv_d = C.din("gv", [128, NCH * 64], BF16)
    la_d = C.din("la", [128, NCH * 64])
    go_d = C.dout("go", [128, NCH * 64])
    triu = C.small("triu", [128, 128])
    tril = C.small("tril", [128, 128])
    QT = P.sb([96, SEQ], BF16, "QT")
    KT = P.sb([96, SEQ], BF16, "KT")
    V1 = P.sb([128, NCH, 65], BF16, "V1")
    trib = P.sb([128, 128], BF16, "trib_sb")
    P.dma('sp', trib[:], tri_d)
    nq = max(1, SEQ // 4096)
    cq = SEQ // nq
    for i in range(nq):
        P.dma('sp', QT[:, i * cq:(i + 1) * cq], qT_d[:, i * cq:(i + 1) * cq])
        P.dma('pool', KT[:, i * cq:(i + 1) * cq], kT_d[:, i * cq:(i + 1) * cq])
    P.v('dve', 'memset', V1[:], 1.0, writes=[V1])
    P.dma('sp', V1[:, :, 0:64], v_d.rearrange("p (t d) -> p t d", d=64))
    pS = [P.ps([128, 512], F32, f"pS{i}") for i in range(3)]
    pO = [P.ps([128, 512], F32, f"pO{i}") for i in range(4)]
    PTs = [P.sb([128, 512], BF16, f"PT{i}") for i in range(3)]
    obs = [P.sb([128, 4, 64], F32, f"ob{i}") for i in range(2)]
    rinv = P.sb([128, 4], F32, "rinv")
    scale = 96.0 ** -0.5
    it = 0
    for qb in range(SEQ // 512):
        for kt in range(4 * qb + 4):
            ps = pS[it % 3]
            pt = PTs[it % 3]
            it += 1
            jd = max(0, kt - 4 * qb)
            c0 = jd * 128
            P.mm(ps[:, c0:512], KT[:, kt * 128:(kt + 1) * 128], QT[:, qb * 512 + c0:(qb + 1) * 512],
                 start=True, stop=True)
            P.act(pt[:, c0:512], ps[:, c0:512], AF.Exp, scale=scale)
            if kt >= 4 * qb:
                P.v('pool', 'tensor_mul', pt[:, c0:c0 + 128], pt[:, c0:c0 + 128], trib[:],
                    reads=[pt, trib], writes=[pt])
            for j in range(jd, 4):
                P.mm(pO[j][:, 0:65], pt[:, j * 128:(j + 1) * 128], V1[:, kt, :],
                     start=(kt == 0), stop=(kt == 4 * qb + j))
        ob = obs[qb % 2]
        for j in range(4):
            P.v('dve', 'reciprocal', rinv[:, j:j + 1], pO[j][:, 64:65], reads=[pO[j]], writes=[rinv])
            P.act(ob[:, j, :], pO[j][:, 0:64], AF.Copy, scale=rinv[:, j:j + 1])
        P.dma('sp', o_d[:, qb * 256:(qb + 1) * 256], ob[:].rearrange("p j d -> p (j d)"),
              W=[("o_d", qb)], is_out=True)
    GC = 8
    S = P.sb([64, 64], F32, "gS")
    Sb = P.sb([64, 64], BF16, "gSb")
    P.v('dve', 'memset', S[:], 0.0, writes=[S])
    P.v('dve', 'memset', Sb[:], 0.0, writes=[Sb])
    bufs = []
    for i in range(2):
        bufs.append(dict(q=P.sb([64, GC * 128], BF16, f"gq{i}"), k=P.sb([64, GC * 128], BF16, f"gkT{i}"),
                         kt=P.sb([128, GC * 64], BF16, f"gk{i}"), v=P.sb([128, GC * 64], BF16, f"gv{i}"),
                         la=P.sb([128, GC * 64], F32, f"gla{i}"), o=P.sb([128, GC * 64], F32, f"go{i}")))
    eb = [P.sb([64, 128], F32, f"eb{i}") for i in range(2)]
    enb = [P.sb([64, 128], F32, f"enb{i}") for i in range(2)]
    ebr = [P.sb([128, 64], F32, f"ebr{i}") for i in range(2)]
    qs = [P.sb([64, 128], BF16, f"qs{i}") for i in range(2)]
    ks = [P.sb([64, 128], BF16, f"ks{i}") for i in range(2)]
    kh = [P.sb([128, 64], BF16, f"kh{i}") for i in range(2)]
    AT = [P.sb([128, 128], BF16, f"AT{i}") for i in range(2)]
    pA, pB, pC, pD, pE = pS[0], pS[1], pS[2], pO[0], pO[1]
    for g in range(NCH // GC):
        B = bufs[g % 2]
        P.dma('sp', B['q'][:], gqT_d[:, g * GC * 128:(g + 1) * GC * 128])
        P.dma('sp', B['k'][:], gkT_d[:, g * GC * 128:(g + 1) * GC * 128])
        P.dma('pool', B['kt'][:], gk_d[:, g * GC * 64:(g + 1) * GC * 64])
        P.dma('pool', B['v'][:], gv_d[:, g * GC * 64:(g + 1) * GC * 64])
        P.dma('sp', B['la'][:], la_d[:, g * GC * 64:(g + 1) * GC * 64])
        for c in range(GC):
            i = c % 2
            la_c = B['la'][:, c * 64:(c + 1) * 64]
            v_c = B['v'][:, c * 64:(c + 1) * 64]
            P.mm(pA[0:64, 0:128], la_c, triu[:], start=True, stop=True)
            P.mm(pB[:, 0:64], tril[:], la_c, start=True, stop=True)
            P.act(eb[i][:], pA[0:64, 0:128], AF.Exp)
            P.act(enb[i][:], pA[0:64, 0:128], AF.Exp, scale=-1.0)
            P.act(ebr[i][:], pB[:, 0:64], AF.Exp)
            P.v('dve', 'scalar_tensor_tensor', qs[i][:], B['q'][:, c * 128:(c + 1) * 128], 0.125, eb[i][:],
                ALU.mult, ALU.mult, reads=[B['q'], eb[i]], writes=[qs[i]])
            P.v('pool', 'tensor_mul', ks[i][:], B['k'][:, c * 128:(c + 1) * 128], enb[i][:],
                reads=[B['k'], enb[i]], writes=[ks[i]])
            P.v('pool', 'tensor_mul', kh[i][:], B['kt'][:, c * 64:(c + 1) * 64], ebr[i][:],
                reads=[B['kt'], ebr[i]], writes=[kh[i]])
            P.mm(pC[:, 0:128], ks[i][:], qs[i][:], start=True, stop=True)
            P.v('dve', 'tensor_mul', AT[i][:], pC[:, 0:128], triu[:], reads=[pC, triu], writes=[AT[i]])
            P.mm(pD[:, 0:64], AT[i][:], v_c, start=True, stop=False)
            P.mm(pD[:, 0:64], qs[i][:], Sb[:], start=False, stop=True)
            P.mm(pE[0:64, 0:64], kh[i][:], v_c, start=True, stop=True)
            P.act(B['o'][:, c * 64:(c + 1) * 64], pD[:, 0:64], AF.Copy)
            P.v('dve', 'scalar_tensor_tensor', S[:], S[:], eb[i][:, 127:128], pE[0:64, 0:64],
                ALU.mult, ALU.add, reads=[S, eb[i], pE], writes=[S])
            P.v('dve', 'tensor_copy', Sb[:], S[:], reads=[S], writes=[Sb])
        P.dma('sp', go_d[:, g * GC * 64:(g + 1) * GC * 64], B['o'][:], W=[("go_d", g)], is_out=True)
    P.emit()
    return C.nc


def build_cmix_even():
    C = Ctx()
    P = C.P
    h = C.din("h", [TOK, 1024])
    a_d = C.din("a", [TOK, 512])
    go_d = C.din("go", [TOK, 512])
    r_d = C.din("r", [TOK, 512])
    ho = C.dout("ho", [TOK, 1024])
    C.ident()
    ggla = C.small("ggla", [128, 128])
    gpo = C.small("gpo", [128, 1024], q='pool')
    C._stages = [P.sb([128, 1024], F32, f"wstage{i}") for i in range(3)]
    Wout = load_w(C, "w_out", 8, 1024, chunk=1024)
    S = NormScr(C, 1024, "a")
    py = P.ps([128, 1024], F32, "py")
    bufs = []
    for i in range(2):
        bufs.append(dict(h=P.sb([128, 1024], F32, f"ht{i}"), a=P.sb([128, 512], F32, f"at{i}"),
                         g=P.sb([128, 512], F32, f"got{i}"), r=P.sb([128, 512], F32, f"rt{i}")))
    cat = P.sb([128, 1024], BF16, "cat")
    catT = P.sb([128, 8, 128], BF16, "catT")
    ss4 = P.sb([128, 4], F32, "ss4")
    rstd4 = P.sb([128, 4], F32, "rstd4")
    sr = P.sb([128, 512], F32, "sr")
    gn = P.sb([128, 512], F32, "gn")
    for j in range(NT):
        r0 = j * 128
        B = bufs[j % 2]
        P.dma('sp', B['h'][:], h[r0:r0 + 128, :])
        P.dma('pool', B['a'][:], a_d[r0:r0 + 128, :])
        P.dma('sp', B['g'][:], go_d[r0:r0 + 128, :])
        P.dma('pool', B['r'][:], r_d[r0:r0 + 128, :])
        P.v('pool', 'tensor_copy', cat[:, 0:512], B['a'][:], reads=[B['a']], writes=[cat])
        for hh in range(4):
            P.act(S.junk[:, :128], B['g'][:, hh * 128:(hh + 1) * 128], AF.Square, scale=128.0 ** -0.5,
                  accum_out=ss4[:, hh:hh + 1])
        P.act(rstd4[:], ss4[:], AF.Sqrt, bias=EPS)
        P.v('dve', 'reciprocal', rstd4[:], rstd4[:], reads=[rstd4], writes=[rstd4])
        P.act(sr[:], B['r'][:], AF.Silu)
        for hh in range(4):
            P.v('dve', 'scalar_tensor_tensor', gn[:, hh * 128:(hh + 1) * 128], B['g'][:, hh * 128:(hh + 1) * 128],
                rstd4[:, hh:hh + 1], ggla[:], ALU.mult, ALU.mult, reads=[B['g'], rstd4, ggla], writes=[gn])
        P.v('pool', 'tensor_mul', cat[:, 512:1024], gn[:], sr[:], reads=[gn, sr], writes=[cat])
        for c in range(8):
            P.tr(S.pt[:, c, :], cat[:, c * 128:(c + 1) * 128], C.idb[:])
        P.v('dve', 'tensor_copy', catT[:], S.pt[:], reads=[S.pt], writes=[catT])
        for n in range(2):
            for k in range(8):
                P.mm(py[:, n * 512:(n + 1) * 512], catT[:, k, :], Wout.sl(k, n * 512, (n + 1) * 512),
                     start=(k == 0), stop=(k == 7))
        postnorm_res(C, py[:], B['h'][:], gpo, S, S.junk)
        P.dma('sp', ho[r0:r0 + 128, :], B['h'][:], W=[("ho", r0)], is_out=True)
    P.emit()
    return C.nc


def build_a_odd():
    C = Ctx()
    P = C.P
    h = C.din("h", [TOK, 1024])
    hh_d = C.din("hh", [128, 1024])
    z_o = C.dout("z", [TOK, 1024])
    xc_o = C.dout("xcT", [1024, TOK])
    q_o = C.dout("qT", [1024, TOK], BF16)
    k_o = C.dout("kT", [1024, TOK], BF16)
    v_o = C.dout("vT", [1024, TOK], BF16)
    gi_o = C.dout("gi", [8, TOK])
    gl_o = C.dout("gl", [8, TOK])
    C.ident()
    gpre = C.small("gpre", [128, 8])
    cw = C.small("convw", [128, 32])
    cb = C.small("convb", [128, 8])
    bg = C.small("bg", [8, 1])
    C._stages = [P.sb([128, 2048], F32, f"wstage{i}") for i in range(2)]
    Win = load_w(C, "w_in", 8, 2048, g=gpre, chunk=2048)
    Wq = load_w(C, "wq", 8, 256, chunk=256)
    Wk = load_w(C, "wk", 8, 256, chunk=256)
    Wv = load_w(C, "wv", 8, 256, chunk=256)
    Wg = load_w(C, "wg", 24, 8, chunk=8)
    S = NormScr(C, 1024, "a")
    hr = [P.sb([128, 1024], F32, f"hr{j}") for j in range(2)]
    xT = P.sb([128, 8, 512], BF16, "xT")
    xm = [P.sb([128, 515], F32, f"xm{cc}") for cc in range(8)]
    xmb = [P.sb([128, 512], BF16, f"xmb{cc}") for cc in range(8)]
    xcb = [P.sb([128, 512], BF16, f"xcb{cc}") for cc in range(8)]
    qkvb = [[P.sb([128, 512], BF16, f"qkv{t}_{i}") for i in range(8)] for t in range(3)]
    acc = [P.sb([128, 512], F32, f"acc{i}") for i in range(2)]
    xcf = [P.sb([128, 512], F32, f"xcf{i}") for i in range(2)]
    zt = [P.sb([128, 1024], F32, f"zt{i}") for i in range(2)]
    gi = P.sb([8, 512], F32, "gi_sb")
    gl = P.sb([8, 512], F32, "gl_sb")
    pr = [P.ps([128, 512], F32, f"pr{i}") for i in range(3)]
    pz = P.ps([128, 1024], F32, "pz")
    pgt = P.ps([128, 512], F32, "pgt")
    P.dma('sp', hr[0][:], hh_d)
    norm_T(C, hr[0][:], 1024, S, xT[:, :, 0:128])
    npr = 0
    for cc in range(8):
        ps = pr[npr % 3]
        npr += 1
        for k in range(8):
            P.mm(ps[:, 0:128], Win.sl(k, cc * 128, (cc + 1) * 128), xT[:, k, 0:128], start=(k == 0), stop=(k == 7))
        P.act(xm[cc][:, 0:3], ps[:, 125:128], AF.Copy)
    nh = 1
    for g in range(TOK // 512):
        c0 = g * 512
        for j in range(4):
            ht = hr[nh % 2]
            nh += 1
            P.dma('sp', ht[:], h[c0 + j * 128:c0 + (j + 1) * 128, :])
            norm_T(C, ht[:], 1024, S, xT[:, :, j * 128:(j + 1) * 128])
        for cc in range(8):
            ps = pr[npr % 3]
            npr += 1
            for k in range(8):
                P.mm(ps[:], Win.sl(k, cc * 128, (cc + 1) * 128), xT[:, k, :], start=(k == 0), stop=(k == 7))
            P.act(xm[cc][:, 3:515], ps[:], AF.Copy)
            P.v('dve', 'tensor_copy', xmb[cc][:], xm[cc][:, 3:515], reads=[xm[cc]], writes=[xmb[cc]])
            a = acc[cc % 2]
            P.v('dve', 'tensor_scalar', a[:], xm[cc][:, 0:512], cw[:, cc * 4:cc * 4 + 1], cb[:, cc:cc + 1],
                ALU.mult, ALU.add, reads=[xm[cc], cw, cb], writes=[a])
            for i in range(1, 4):
                P.v('dve', 'scalar_tensor_tensor', a[:], xm[cc][:, i:i + 512],
                    cw[:, cc * 4 + i:cc * 4 + i + 1], a[:], ALU.mult, ALU.add, reads=[xm[cc], cw, a], writes=[a])
            xf = xcf[cc % 2]
            P.act(xf[:], a[:], AF.Silu)
            P.dma('pool', xc_o[cc * 128:(cc + 1) * 128, c0:c0 + 512], xf[:], W=[("xc_o", g, cc)], is_out=True)
            P.v('pool', 'tensor_copy', xcb[cc][:], xf[:], reads=[xf], writes=[xcb[cc]])
            P.v('pool', 'tensor_copy', xm[cc][:, 0:3], xm[cc][:, 512:515], reads=[xm[cc]], writes=[xm[cc]])
        for j in range(4):
            z = zt[j % 2]
            for n in range(2):
                for k in range(8):
                    P.mm(pz[:, n * 512:(n + 1) * 512], xT[:, k, j * 128:(j + 1) * 128],
                         Win.sl(k, 1024 + n * 512, 1024 + (n + 1) * 512), start=(k == 0), stop=(k == 7))
            P.act(z[:, 0:512], pz[:, 0:512], AF.Copy)
            P.v('dve', 'tensor_copy', z[:, 512:1024], pz[:, 512:1024], reads=[pz], writes=[z])
            P.dma('sp', z_o[c0 + j * 128:c0 + (j + 1) * 128, :], z[:], W=[("z_o", g, j)], is_out=True)
        for t, (W_, src, dst) in enumerate(((Wq, xcb, q_o), (Wk, xcb, k_o), (Wv, xmb, v_o))):
            for hd in range(4):
                for ec in range(2):
                    ps = pr[npr % 3]
                    npr += 1
                    for dc in range(2):
                        P.mm(ps[:], W_.sl(hd * 2 + dc, ec * 128, (ec + 1) * 128), src[hd * 2 + dc][:],
                             start=(dc == 0), stop=(dc == 1))
                    ob = qkvb[t][hd * 2 + ec]
                    if (hd * 2 + ec) % 2 == 0:
                        P.act(ob[:], ps[:], AF.Copy)
                    else:
                        P.v('dve', 'tensor_copy', ob[:], ps[:], reads=[ps], writes=[ob])
                    r0 = hd * 256 + ec * 128
                    P.dma('sp' if t != 1 else 'pool', dst[r0:r0 + 128, c0:c0 + 512], ob[:],
                          W=[("qkv_o", t, g, hd, ec)], is_out=True)
        n = 0
        for hd in range(4):
            for t in range(3):
                for ec in range(2):
                    P.mm(pgt[0:8, :], Wg.sl(hd * 6 + t * 2 + ec, 0, 8), qkvb[t][hd * 2 + ec][:],
                         start=(n == 0), stop=(n == 23))
                    n += 1
        P.act(gi[:], pgt[0:8, :], AF.Identity, bias=bg[:, 0:1])
        P.act(gl[:], gi[:], AF.Exp, scale=-1.0)
        P.act(gl[:], gl[:], AF.Ln, bias=1.0)
        P.v('dve', 'tensor_scalar_mul', gl[:], gl[:], -1.0, reads=[gl], writes=[gl])
        P.dma('sp', gi_o[:, c0:c0 + 512], gi[:], W=[("gi_o", g)], is_out=True)
        P.dma('sp', gl_o[:, c0:c0 + 512], gl[:], W=[("gl_o", g)], is_out=True)
    P.emit()
    return C.nc


def build_m_odd():
    C = Ctx()
    P = C.P
    nch = NCH
    qT_d = C.din("qT", [256, SEQ], BF16)
    kT_d = C.din("kT", [256, SEQ], BF16)
    kt_d = C.din("ktok", [128, nch * 256], BF16)
    v_d = C.din("v", [128, nch * 128], BF16)
    ho_d = C.dout("ho", [128, nch * 128])
    C.ident()
    idf = C.idf
    ii = C.small("ii", [128, nch])
    lf = C.small("lf", [128, nch])
    triu = C.small("triu", [128, 128])
    ones = P.sb([128, 128], F32, "ones")
    P.v('dve', 'memset', ones[:], 1.0, writes=[ones])
    pX = P.ps([128, 512], F32, "pX")
    pN = P.ps([128, 512], F32, "pN")
    pQ = P.ps([128, 512], F32, "pQ")
    pI = P.ps([128, 512], F32, "pI")
    pR = P.ps([128, 512], F32, "pR")
    pU = P.ps([128, 512], F32, "pU")

    def sbt(name, shape=None, dt=F32):
        return P.sb(shape or [128, nch], dt, name)

    def dv(name, *a, reads, writes, eng='dve'):
        P.v(eng, name, *a, reads=reads, writes=writes)

    b_all, u_all, c_all = sbt("b_all"), sbt("u_all"), sbt("c_all")
    P.mm(pX[:, 0:nch], triu[:], lf[:], start=True, stop=True)
    dv('tensor_copy', b_all[:], pX[:, 0:nch], reads=[pX], writes=[b_all])
    dv('tensor_sub', u_all[:], ii[:], b_all[:], reads=[ii, b_all], writes=[u_all])

    def prefix_max(src, dst, rows, n):
        sh = 1
        while sh < n:
            dv('tensor_copy', dst[0:rows, 0:sh], src[0:rows, 0:sh], reads=[src], writes=[dst])
            dv('tensor_max', dst[0:rows, sh:n], src[0:rows, sh:n], src[0:rows, 0:n - sh], reads=[src], writes=[dst])
            src, dst = dst, src
            sh *= 2
        return src

    x0, x1 = sbt("x0", [128, 128]), sbt("x1", [128, 128])
    P.tr(pX[0:nch, 0:128], u_all[:], idf[:])
    dv('tensor_copy', x0[0:nch, :], pX[0:nch, 0:128], reads=[pX], writes=[x0])
    cT = prefix_max(x0, x1, nch, 128)
    P.tr(pX[:, 0:nch], cT[0:nch, :], idf[0:nch, 0:nch])
    dv('tensor_copy', c_all[:], pX[:, 0:nch], reads=[pX], writes=[c_all])
    bT = sbt("bT", [128, 128])
    P.tr(pX[0:nch, 0:128], b_all[:], idf[:])
    dv('tensor_copy', bT[0:nch, :], pX[0:nch, 0:128], reads=[pX], writes=[bT])
    PB = sbt("PB", [128, 1])
    P.mm(pX[0:nch, 0:1], triu[0:nch, 0:nch], bT[0:nch, 127:128], start=True, stop=True)
    dv('tensor_copy', PB[0:nch, :], pX[0:nch, 0:1], reads=[pX], writes=[PB])
    wcol = sbt("wcol", [128, 1])
    dv('tensor_sub', wcol[0:nch, :], cT[0:nch, 127:128], PB[0:nch, :], reads=[cT, PB], writes=[wcol])
    dv('tensor_add', wcol[0:nch, :], wcol[0:nch, :], bT[0:nch, 127:128], reads=[wcol, bT], writes=[wcol])
    r0, r1 = sbt("r0", [1, 128]), sbt("r1", [1, 128])
    pbrow, brow, mrow, mprow = sbt("pbrow", [1, 128]), sbt("brow", [1, 128]), sbt("mrow", [1, 128]), sbt("mprow", [1, 128])
    for col, dst in ((wcol[0:nch, 0:1], r0), (PB[0:nch, 0:1], pbrow), (bT[0:nch, 127:128], brow)):
        P.tr(pX[0:1, 0:nch], col, idf[0:nch, 0:nch])
        dv('tensor_copy', dst[0:1, 0:nch], pX[0:1, 0:nch], reads=[pX], writes=[dst])
    pm = prefix_max(r0, r1, 1, nch)
    dv('tensor_scalar_max', mrow[0:1, 0:nch], pm[0:1, 0:nch], 0.0, reads=[pm], writes=[mrow])
    dv('tensor_add', mrow[0:1, 0:nch], mrow[0:1, 0:nch], pbrow[0:1, 0:nch], reads=[mrow, pbrow], writes=[mrow])
    dv('memset', mprow[:], 0.0, reads=[], writes=[mprow])
    if nch > 1:
        dv('tensor_copy', mprow[0:1, 1:nch], mrow[0:1, 0:nch - 1], reads=[mrow], writes=[mprow])
    mp_all, mn_all, be_all = sbt("mp_all"), sbt("mn_all"), sbt("be_all")
    for row, dst in ((mprow, mp_all), (mrow, mn_all), (brow, be_all)):
        P.mm(pX[:, 0:nch], ones[0:1, :], row[0:1, 0:nch], start=True, stop=True)
        dv('tensor_copy', dst[:], pX[:, 0:nch], reads=[pX], writes=[dst])
    mx_all, m_all, nmx_all, wi_all, em_all = sbt("mx_all"), sbt("m_all"), sbt("nmx_all"), sbt("wi_all"), sbt("em_all")
    d_all, ws_all, dec_all = sbt("d_all"), sbt("ws_all"), sbt("dec_all")
    dv('tensor_max', mx_all[:], c_all[:], mp_all[:], reads=[c_all, mp_all], writes=[mx_all])
    dv('tensor_add', m_all[:], b_all[:], mx_all[:], reads=[b_all, mx_all], writes=[m_all])
    dv('tensor_scalar_mul', nmx_all[:], mx_all[:], -1.0, reads=[mx_all], writes=[nmx_all])
    dv('tensor_sub', wi_all[:], mp_all[:], mx_all[:], reads=[mp_all, mx_all], writes=[wi_all])
    P.act(wi_all[:], wi_all[:], AF.Exp)
    dv('tensor_scalar_mul', wi_all[:], wi_all[:], 0.0625, reads=[wi_all], writes=[wi_all])
    P.act(em_all[:], m_all[:], AF.Exp, scale=-1.0)
    dv('tensor_sub', d_all[:], be_all[:], mn_all[:], reads=[be_all, mn_all], writes=[d_all])
    dv('tensor_add', ws_all[:], u_all[:], d_all[:], reads=[u_all, d_all], writes=[ws_all])
    P.act(ws_all[:], ws_all[:], AF.Exp)
    dv('tensor_add', dec_all[:], mp_all[:], d_all[:], reads=[mp_all, d_all], writes=[dec_all])
    P.act(dec_all[:], dec_all[:], AF.Exp)
    GC = min(8, nch)
    Cn = [P.sb([128, 129], F32, f"Cn{dc}") for dc in range(2)]
    Cnb = [P.sb([128, 129], BF16, f"Cnb{dc}") for dc in range(2)]
    for dc in range(2):
        dv('memset', Cn[dc][:], 0.0, reads=[], writes=[Cn[dc]])
        dv('memset', Cnb[dc][:], 0.0, reads=[], writes=[Cnb[dc]])
    bufs = []
    for i in range(2):
        B = dict(q=P.sb([128, 2, GC * 128], BF16, f"qg{i}"), k=P.sb([128, 2, GC * 128], BF16, f"kg{i}"),
                 kt=P.sb([128, GC * 256], BF16, f"ktg{i}"), v1=P.sb([128, GC, 129], BF16, f"v1g{i}"),
                 ho=P.sb([128, GC, 128], F32, f"hog{i}"))
        dv('memset', B['v1'][:], 1.0, reads=[], writes=[B['v1']], eng='pool')
        bufs.append(B)
    dN = [sbt(f"dN{i}", [128, 128]) for i in range(2)]
    E = [sbt(f"E{i}", [128, 128]) for i in range(2)]
    WT = [sbt(f"WT{i}", [128, 128], BF16) for i in range(2)]
    isb = [sbt(f"isb{i}", [128, 129]) for i in range(2)]
    tot = [sbt(f"tot{i}", [128, 129]) for i in range(2)]
    den = [sbt(f"den{i}", [128, 1]) for i in range(2)]
    kw = [sbt(f"kw{i}", [128, 256], BF16) for i in range(2)]
    for g in range(nch // GC):
        B = bufs[g % 2]
        t0 = g * GC * 128
        for dc in range(2):
            P.dma('sp', B['q'][:, dc, :], qT_d[dc * 128:(dc + 1) * 128, t0:t0 + GC * 128])
            P.dma('pool', B['k'][:, dc, :], kT_d[dc * 128:(dc + 1) * 128, t0:t0 + GC * 128])
        P.dma('sp', B['kt'][:], kt_d[:, g * GC * 256:(g + 1) * GC * 256])
        P.dma('pool', B['v1'][:, :, 0:128], v_d[:, g * GC * 128:(g + 1) * GC * 128].rearrange("p (c e) -> p c e", e=128))
        for c in range(GC):
            j = g * GC + c
            i = c % 2
            sl = slice(c * 128, (c + 1) * 128)
            dv('tensor_scalar_mul', dN[i][:], idf[:], nmx_all[:, j:j + 1], reads=[idf, nmx_all], writes=[dN[i]])
            P.mm(pN[:, 0:128], ones[:], dN[i][:], start=True, stop=True)
            P.act(E[i][:], pN[:, 0:128], AF.Exp, bias=u_all[:, j:j + 1])
            dv('tensor_mul', E[i][:], E[i][:], triu[:], reads=[E[i], triu], writes=[E[i]], eng='pool')
            for dc in range(2):
                P.mm(pQ[:, 0:128], B['k'][:, dc, sl], B['q'][:, dc, sl], start=(dc == 0), stop=(dc == 1))
            dv('scalar_tensor_tensor', WT[i][:], pQ[:, 0:128], 0.0625, E[i][:], ALU.mult, ALU.mult,
               reads=[pQ, E[i]], writes=[WT[i]])
            P.mm(pI[:, 0:129], WT[i][:], B['v1'][:, c, :], start=True, stop=True)
            for dc in range(2):
                P.mm(pR[:, 0:129], B['q'][:, dc, sl], Cnb[dc][:], start=(dc == 0), stop=(dc == 1))
            P.act(isb[i][:], pI[:, 0:129], AF.Copy)
            dv('scalar_tensor_tensor', tot[i][:], pR[:, 0:129], wi_all[:, j:j + 1], isb[i][:], ALU.mult, ALU.add,
               reads=[pR, wi_all, isb[i]], writes=[tot[i]])
            dv('tensor_scalar_mul', den[i][:], tot[i][:, 128:129], -1.0, reads=[tot[i]], writes=[den[i]])
            dv('tensor_max', den[i][:], den[i][:], tot[i][:, 128:129], reads=[den[i], tot[i]], writes=[den[i]])
            dv('tensor_max', den[i][:], den[i][:], em_all[:, j:j + 1], reads=[den[i], em_all], writes=[den[i]])
            dv('reciprocal', den[i][:], den[i][:], reads=[den[i]], writes=[den[i]])
            P.act(B['ho'][:, c, :], tot[i][:, 0:128], AF.Copy, scale=den[i][:])
            dv('tensor_scalar_mul', kw[i][:], B['kt'][:, c * 256:(c + 1) * 256], ws_all[:, j:j + 1],
               reads=[B['kt'], ws_all], writes=[kw[i]], eng='pool')
            for dc in range(2):
                P.mm(pU[:, dc * 129:(dc + 1) * 129], kw[i][:, dc * 128:(dc + 1) * 128], B['v1'][:, c, :],
                     start=True, stop=True)
            for dc in range(2):
                dv('scalar_tensor_tensor', Cn[dc][:], Cn[dc][:], dec_all[:, j:j + 1], pU[:, dc * 129:(dc + 1) * 129],
                   ALU.mult, ALU.add, reads=[Cn[dc], dec_all, pU], writes=[Cn[dc]])
                P.act(Cnb[dc][:], Cn[dc][:], AF.Copy)
        P.dma('sp', ho_d[:, g * GC * 128:(g + 1) * GC * 128], B['ho'][:].rearrange("p c e -> p (c e)"),
              W=[("ho_d", g)], is_out=True)
    P.emit()
    return C.nc


def build_cmix_odd():
    C = Ctx()
    P = C.P
    h = C.din("h", [TOK, 1024])
    hc_d = C.din("hc", [TOK, 1024])
    xc_d = C.din("xc", [TOK, 1024])
    z_d = C.din("z", [TOK, 1024])
    ho = C.dout("ho", [TOK, 1024])
    C.ident()
    ghn = C.small("ghn", [128, 1024])
    skp = C.small("skip", [128, 1024], q='pool')
    gpo = C.small("gpo", [128, 1024], q='pool')
    C._stages = [P.sb([128, 1024], F32, f"wstage{i}") for i in range(3)]
    Wout = load_w(C, "w_out", 8, 1024, chunk=1024)
    S = NormScr(C, 1024, "a")
    py = P.ps([128, 1024], F32, "py")
    bufs = []
    for i in range(2):
        bufs.append(dict(h=P.sb([128, 1024], F32, f"ht{i}"), c=P.sb([128, 1024], F32, f"hct{i}"),
                         x=P.sb([128, 1024], F32, f"xct{i}"), z=P.sb([128, 1024], F32, f"zt{i}")))
    cat = P.sb([128, 1024], BF16, "cat")
    catT = P.sb([128, 8, 128], BF16, "catT")
    mu4 = P.sb([128, 4], F32, "mu4")
    ss4 = P.sb([128, 4], F32, "ss4")
    rstd4 = P.sb([128, 4], F32, "rstd4")
    cen = P.sb([128, 1024], F32, "cen")
    hn = P.sb([128, 1024], F32, "hn")
    sz = P.sb([128, 1024], F32, "sz")
    for j in range(NT):
        r0 = j * 128
        B = bufs[j % 2]
        P.dma('sp', B['h'][:], h[r0:r0 + 128, :])
        P.dma('pool', B['c'][:], hc_d[r0:r0 + 128, :])
        P.dma('sp', B['x'][:], xc_d[r0:r0 + 128, :])
        P.dma('pool', B['z'][:], z_d[r0:r0 + 128, :])
        for hh in range(4):
            sl = slice(hh * 256, (hh + 1) * 256)
            P.act(S.junk[:, :256], B['c'][:, sl], AF.Copy, scale=1.0 / 256.0, accum_out=mu4[:, hh:hh + 1])
            P.v('dve', 'tensor_scalar_sub', cen[:, sl], B['c'][:, sl], mu4[:, hh:hh + 1],
                reads=[B['c'], mu4], writes=[cen])
            P.act(S.junk[:, :256], cen[:, sl], AF.Square, scale=1.0 / 16.0, accum_out=ss4[:, hh:hh + 1])
        P.act(rstd4[:], ss4[:], AF.Sqrt, bias=EPS)
        P.v('dve', 'reciprocal', rstd4[:], rstd4[:], reads=[rstd4], writes=[rstd4])
        P.act(sz[:], B['z'][:], AF.Silu)
        for hh in range(4):
            sl = slice(hh * 256, (hh + 1) * 256)
            P.v('dve', 'scalar_tensor_tensor', hn[:, sl], cen[:, sl], rstd4[:, hh:hh + 1], ghn[:, sl],
                ALU.mult, ALU.mult, reads=[cen, rstd4, ghn], writes=[hn])
        P.v('pool', 'tensor_mul', B['x'][:], B['x'][:], skp[:], reads=[B['x'], skp], writes=[B['x']])
        P.v('pool', 'tensor_add', hn[:], hn[:], B['x'][:], reads=[hn, B['x']], writes=[hn])
        P.v('pool', 'tensor_mul', cat[:], hn[:], sz[:], reads=[hn, sz], writes=[cat])
        for c in range(8):
            P.tr(S.pt[:, c, :], cat[:, c * 128:(c + 1) * 128], C.idb[:])
        P.v('dve', 'tensor_copy', catT[:], S.pt[:], reads=[S.pt], writes=[catT])
        for n in range(2):
            for k in range(8):
                P.mm(py[:, n * 512:(n + 1) * 512], catT[:, k, :], Wout.sl(k, n * 512, (n + 1) * 512),
                     start=(k == 0), stop=(k == 7))
        postnorm_res(C, py[:], B['h'][:], gpo, S, S.junk)
        P.dma('sp', ho[r0:r0 + 128, :], B['h'][:], W=[("ho", r0)], is_out=True)
    P.emit()
    return C.nc


def wl(w):
    K, N = w.shape
    KC = K // 128
    return np.ascontiguousarray(w.reshape(KC, 128, N).transpose(1, 0, 2).reshape(128, KC * N))


def vl(g):
    return np.ascontiguousarray(g.reshape(-1, 128).T)


def bc(g, p=128):
    return np.ascontiguousarray(np.broadcast_to(g.reshape(1, -1), (p, g.size)))


_IDN = np.eye(128, dtype=np.float32)
_PROGS = {}


def prog(name, builder):
    if name not in _PROGS:
        _PROGS[name] = builder()
    return _PROGS[name]


def run(nc, maps):
    res = run_bass_kernel_spmd(nc, maps, core_ids=list(range(NCORES)))
    return res.results


def run_ffn(h, w1, w2, gpre, gpost):
    nc = prog("ffn", build_ffn)
    com = {"idn": _IDN, "gpre": vl(gpre), "gpo": bc(gpost), "w1": wl(w1), "w2": wl(w2)}
    maps = [dict(com, h=np.ascontiguousarray(h[c * TOK:(c + 1) * TOK])) for c in range(NCORES)]
    r = run(nc, maps)
    return np.concatenate([r[c]["ho"] for c in range(NCORES)], axis=0)


def run_xa(h, mem, wq, wk, wv, wo, gpre, gmem, gpost):
    nc = prog("xa", build_xa)
    com = {"idn": _IDN, "gpre": vl(gpre), "gmem": vl(gmem), "gpo": bc(gpost), "wq": wl(wq), "wk": wl(wk),
           "wv": wl(wv), "wo": wl(wo), "mem": np.ascontiguousarray(mem)}
    maps = [dict(com, h=np.ascontiguousarray(h[c * TOK:(c + 1) * TOK])) for c in range(NCORES)]
    r = run(nc, maps)
    return np.concatenate([r[c]["ho"] for c in range(NCORES)], axis=0)


def bf(x):
    import ml_dtypes
    return np.ascontiguousarray(x).astype(ml_dtypes.bfloat16)


def inv_freq_tab():
    inv = (np.float32(10000.0) ** (-np.arange(0, 32, 2, dtype=np.float32) / np.float32(32))).astype(np.float32)
    return np.ascontiguousarray(np.broadcast_to(np.tile(inv, 8)[None, :], (128, 128)))


def run_a_even(h, positions, w_in, gpre, g_q, w_uq, g_kv, w_ukv, w_gate, b_gate):
    nc = prog("a_even", build_a_even)
    com = {"idn": _IDN, "gpre": vl(gpre), "gq": vl(g_q), "gkv": vl(g_kv), "w_in": wl(w_in), "w_uq": wl(w_uq),
           "w_ukv": wl(w_ukv), "wga": np.ascontiguousarray(np.concatenate([w_gate, b_gate[None, :]], 0)),
           "invf": inv_freq_tab()}
    maps = []
    for c in range(NCORES):
        pos = positions[c * TOK:(c + 1) * TOK].reshape(NT, 128).T
        maps.append(dict(com, h=np.ascontiguousarray(h[c * TOK:(c + 1) * TOK]), pos=np.ascontiguousarray(pos)))
    r = run(nc, maps)
    return {k: np.concatenate([r[c][k] for c in range(NCORES)], axis=0) for k in ("q", "kv", "kpe", "gqkv", "la", "r")}


def tiles_pm(x):
    T = x.shape[0] // 128
    d = x.shape[1]
    return np.ascontiguousarray(x.reshape(T, 128, d).transpose(1, 0, 2).reshape(128, T * d))


def untiles_pm(y, d):
    T = y.shape[1] // d
    return np.ascontiguousarray(y.reshape(128, T, d).transpose(1, 0, 2).reshape(T * 128, d))


_TRIU = np.triu(np.ones((128, 128), np.float32))
_TRIL = np.tril(np.ones((128, 128), np.float32), -1).T.copy()
_TRIL = np.ascontiguousarray((np.arange(128)[:, None] > np.arange(128)[None, :]).astype(np.float32))


def run_m_even(q, kv, kpe, gqkv, la):
    nc = prog("m_even", build_m_even)
    maps = []
    kpeT = np.ascontiguousarray(kpe.T)
    for c in range(NCORES):
        hg, half = c // 2, c % 2
        qT = np.ascontiguousarray(q[:, c * 96:(c + 1) * 96].T)
        kT = np.ascontiguousarray(np.concatenate([kv[:, c * 128:c * 128 + 64].T, kpeT], axis=0))
        maps.append({
            "qT": qT, "kT": kT, "v": tiles_pm(kv[:, c * 128 + 64:(c + 1) * 128]),
            "trib": bf(_TRIU), "triu": _TRIU, "tril": _TRIL,
            "gqT": np.ascontiguousarray(gqkv[:, hg * 64:(hg + 1) * 64].T),
            "gkT": np.ascontiguousarray(gqkv[:, 256 + hg * 64:256 + (hg + 1) * 64].T),
            "gk": tiles_pm(gqkv[:, 256 + hg * 64:256 + (hg + 1) * 64]),
            "gv": tiles_pm(gqkv[:, 512 + hg * 128 + half * 64:512 + hg * 128 + (half + 1) * 64]),
            "la": tiles_pm(la[:, hg * 64:(hg + 1) * 64]),
        })
    r = run(nc, maps)
    a = np.concatenate([untiles_pm(r[c]["o"], 64) for c in range(NCORES)], axis=1)
    go = np.concatenate([untiles_pm(r[c]["go"], 64) for c in range(NCORES)], axis=1)
    return a, go


def shard(x):
    return [np.ascontiguousarray(x[c * TOK:(c + 1) * TOK]) for c in range(NCORES)]


def run_cmix_even(h, a, go, r, g_gla, w_out, gpost):
    nc = prog("cmix_even", build_cmix_even)
    com = {"idn": _IDN, "ggla": bc(g_gla), "gpo": bc(gpost), "w_out": wl(w_out)}
    hs, as_, gs, rs = shard(h), shard(a), shard(go), shard(r)
    maps = [dict(com, h=hs[c], a=as_[c], go=gs[c], r=rs[c]) for c in range(NCORES)]
    res = run(nc, maps)
    return np.concatenate([res[c]["ho"] for c in range(NCORES)], axis=0)


def run_a_odd(h, w_in, gpre, conv_w, conv_b, w_q, w_k, w_v, w_gates, b_gates):
    nc = prog("a_odd", build_a_odd)

    def hw(w):
        return np.ascontiguousarray(w.reshape(4, 2, 128, 256).transpose(2, 0, 1, 3).reshape(128, 8 * 256))
    com = {"idn": _IDN, "gpre": vl(gpre), "w_in": wl(w_in),
           "convw": np.ascontiguousarray(conv_w.reshape(4, 8, 128).transpose(2, 1, 0).reshape(128, 32)),
           "convb": vl(conv_b), "bg": np.ascontiguousarray(b_gates.reshape(8, 1)),
           "wq": hw(w_q), "wk": hw(w_k), "wv": hw(w_v), "wg": wl(w_gates)}
    maps = []
    for c in range(NCORES):
        hh = h[c * TOK - 128:c * TOK] if c > 0 else np.zeros((128, 1024), np.float32)
        maps.append(dict(com, h=np.ascontiguousarray(h[c * TOK:(c + 1) * TOK]), hh=np.ascontiguousarray(hh)))
    r = run(nc, maps)
    out = {k: np.concatenate([r[c][k] for c in range(NCORES)], axis=1) for k in ("xcT", "qT", "kT", "vT", "gi", "gl")}
    out["z"] = np.concatenate([r[c]["z"] for c in range(NCORES)], axis=0)
    return out


def run_m_odd(qT, kT, vT, gi, gl):
    nc = prog("m_odd", build_m_odd)
    maps = []
    for c in range(NCORES):
        hd, half = c // 2, c % 2
        q = qT[hd * 256:(hd + 1) * 256]
        k = kT[hd * 256:(hd + 1) * 256]
        v = vT[hd * 256 + half * 128:hd * 256 + (half + 1) * 128]
        maps.append({"idn": _IDN, "triu": _TRIU, "qT": np.ascontiguousarray(q), "kT": np.ascontiguousarray(k),
                     "ktok": tiles_pm(np.ascontiguousarray(k.T)), "v": tiles_pm(np.ascontiguousarray(v.T)),
                     "ii": np.ascontiguousarray(gi[hd].reshape(-1, 128).T),
                     "lf": np.ascontiguousarray(gl[4 + hd].reshape(-1, 128).T)})
    r = run(nc, maps)
    return np.concatenate([untiles_pm(r[c]["ho"], 128) for c in range(NCORES)], axis=1)


def run_cmix_odd(h, hc, xc, z, g_hnorm, skip, w_out, gpost):
    nc = prog("cmix_odd", build_cmix_odd)
    com = {"idn": _IDN, "ghn": bc(g_hnorm), "skip": bc(skip), "gpo": bc(gpost), "w_out": wl(w_out)}
    hs, cs, xs, zs = shard(h), shard(hc), shard(xc), shard(z)
    maps = [dict(com, h=hs[c], hc=cs[c], xc=xs[c], z=zs[c]) for c in range(NCORES)]
    res = run(nc, maps)
    return np.concatenate([res[c]["ho"] for c in range(NCORES)], axis=0)


def kernel(x, mem, positions,
           g_mix_pre, g_mix_post, g_xattn_pre, g_xattn_post, g_mem, g_ffn_pre, g_ffn_post,
           ev_w_in, ev_g_q, ev_w_uq, ev_g_kv, ev_w_ukv, ev_w_gate, ev_b_gate, ev_g_gla, ev_w_out,
           od_w_in, od_conv_w, od_conv_b, od_w_q, od_w_k, od_w_v, od_w_gates, od_b_gates,
           od_g_hnorm, od_skip, od_w_out,
           xa_w_q, xa_w_k, xa_w_v, xa_w_o,
           ffn_w1, ffn_w2):
    f = lambda a: np.asarray(a)
    h = np.ascontiguousarray(f(x)[0], dtype=np.float32)
    memv = np.ascontiguousarray(f(mem)[0], dtype=np.float32)
    pos = np.ascontiguousarray(f(positions)[0]).astype(np.int32)
    for layer in range(4):
        j = layer // 2
        if layer % 2 == 0:
            o = run_a_even(h, pos, f(ev_w_in)[j], f(g_mix_pre)[layer], f(ev_g_q)[j], f(ev_w_uq)[j], f(ev_g_kv)[j],
                           f(ev_w_ukv)[j], f(ev_w_gate)[j], f(ev_b_gate)[j])
            a, go = run_m_even(o["q"], o["kv"], o["kpe"], o["gqkv"], o["la"])
            h = run_cmix_even(h, a, go, o["r"], f(ev_g_gla)[j], f(ev_w_out)[j], f(g_mix_post)[layer])
        else:
            o = run_a_odd(h, f(od_w_in)[j], f(g_mix_pre)[layer], f(od_conv_w)[j], f(od_conv_b)[j], f(od_w_q)[j],
                          f(od_w_k)[j], f(od_w_v)[j], f(od_w_gates)[j], f(od_b_gates)[j])
            hc = run_m_odd(o["qT"], o["kT"], o["vT"], o["gi"], o["gl"])
            h = run_cmix_odd(h, hc, np.ascontiguousarray(o["xcT"].T), o["z"], f(od_g_hnorm)[j], f(od_skip)[j],
                             f(od_w_out)[j], f(g_mix_post)[layer])
        h = run_xa(h, memv, f(xa_w_q)[layer], f(xa_w_k)[layer], f(xa_w_v)[layer], f(xa_w_o)[layer],
                   f(g_xattn_pre)[layer], f(g_mem)[layer], f(g_xattn_post)[layer])
        h = run_ffn(h, f(ffn_w1)[layer], f(ffn_w2)[layer], f(g_ffn_pre)[layer], f(g_ffn_post)[layer])
    return h[None].astype(np.float32)
```

```python
import contextlib
import numpy as np
import concourse.bass as bass
import concourse.mybir as mybir
from concourse.bass_utils import run_bass_kernel_spmd

F32 = mybir.dt.float32
BF16 = mybir.dt.bfloat16
I32 = mybir.dt.int32
AF = mybir.ActivationFunctionType
ALU = mybir.AluOpType
AX = mybir.AxisListType

COMPUTE = ('pe', 'act', 'dve', 'pool')
NDSEM = 8


class _Op:
    __slots__ = ('eng', 'fn', 'dma', 'deps', 'has_dep', 'cval', 'dq', 'dslot', 'dval', 'idx')


class Prog:
    def __init__(self, nc, self_sync=True):
        self.nc = nc
        self.ops = []
        self.last_w = {}
        self.readers = {}
        self.stack = contextlib.ExitStack()
        self.self_sync = self_sync
        self.ndma = {}
        self.out_dmas = []
        self._n = 0
        self.psum_names = set()

    def sb(self, shape, dt, name=None):
        self._n += 1
        name = name or f"sb{self._n}"
        return self.stack.enter_context(self.nc.sbuf_tensor(name, list(shape), dt))

    def ps(self, shape, dt=F32, name=None):
        self._n += 1
        name = name or f"ps{self._n}"
        self.psum_names.add(name)
        return self.stack.enter_context(self.nc.psum_tensor(name, list(shape), dt))

    @staticmethod
    def _keys(xs):
        out = []
        for x in xs:
            if x is None or isinstance(x, (int, float)):
                continue
            if isinstance(x, (str, tuple)):
                out.append(x)
            else:
                out.append(x.name)
        return out

    def op(self, eng, fn, reads=(), writes=(), dma=False):
        o = _Op()
        o.eng = eng
        o.fn = fn
        o.dma = dma
        o.has_dep = False
        o.idx = len(self.ops)
        deps = set()
        pn = self.psum_names
        rk = [k[0] if (isinstance(k, tuple) and k[0] in pn) else k for k in self._keys(reads)]
        wk = [k[0] if (isinstance(k, tuple) and k[0] in pn) else k for k in self._keys(writes)]
        for k in rk:
            if k in self.last_w:
                deps.add(self.last_w[k])
            if k in pn:
                r = self.readers.get(k)
                if r:
                    for e2, v in r.items():
                        if e2 != eng and not isinstance(v, list):
                            deps.add(v)
        for k in wk:
            if k in self.last_w:
                deps.add(self.last_w[k])
            r = self.readers.get(k)
            if r:
                for v in r.values():
                    if isinstance(v, list):
                        deps.update(v)
                    else:
                        deps.add(v)
        deps.discard(o.idx)
        o.deps = deps
        if dma:
            j = self.ndma.get(eng, 0)
            self.ndma[eng] = j + 1
            o.dq = eng
            o.dslot = j % NDSEM
            o.dval = 16 * (j // NDSEM + 1)
        self.ops.append(o)
        for k in wk:
            self.last_w[k] = o.idx
            self.readers[k] = {}
        for k in rk:
            if k in wk:
                continue
            r = self.readers.setdefault(k, {})
            if dma:
                r.setdefault('dma', []).append(o.idx)
            else:
                r[eng] = o.idx
        return o

    def dma(self, q, out, in_, R=None, W=None, is_out=False, **kw):
        o = self.op(q, lambda e: e.dma_start(out=out, in_=in_, **kw),
                    reads=[in_] if R is None else R, writes=[out] if W is None else W, dma=True)
        if is_out:
            self.out_dmas.append(o.idx)
        return o

    def mm(self, out, lhsT, rhs, start=True, stop=True, R=None, W=None, **kw):
        return self.op('pe', lambda e: e.matmul(out, lhsT, rhs, start=start, stop=stop, **kw),
                       reads=[lhsT, rhs] if R is None else R, writes=[out] if W is None else W)

    def tr(self, out, in_, ident, R=None, W=None):
        return self.op('pe', lambda e: e.transpose(out, in_, ident),
                       reads=[in_, ident] if R is None else R, writes=[out] if W is None else W)

    def act(self, out, in_, func, bias=None, scale=None, accum_out=None, R=None, W=None, eng='act'):
        kw = {}
        if bias is not None:
            kw['bias'] = bias
        if scale is not None:
            kw['scale'] = scale
        if accum_out is not None:
            kw['accum_out'] = accum_out
        return self.op(eng, lambda e: e.activation(out, in_, func, **kw),
                       reads=[in_, bias, scale] if R is None else R, writes=[out, accum_out] if W is None else W)

    def v(self, eng, name, *args, reads=(), writes=(), **kw):
        return self.op(eng, lambda e: getattr(e, name)(*args, **kw), reads=reads, writes=writes)

    def emit(self):
        nc = self.nc
        ops = self.ops
        for o in ops:
            for d in o.deps:
                ops[d].has_dep = True
        for i in self.out_dmas:
            ops[i].has_dep = True
        st = self.stack
        csem = {e: st.enter_context(nc.semaphore(f"c_{e}")) for e in COMPUTE}
        dsem = {}
        for q in self.ndma:
            dsem[q] = [st.enter_context(nc.semaphore(f"d_{q}_{i}")) for i in range(NDSEM)]
        cnt = {e: 0 for e in COMPUTE}
        for o in ops:
            if not o.dma and o.has_dep:
                cnt[o.eng] += 1
                o.cval = cnt[o.eng]
            elif not o.dma:
                o.cval = None
        per_eng = {}
        for o in ops:
            per_eng.setdefault(o.eng, []).append(o)
        waited = {}
        dma_hist = {q: [] for q in self.ndma}

        def emit_engine(ename, e):
            wl = waited.setdefault(ename, {})
            for o in per_eng.get(ename, []):
                need = {}
                for d in o.deps:
                    p = ops[d]
                    if p.dma:
                        sem = dsem[p.dq][p.dslot]
                        val = p.dval
                    else:
                        if p.eng == ename and (ename == 'pe' or not self.self_sync):
                            continue
                        sem = csem[p.eng]
                        val = p.cval
                    if need.get(id(sem), (None, 0))[1] < val:
                        need[id(sem)] = (sem, val)
                if o.dma and o.dval > 16:
                    sem = dsem[o.dq][o.dslot]
                    k = id(sem)
                    if need.get(k, (None, 0))[1] < o.dval - 16:
                        need[k] = (sem, o.dval - 16)
                for k, (sem, val) in need.items():
                    if wl.get(k, 0) >= val:
                        continue
                    wl[k] = val
                    e.wait_ge(sem, val)
                ins = o.fn(e)
                if o.dma:
                    ins.then_inc(dsem[o.dq][o.dslot], 16)
                elif o.has_dep:
                    ins.then_inc(csem[o.eng], 1)
            if ename == 'sp':
                for i in self.out_dmas:
                    p = ops[i]
                    e.wait_ge(dsem[p.dq][p.dslot], p.dval)

        with nc.Block() as block:
            @block.sync
            def _(e):
                emit_engine('sp', e)

            @block.tensor
            def _(e):
                emit_engine('pe', e)

            @block.scalar
            def _(e):
                emit_engine('act', e)

            @block.vector
            def _(e):
                emit_engine('dve', e)

            @block.gpsimd
            def _(e):
                emit_engine('pool', e)
        self.stack.close()


EPS = 1e-6
NCORES = 8
TOK = 2048
NT = TOK // 128


class Ctx:
    def __init__(self):
        self.nc = bass.Bass("TRN2", target_bir_lowering=False)
        self.P = Prog(self.nc)
        self.rr = 0

    def din(self, name, shape, dt=F32):
        return self.nc.dram_tensor(name, list(shape), dt, kind="ExternalInput").ap()

    def dout(self, name, shape, dt=F32):
        return self.nc.dram_tensor(name, list(shape), dt, kind="ExternalOutput").ap()

    def ident(self):
        P = self.P
        idn = self.din("idn", [128, 128])
        self.idf = P.sb([128, 128], F32, "idf")
        self.idb = P.sb([128, 128], BF16, "idb")
        P.dma('sp', self.idf[:], idn)
        P.v('dve', 'tensor_copy', self.idb[:], self.idf[:], reads=[self.idf], writes=[self.idb])

    def small(self, name, shape, dt=F32, q='sp'):
        d = self.din(name, shape, dt)
        t = self.P.sb(shape, dt, "s_" + name)
        self.P.dma(q, t[:], d)
        return t


class WMat:
    def __init__(self, C, name, KC, N, rows=128):
        self.t = [C.P.sb([rows, N], BF16, f"{name}_{c}") for c in range(KC)]
        self.KC, self.N, self.rows = KC, N, rows

    def sl(self, c, lo, hi):
        return self.t[c][:, lo:hi]


def load_w(C, name, KC, N, g=None, rows=128, chunk=2048, stages=None, q=('sp', 'pool')):
    P = C.P
    w = C.din(name, [rows, KC * N])
    W = WMat(C, name, KC, N, rows)
    if stages is None:
        if not hasattr(C, '_stages'):
            C._stages = [P.sb([128, chunk], F32, f"wstage{i}") for i in range(3)]
        stages = C._stages
    engs = ('act', 'dve', 'pool')
    i = C.__dict__.get('_wi', 0)
    for c in range(KC):
        eng = engs[c % 3]
        for n0 in range(0, N, chunk):
            n = min(chunk, N - n0)
            st = stages[i % len(stages)]
            P.dma(q[i % len(q)], st[:rows, :n], w[:, c * N + n0:c * N + n0 + n])
            dst = W.t[c][:, n0:n0 + n]
            src = st[:rows, :n]
            if g is None:
                if eng == 'act':
                    P.act(dst, src, AF.Copy)
                else:
                    P.v(eng, 'tensor_copy', dst, src, reads=[st], writes=[W.t[c]])
            else:
                gs = g[:rows, c:c + 1]
                if eng == 'act':
                    P.act(dst, src, AF.Copy, scale=gs)
                else:
                    P.v(eng, 'tensor_scalar_mul', dst, src, gs, reads=[st, g], writes=[W.t[c]])
            i += 1
    C._wi = i
    return W


class NormScr:
    def __init__(self, C, D, tag, pt=None):
        P = C.P
        self.junk = P.sb([128, D], F32, f"junk_{tag}")
        self.ss = P.sb([128, 1], F32, f"ss_{tag}")
        self.rstd = P.sb([128, 1], F32, f"rstd_{tag}")
        self.xb = P.sb([128, D], BF16, f"xb_{tag}")
        self.pt = pt if pt is not None else P.ps([128, D // 128, 128], BF16, f"pt_{tag}")
        self.D = D


def rstd_of(C, src, D, S, ss=None, rstd=None, junk=None):
    P = C.P
    ss = S.ss if ss is None else ss
    rstd = S.rstd if rstd is None else rstd
    junk = S.junk if junk is None else junk
    P.act(junk, src, AF.Square, scale=float(D) ** -0.5, accum_out=ss)
    P.act(rstd, ss, AF.Sqrt, bias=EPS)
    P.v('dve', 'reciprocal', rstd, rstd, reads=[rstd], writes=[rstd])


def norm_T(C, src, D, S, dst, copy_eng='dve'):
    P = C.P
    rstd_of(C, src, D, S, S.ss[:], S.rstd[:], S.junk[:, :D])
    P.act(S.xb[:, :D], src, AF.Copy, scale=S.rstd[:])
    for c in range(D // 128):
        P.tr(S.pt[:, c, :], S.xb[:, c * 128:(c + 1) * 128], C.idb[:])
    if copy_eng == 'act':
        P.act(dst, S.pt[:, :D // 128, :], AF.Copy)
    else:
        P.v('dve', 'tensor_copy', dst, S.pt[:, :D // 128, :], reads=[S.pt], writes=[dst])


def postnorm_res(C, py, ht, gpo, S, tmp):
    P = C.P
    rstd_of(C, py, 1024, S, S.ss[:], S.rstd[:], S.junk[:, :1024])
    P.v('dve', 'scalar_tensor_tensor', tmp[:], py, S.rstd[:], gpo[:], ALU.mult, ALU.mult,
        reads=[py, S.rstd, gpo], writes=[tmp])
    P.v('pool', 'tensor_add', ht, ht, tmp[:], reads=[ht, tmp], writes=[ht])


def build_ffn():
    C = Ctx()
    P = C.P
    h = C.din("h", [TOK, 1024])
    ho = C.dout("ho", [TOK, 1024])
    C.ident()
    gpre = C.small("gpre", [128, 8])
    gpo = C.small("gpo", [128, 1024], q='pool')
    C._stages = [P.sb([128, 1024], F32, f"wstage{i}") for i in range(2)]
    W1 = load_w(C, "w1", 8, 4096, g=gpre, chunk=1024)
    W2 = load_w(C, "w2", 32, 1024, chunk=1024)
    S = NormScr(C, 1024, "a")
    hr = [P.sb([128, 1024], F32, f"hr{j}") for j in range(3)]
    xT = P.sb([128, 8, 512], BF16, "xT")
    h1T = [P.sb([128, 512], BF16, f"h1T{f}") for f in range(32)]
    rl = [P.sb([128, 512], BF16, f"rl{i}") for i in range(3)]
    p1 = [P.ps([128, 512], F32, f"p1_{i}") for i in range(3)]
    pys = [P.ps([128, 1024], F32, f"py{i}") for i in range(2)]
    n1 = 0
    nh = 0
    for g in range(TOK // 512):
        for j in range(4):
            r0 = g * 512 + j * 128
            ht = hr[nh % 3]
            nh += 1
            P.dma('sp', ht[:], h[r0:r0 + 128, :])
            norm_T(C, ht[:], 1024, S, xT[:, :, j * 128:(j + 1) * 128])
        for f in range(32):
            ps = p1[n1 % 3]
            r = rl[n1 % 3]
            n1 += 1
            for k in range(8):
                P.mm(ps[:], W1.sl(k, f * 128, (f + 1) * 128), xT[:, k, :], start=(k == 0), stop=(k == 7))
            P.act(r[:], ps[:], AF.Relu)
            P.v('pool', 'tensor_mul', h1T[f][:], r[:], r[:], reads=[r], writes=[h1T[f]])
        for j in range(4):
            r0 = g * 512 + j * 128
            ht = hr[nh % 3]
            nh += 1
            P.dma('pool', ht[:], h[r0:r0 + 128, :])
            py = pys[j % 2]
            for n in range(2):
                for k in range(32):
                    P.mm(py[:, n * 512:(n + 1) * 512], h1T[k][:, j * 128:(j + 1) * 128],
                         W2.sl(k, n * 512, (n + 1) * 512), start=(k == 0), stop=(k == 31))
            postnorm_res(C, py[:], ht[:], gpo, S, S.junk)
            P.dma('sp', ho[r0:r0 + 128, :], ht[:], W=[("ho", r0)], is_out=True)
    P.emit()
    return C.nc


def build_xa():
    C = Ctx()
    P = C.P
    h = C.din("h", [TOK, 1024])
    mem = C.din("mem", [256, 1024])
    ho = C.dout("ho", [TOK, 1024])
    C.ident()
    gpre = C.small("gpre", [128, 8])
    gmem = C.small("gmem", [128, 8])
    gpo = C.small("gpo", [128, 1024], q='pool')
    C._stages = [P.sb([128, 1024], F32, f"wstage{i}") for i in range(3)]
    Wq = load_w(C, "wq", 8, 1024, g=gpre, chunk=1024)
    Wk = load_w(C, "wk", 8, 1024, g=gmem, chunk=1024)
    Wv = load_w(C, "wv", 8, 1024, g=gmem, chunk=1024)
    Wo = load_w(C, "wo", 8, 1024, chunk=1024)
    S = NormScr(C, 1024, "a")
    hr = [P.sb([128, 1024], F32, f"hr{j}") for j in range(3)]
    pa = [P.ps([128, 512], F32, f"pa{i}") for i in range(2)]
    po = [P.ps([128, 512], F32, f"po{i}") for i in range(2)]
    py = P.ps([128, 1024], F32, "py")
    memT = P.sb([128, 8, 256], BF16, "memT")
    for m in range(2):
        mt = hr[m]
        P.dma('sp', mt[:], mem[m * 128:(m + 1) * 128, :])
        norm_T(C, mt[:], 1024, S, memT[:, :, m * 128:(m + 1) * 128])
    kT = [P.sb([128, 256], BF16, f"kT{oc}") for oc in range(8)]
    for oc in range(8):
        ps = pa[oc % 2]
        for k in range(8):
            P.mm(ps[:, :256], Wk.sl(k, oc * 128, (oc + 1) * 128), memT[:, k, :], start=(k == 0), stop=(k == 7))
        P.v('dve', 'tensor_copy', kT[oc][:], ps[:, :256], reads=[ps], writes=[kT[oc]])
    V1 = [P.sb([128, 4, 257], BF16, f"V1_{m}") for m in range(2)]
    for m in range(2):
        P.v('pool', 'memset', V1[m][:], 1.0, writes=[V1[m]])
        for n in range(2):
            ps = pa[n % 2]
            for k in range(8):
                P.mm(ps[:], memT[:, k, m * 128:(m + 1) * 128], Wv.sl(k, n * 512, (n + 1) * 512),
                     start=(k == 0), stop=(k == 7))
            P.v('dve', 'tensor_copy', V1[m][:, 2 * n:2 * n + 2, 0:256],
                ps[:].rearrange("p (h d) -> p h d", h=2), reads=[ps], writes=[V1[m]])
    xT = P.sb([128, 8, 512], BF16, "xT")
    qT = [P.sb([128, 512], BF16, f"qT{oc}") for oc in range(8)]
    PT = [[P.sb([128, 512], BF16, f"PT{hh}_{m}") for m in range(2)] for hh in range(4)]
    oa = P.sb([128, 1024], BF16, "oa")
    oaT = P.sb([128, 8, 128], BF16, "oaT")
    rinv = P.sb([128, 4], F32, "rinv")
    nh = 0
    na = 0
    no = 0
    for g in range(TOK // 512):
        for j in range(4):
            r0 = g * 512 + j * 128
            ht = hr[nh % 3]
            nh += 1
            P.dma('sp', ht[:], h[r0:r0 + 128, :])
            norm_T(C, ht[:], 1024, S, xT[:, :, j * 128:(j + 1) * 128])
        for oc in range(8):
            ps = pa[na % 2]
            na += 1
            for k in range(8):
                P.mm(ps[:], Wq.sl(k, oc * 128, (oc + 1) * 128), xT[:, k, :], start=(k == 0), stop=(k == 7))
            if oc % 2 == 0:
                P.act(qT[oc][:], ps[:], AF.Copy)
            else:
                P.v('dve', 'tensor_copy', qT[oc][:], ps[:], reads=[ps], writes=[qT[oc]])
        for hh in range(4):
            for m in range(2):
                ps = pa[na % 2]
                na += 1
                for dc in range(2):
                    P.mm(ps[:], kT[2 * hh + dc][:, m * 128:(m + 1) * 128], qT[2 * hh + dc][:],
                         start=(dc == 0), stop=(dc == 1))
                P.act(PT[hh][m][:], ps[:], AF.Exp, scale=1.0 / 16.0)
        for j in range(4):
            r0 = g * 512 + j * 128
            ht = hr[nh % 3]
            nh += 1
            P.dma('pool', ht[:], h[r0:r0 + 128, :])
            for hh in range(4):
                ps = po[no % 2]
                no += 1
                for m in range(2):
                    P.mm(ps[:, :257], PT[hh][m][:, j * 128:(j + 1) * 128], V1[m][:, hh, :],
                         start=(m == 0), stop=(m == 1))
                P.v('dve', 'reciprocal', rinv[:, hh:hh + 1], ps[:, 256:257], reads=[ps], writes=[rinv])
                P.act(oa[:, hh * 256:(hh + 1) * 256], ps[:, 0:256], AF.Copy, scale=rinv[:, hh:hh + 1])
            for c in range(8):
                P.tr(S.pt[:, c, :], oa[:, c * 128:(c + 1) * 128], C.idb[:])
            P.v('dve', 'tensor_copy', oaT[:], S.pt[:], reads=[S.pt], writes=[oaT])
            for n in range(2):
                for k in range(8):
                    P.mm(py[:, n * 512:(n + 1) * 512], oaT[:, k, :], Wo.sl(k, n * 512, (n + 1) * 512),
                         start=(k == 0), stop=(k == 7))
            postnorm_res(C, py[:], ht[:], gpo, S, S.junk)
            P.dma('sp', ho[r0:r0 + 128, :], ht[:], W=[("ho", r0)], is_out=True)
    P.emit()
    return C.nc


TWO_PI = 6.283185307179586
SKIP = set()


def rope_tables(C, pos_i, invf):
    P = C.P
    posf = P.sb([128, NT], F32, "posf")
    P.v('dve', 'tensor_copy', posf[:], pos_i[:], reads=[pos_i], writes=[posf])
    ang = P.sb([128, NT, 128], F32, "ang")
    for j in range(NT):
        P.v('dve', 'tensor_scalar_mul', ang[:, j, :], invf[:], posf[:, j:j + 1], reads=[invf, posf], writes=[ang])
    tabs = []
    for nm, off in (("sin", 0.0), ("cos", 0.25)):
        t = P.sb([128, NT, 128], F32, "t_" + nm)
        ti = P.sb([128, NT, 128], I32, "ti_" + nm)
        P.v('dve', 'tensor_scalar', t[:], ang[:], 1.0 / TWO_PI, off, ALU.mult, ALU.add, reads=[ang], writes=[t])
        P.v('dve', 'tensor_copy', ti[:], t[:], reads=[t], writes=[ti])
        tf = P.sb([128, NT, 128], F32, "tf_" + nm)
        P.v('dve', 'tensor_copy', tf[:], ti[:], reads=[ti], writes=[tf])
        P.v('dve', 'tensor_sub', t[:], t[:], tf[:], reads=[t, tf], writes=[t])
        P.act(tf[:], t[:], AF.Sin, scale=6.28318)
        tabs.append(tf)
    return tabs[0], tabs[1]


def rope_apply(C, x1, x2, cs, sn, o1, o2, tmps, shape):
    P = C.P
    t1, t2, t3, t4 = tmps
    P.v('dve', 'tensor_mul', t1, x1, cs, reads=[x1, cs], writes=[t1])
    P.v('dve', 'tensor_mul', t2, x2, sn, reads=[x2, sn], writes=[t2])
    P.v('dve', 'tensor_mul', t3, x2, cs, reads=[x2, cs], writes=[t3])
    P.v('dve', 'tensor_mul', t4, x1, sn, reads=[x1, sn], writes=[t4])
    P.v('pool', 'tensor_sub', o1, t1, t2, reads=[t1, t2], writes=[o1])
    P.v('pool', 'tensor_add', o2, t3, t4, reads=[t3, t4], writes=[o2])


def build_a_even():
    C = Ctx()
    P = C.P
    h = C.din("h", [TOK, 1024])
    q_o = C.dout("q", [TOK, 768], BF16)
    kv_o = C.dout("kv", [TOK, 1024], BF16)
    kpe_o = C.dout("kpe", [TOK, 32], BF16)
    g_o = C.dout("gqkv", [TOK, 1024], BF16)
    la_o = C.dout("la", [TOK, 256])
    r_o = C.dout("r", [TOK, 512])
    C.ident()
    gpre = C.small("gpre", [128, 8])
    gq = C.small("gq", [128, 2])
    gkv = C.small("gkv", [128, 1])
    wga = C.small("wga", [17, 256])
    pos_i = C.small("pos", [128, NT], I32)
    invf = C.small("invf", [128, 128])
    C._stages = [P.sb([128, 1968], F32, f"wstage{i}") for i in range(2)]
    Win = load_w(C, "w_in", 8, 1968, g=gpre, chunk=1968)
    Wuq = load_w(C, "w_uq", 2, 768, g=gq, chunk=768)
    Wukv = load_w(C, "w_ukv", 1, 1024, g=gkv, chunk=1024)
    sin_t, cos_t = rope_tables(C, pos_i, invf)
    S = NormScr(C, 1024, "a")
    hr = [P.sb([128, 1024], F32, f"hr{j}") for j in range(2)]
    xTt = [P.sb([128, 8, 128], BF16, f"xTt{j}") for j in range(2)]
    pp = [P.ps([128, 512], F32, f"pp{i}") for i in range(4)]
    pg = P.ps([128, 512], F32, "pg")
    pjs = [P.sb([128, 1968], F32, f"pj{i}") for i in range(2)]
    cqT = P.sb([128, 2, 128], BF16, "cqT")
    ckvT = P.sb([128, 128], BF16, "ckvT")
    aug = P.sb([32, 128], F32, "aug")
    P.v('dve', 'memset', aug[:], 1.0, writes=[aug])
    qos = [P.sb([128, 8, 96], BF16, f"qo{i}") for i in range(2)]
    kvos = [P.sb([128, 1024], BF16, f"kvo{i}") for i in range(2)]
    kpos = [P.sb([128, 32], BF16, f"kpo{i}") for i in range(2)]
    gbs = [P.sb([128, 1024], BF16, f"gb{i}") for i in range(2)]
    las = [P.sb([128, 256], F32, f"la{i}") for i in range(2)]
    ex = P.sb([128, 256], F32, "ex")
    tq = [P.sb([128, 4, 16], F32, f"tq{i}") for i in range(4)]
    tk = [P.sb([128, 16], F32, f"tk{i}") for i in range(4)]
    for j in range(NT):
        r0 = j * 128
        ht = hr[j % 2]
        xT = xTt[j % 2]
        pj = pjs[j % 2]
        P.dma('sp', ht[:], h[r0:r0 + 128, :])
        norm_T(C, ht[:], 1024, S, xT[:])
        for n in range(4):
            w = min(512, 1968 - n * 512)
            for k in range(8):
                P.mm(pp[n][:, :w], xT[:, k, :], Win.sl(k, n * 512, n * 512 + w), start=(k == 0), stop=(k == 7))
        for n in range(4):
            w = min(512, 1968 - n * 512)
            if n % 2 == 0:
                P.act(pj[:, n * 512:n * 512 + w], pp[n][:, :w], AF.Copy)
            else:
                P.v('dve', 'tensor_copy', pj[:, n * 512:n * 512 + w], pp[n][:, :w], reads=[pp[n]], writes=[pj])
        P.dma('pool', r_o[r0:r0 + 128, :], pj[:, 1456:1968], W=[("r_o", j)], is_out=True)
        gb = gbs[j % 2]
        P.v('pool', 'tensor_copy', gb[:], pj[:, 416:1440], reads=[pj], writes=[gb])
        P.dma('pool', g_o[r0:r0 + 128, :], gb[:], W=[("g_o", j)], is_out=True)
        rstd_of(C, pj[:, 0:256], 256, S, S.ss[:], S.rstd[:], S.junk[:, :256])
        P.act(S.xb[:, :256], pj[:, 0:256], AF.Copy, scale=S.rstd[:])
        for c in range(2):
            P.tr(S.pt[:, c, :], S.xb[:, c * 128:(c + 1) * 128], C.idb[:])
        P.v('dve', 'tensor_copy', cqT[:], S.pt[:, 0:2, :], reads=[S.pt], writes=[cqT])
        for hf in range(2):
            for kc in range(2):
                P.mm(pp[hf][:, :384], cqT[:, kc, :], Wuq.sl(kc, hf * 384, (hf + 1) * 384),
                     start=(kc == 0), stop=(kc == 1))
        qo = qos[j % 2]
        cs4 = cos_t[:, j, 0:64].rearrange("p (h f) -> p h f", h=4)
        sn4 = sin_t[:, j, 0:64].rearrange("p (h f) -> p h f", h=4)
        for hf in range(2):
            pq = pp[hf][:, :384].rearrange("p (h d) -> p h d", h=4)
            P.act(qo[:, 4 * hf:4 * hf + 4, 0:64], pq[:, :, 0:64], AF.Copy)
            rope_apply(C, pq[:, :, 64:80], pq[:, :, 80:96], cs4, sn4,
                       qo[:, 4 * hf:4 * hf + 4, 64:80], qo[:, 4 * hf:4 * hf + 4, 80:96],
                       [t[:] for t in tq], None)
        P.dma('sp', q_o[r0:r0 + 128, :], qo[:].rearrange("p h d -> p (h d)"), W=[("q_o", j)], is_out=True)
        rstd_of(C, pj[:, 256:384], 128, S, S.ss[:], S.rstd[:], S.junk[:, :128])
        P.act(S.xb[:, :128], pj[:, 256:384], AF.Copy, scale=S.rstd[:])
        P.tr(S.pt[:, 0, :], S.xb[:, 0:128], C.idb[:])
        P.v('dve', 'tensor_copy', ckvT[:], S.pt[:, 0, :], reads=[S.pt], writes=[ckvT])
        for n in range(2):
            P.mm(pp[2 + n][:], ckvT[:], Wukv.sl(0, n * 512, (n + 1) * 512), start=True, stop=True)
        kvo = kvos[j % 2]
        P.act(kvo[:, 0:512], pp[2][:], AF.Copy)
        P.v('dve', 'tensor_copy', kvo[:, 512:1024], pp[3][:], reads=[pp[3]], writes=[kvo])
        P.dma('sp', kv_o[r0:r0 + 128, :], kvo[:], W=[("kv_o", j)], is_out=True)
        kpo = kpos[j % 2]
        rope_apply(C, pj[:, 384:400], pj[:, 400:416], cos_t[:, j, 0:16], sin_t[:, j, 0:16],
                   kpo[:, 0:16], kpo[:, 16:32], [t[:] for t in tk], None)
        P.dma('sp', kpe_o[r0:r0 + 128, :], kpo[:], W=[("kpe_o", j)], is_out=True)
        if 'gate' in SKIP:
            continue
        P.tr(pg[0:16, 256:384], pj[:, 1440:1456], C.idf[:], W=[("pg", "t")])
        P.v('dve', 'tensor_copy', aug[0:16, :], pg[0:16, 256:384], reads=[("pg", "t")], writes=[aug])
        P.mm(pg[:, 0:256], aug[0:17, :], wga[0:17, :], start=True, stop=True, W=[("pg", "m")])
        P.act(ex[:], pg[:, 0:256], AF.Exp, scale=-1.0, R=[("pg", "m")])
        P.act(ex[:], ex[:], AF.Ln, bias=1.0)
        la = las[j % 2]
        P.v('dve', 'tensor_scalar_mul', la[:], ex[:], -1.0 / 16.0, reads=[ex], writes=[la])
        P.dma('sp', la_o[r0:r0 + 128, :], la[:], W=[("la_o", j)], is_out=True)
    P.emit()
    return C.nc


SEQ = 16384
NCH = SEQ // 128


def build_m_even():
    C = Ctx()
    P = C.P
    qT_d = C.din("qT", [96, SEQ], BF16)
    kT_d = C.din("kT", [96, SEQ], BF16)
    v_d = C.din("v", [128, NCH * 64], BF16)
    tri_d = C.din("trib", [128, 128], BF16)
    o_d = C.dout("o", [128, NCH * 64])
    gqT_d = C.din("gqT", [64, SEQ], BF16)
    gkT_d = C.din("gkT", [64, SEQ], BF16)
    gk_d = C.din("gk", [128, NCH * 64], BF16)
    gv_d = C.din("gv", [128, NCH * 64], BF16)
    la_d = C.din("la", [128, NCH * 64])
    go_d = C.dout("go", [128, NCH * 64])
    triu = C.small("triu", [128, 128])
    tril = C.small("tril", [128, 128])
    QT = P.sb([96, SEQ], BF16, "QT")
    KT = P.sb([96, SEQ], BF16, "KT")
    V1 = P.sb([128, NCH, 65], BF16, "V1")
    trib = P.sb([128, 128], BF16, "trib_sb")
    P.dma('sp', trib[:], tri_d)
    nq = max(1, SEQ // 4096)
    cq = SEQ // nq
    for i in range(nq):
        P.dma('sp', QT[:, i * cq:(i + 1) * cq], qT_d[:, i * cq:(i + 1) * cq])
        P.dma('pool', KT[:, i * cq:(i + 1) * cq], kT_d[:, i * cq:(i + 1) * cq])
    P.v('dve', 'memset', V1[:], 1.0, writes=[V1])
    P.dma('sp', V1[:, :, 0:64], v_d.rearrange("p (t d) -> p t d", d=64))
    pS = [P.ps([128, 512], F32, f"pS{i}") for i in range(3)]
    pO = [P.ps([128, 512], F32, f"pO{i}") for i in range(4)]
    PTs = [P.sb([128, 512], BF16, f"PT{i}") for i in range(3)]
    obs = [P.sb([128, 4, 64], F32, f"ob{i}") for i in range(2)]
    rinv = P.sb([128, 4], F32, "rinv")
    scale = 96.0 ** -0.5
    NR = 3
    blocks = [(qb, kt) for qb in range(0 if 'mla' not in SKIP else SEQ, SEQ // 512) for kt in range(4 * qb + 4)]

    def stage_s(i):
        qb, kt = blocks[i]
        ps, pt = pS[i % NR], PTs[i % NR]
        c0 = max(0, kt - 4 * qb) * 128
        P.mm(ps[:, c0:512], KT[:, kt * 128:(kt + 1) * 128], QT[:, qb * 512 + c0:(qb + 1) * 512],
             start=True, stop=True)
        P.act(pt[:, c0:512], ps[:, c0:512], AF.Exp, scale=scale)
        if kt >= 4 * qb:
            P.v('pool', 'tensor_mul', pt[:, c0:c0 + 128], pt[:, c0:c0 + 128], trib[:],
                reads=[pt, trib], writes=[pt])

    def stage_pv(i):
        qb, kt = blocks[i]
        pt = PTs[i % NR]
        jd = max(0, kt - 4 * qb)
        for j in range(jd, 4):
            P.mm(pO[j][:, 0:65], pt[:, j * 128:(j + 1) * 128], V1[:, kt, :],
                 start=(kt == 0), stop=(kt == 4 * qb + j))
        if kt == 4 * qb + 3:
            ob = obs[qb % 2]
            for j in range(4):
                P.v('dve', 'reciprocal', rinv[:, j:j + 1], pO[j][:, 64:65], reads=[pO[j]], writes=[rinv])
                P.act(ob[:, j, :], pO[j][:, 0:64], AF.Copy, scale=rinv[:, j:j + 1])
            P.dma('sp', o_d[:, qb * 256:(qb + 1) * 256], ob[:].rearrange("p j d -> p (j d)"),
                  W=[("o_d", qb)], is_out=True)

    GC = 8
    S = P.sb([64, 64], F32, "gS")
    Sb = P.sb([64, 64], BF16, "gSb")
    P.v('dve', 'memset', S[:], 0.0, writes=[S])
    P.v('dve', 'memset', Sb[:], 0.0, writes=[Sb])
    bufs = []
    for i in range(2):
        bufs.append(dict(q=P.sb([64, GC * 128], BF16, f"gq{i}"), k=P.sb([64, GC * 128], BF16, f"gkT{i}"),
                         kt=P.sb([128, GC * 64], BF16, f"gk{i}"), v=P.sb([128, GC * 64], BF16, f"gv{i}"),
                         la=P.sb([128, GC * 64], F32, f"gla{i}"), o=P.sb([128, GC * 64], F32, f"go{i}")))
    ND = 3
    eb = [P.sb([64, 128], F32, f"eb{i}") for i in range(ND)]
    enb = [P.sb([64, 128], F32, f"enb{i}") for i in range(ND)]
    ebr = [P.sb([128, 64], F32, f"ebr{i}") for i in range(ND)]
    qs = [P.sb([64, 128], BF16, f"qs{i}") for i in range(ND)]
    ks = [P.sb([64, 128], BF16, f"ks{i}") for i in range(ND)]
    kh = [P.sb([128, 64], BF16, f"kh{i}") for i in range(ND)]
    AT = [P.sb([128, 128], BF16, f"AT{i}") for i in range(ND)]
    pG = P.ps([128, 512], F32, "pG")
    rA, rB, rC, rD, rE = pG[0:64, 0:128], pG[:, 128:192], pG[:, 192:320], pG[:, 320:384], pG[0:64, 384:448]
    gstages = []

    def gla_chunk(g, c):
        B = bufs[g % 2]
        i = (g * GC + c) % ND
        la_c = B['la'][:, c * 64:(c + 1) * 64]
        v_c = B['v'][:, c * 64:(c + 1) * 64]

        def s0():
            if c == 0:
                P.dma('sp', B['q'][:], gqT_d[:, g * GC * 128:(g + 1) * GC * 128])
                P.dma('sp', B['k'][:], gkT_d[:, g * GC * 128:(g + 1) * GC * 128])
                P.dma('pool', B['kt'][:], gk_d[:, g * GC * 64:(g + 1) * GC * 64])
                P.dma('pool', B['v'][:], gv_d[:, g * GC * 64:(g + 1) * GC * 64])
                P.dma('sp', B['la'][:], la_d[:, g * GC * 64:(g + 1) * GC * 64])

        def s1():
            P.mm(rA, la_c, triu[:], start=True, stop=True)
            P.mm(rB, tril[:], la_c, start=True, stop=True)

        def s2():
            P.act(eb[i][:], rA, AF.Exp)
            P.act(enb[i][:], rA, AF.Exp, scale=-1.0)
            P.act(ebr[i][:], rB, AF.Exp)

        def s3():
            P.v('dve', 'scalar_tensor_tensor', qs[i][:], B['q'][:, c * 128:(c + 1) * 128], 0.125, eb[i][:],
                ALU.mult, ALU.mult, reads=[B['q'], eb[i]], writes=[qs[i]])
            P.v('pool', 'tensor_mul', ks[i][:], B['k'][:, c * 128:(c + 1) * 128], enb[i][:],
                reads=[B['k'], enb[i]], writes=[ks[i]])
            P.v('pool', 'tensor_mul', kh[i][:], B['kt'][:, c * 64:(c + 1) * 64], ebr[i][:],
                reads=[B['kt'], ebr[i]], writes=[kh[i]])

        def s4():
            P.mm(rC, ks[i][:], qs[i][:], start=True, stop=True)

        def s5():
            P.v('dve', 'tensor_mul', AT[i][:], rC, triu[:], reads=[pG, triu], writes=[AT[i]])

        def s6():
            P.mm(rD, AT[i][:], v_c, start=True, stop=False)
            P.mm(rD, qs[i][:], Sb[:], start=False, stop=True)
            P.mm(rE, kh[i][:], v_c, start=True, stop=True)

        def s7():
            P.act(B['o'][:, c * 64:(c + 1) * 64], rD, AF.Copy)
            P.v('dve', 'scalar_tensor_tensor', S[:], S[:], eb[i][:, 127:128], rE,
                ALU.mult, ALU.add, reads=[S, eb[i], pG], writes=[S])
            P.v('dve', 'tensor_copy', Sb[:], S[:], reads=[S], writes=[Sb])
            if c == GC - 1:
                P.dma('sp', go_d[:, g * GC * 64:(g + 1) * GC * 64], B['o'][:], W=[("go_d", g)], is_out=True)
        return [s0, s1, s2, s3, s4, s5, s6, s7]

    if 'gla' not in SKIP:
        for g in range(NCH // GC):
            for c in range(GC):
                gstages.extend(gla_chunk(g, c))
    LA = 2
    nb = len(blocks)
    gi_ = 0
    for i in range(nb + LA):
        if i < nb:
            stage_s(i)
        if i - LA >= 0:
            stage_pv(i - LA)
        want = len(gstages) if nb == 0 else min(len(gstages), (max(0, i - 4) * len(gstages)) // max(1, nb - 8) + 0)
        while gi_ < want:
            gstages[gi_]()
            gi_ += 1
    while gi_ < len(gstages):
        gstages[gi_]()
        gi_ += 1
    P.emit()
    return C.nc


def build_cmix_even():
    C = Ctx()
    P = C.P
    h = C.din("h", [TOK, 1024])
    a_d = C.din("a", [TOK, 512])
    go_d = C.din("go", [TOK, 512])
    r_d = C.din("r", [TOK, 512])
    ho = C.dout("ho", [TOK, 1024])
    C.ident()
    ggla = C.small("ggla", [128, 128])
    gpo = C.small("gpo", [128, 1024], q='pool')
    C._stages = [P.sb([128, 1024], F32, f"wstage{i}") for i in range(3)]
    Wout = load_w(C, "w_out", 8, 1024, chunk=1024)
    S = NormScr(C, 1024, "a")
    py = P.ps([128, 1024], F32, "py")
    bufs = []
    for i in range(2):
        bufs.append(dict(h=P.sb([128, 1024], F32, f"ht{i}"), a=P.sb([128, 512], F32, f"at{i}"),
                         g=P.sb([128, 512], F32, f"got{i}"), r=P.sb([128, 512], F32, f"rt{i}")))
    cat = P.sb([128, 1024], BF16, "cat")
    catT = P.sb([128, 8, 128], BF16, "catT")
    ss4 = P.sb([128, 4], F32, "ss4")
    rstd4 = P.sb([128, 4], F32, "rstd4")
    sr = P.sb([128, 512], F32, "sr")
    gn = P.sb([128, 512], F32, "gn")
    for j in range(NT):
        r0 = j * 128
        B = bufs[j % 2]
        P.dma('sp', B['h'][:], h[r0:r0 + 128, :])
        P.dma('pool', B['a'][:], a_d[r0:r0 + 128, :])
        P.dma('sp', B['g'][:], go_d[r0:r0 + 128, :])
        P.dma('pool', B['r'][:], r_d[r0:r0 + 128, :])
        P.v('pool', 'tensor_copy', cat[:, 0:512], B['a'][:], reads=[B['a']], writes=[cat])
        for hh in range(4):
            P.act(S.junk[:, :128], B['g'][:, hh * 128:(hh + 1) * 128], AF.Square, scale=128.0 ** -0.5,
                  accum_out=ss4[:, hh:hh + 1])
        P.act(rstd4[:], ss4[:], AF.Sqrt, bias=EPS)
        P.v('dve', 'reciprocal', rstd4[:], rstd4[:], reads=[rstd4], writes=[rstd4])
        P.act(sr[:], B['r'][:], AF.Silu)
        for hh in range(4):
            P.v('dve', 'scalar_tensor_tensor', gn[:, hh * 128:(hh + 1) * 128], B['g'][:, hh * 128:(hh + 1) * 128],
                rstd4[:, hh:hh + 1], ggla[:], ALU.mult, ALU.mult, reads=[B['g'], rstd4, ggla], writes=[gn])
        P.v('pool', 'tensor_mul', cat[:, 512:1024], gn[:], sr[:], reads=[gn, sr], writes=[cat])
        for c in range(8):
            P.tr(S.pt[:, c, :], cat[:, c * 128:(c + 1) * 128], C.idb[:])
        P.v('dve', 'tensor_copy', catT[:], S.pt[:], reads=[S.pt], writes=[catT])
        for n in range(2):
            for k in range(8):
                P.mm(py[:, n * 512:(n + 1) * 512], catT[:, k, :], Wout.sl(k, n * 512, (n + 1) * 512),
                     start=(k == 0), stop=(k == 7))
        postnorm_res(C, py[:], B['h'][:], gpo, S, S.junk)
        P.dma('sp', ho[r0:r0 + 128, :], B['h'][:], W=[("ho", r0)], is_out=True)
    P.emit()
    return C.nc


def build_a_odd():
    C = Ctx()
    P = C.P
    h = C.din("h", [TOK, 1024])
    hh_d = C.din("hh", [128, 1024])
    z_o = C.dout("z", [TOK, 1024])
    xc_o = C.dout("xcT", [1024, TOK])
    q_o = C.dout("qT", [1024, TOK], BF16)
    k_o = C.dout("kT", [1024, TOK], BF16)
    v_o = C.dout("vT", [1024, TOK], BF16)
    gi_o = C.dout("gi", [8, TOK])
    gl_o = C.dout("gl", [8, TOK])
    C.ident()
    gpre = C.small("gpre", [128, 8])
    cw = C.small("convw", [128, 32])
    cb = C.small("convb", [128, 8])
    bg = C.small("bg", [8, 1])
    C._stages = [P.sb([128, 2048], F32, f"wstage{i}") for i in range(2)]
    Win = load_w(C, "w_in", 8, 2048, g=gpre, chunk=2048)
    Wq = load_w(C, "wq", 8, 256, chunk=256)
    Wk = load_w(C, "wk", 8, 256, chunk=256)
    Wv = load_w(C, "wv", 8, 256, chunk=256)
    Wg = load_w(C, "wg", 24, 8, chunk=8)
    S = NormScr(C, 1024, "a")
    hr = [P.sb([128, 1024], F32, f"hr{j}") for j in range(2)]
    xT = P.sb([128, 8, 512], BF16, "xT")
    xm = [P.sb([128, 515], F32, f"xm{cc}") for cc in range(8)]
    xmb = [P.sb([128, 512], BF16, f"xmb{cc}") for cc in range(8)]
    xcb = [P.sb([128, 512], BF16, f"xcb{cc}") for cc in range(8)]
    qkvb = [[P.sb([128, 512], BF16, f"qkv{t}_{i}") for i in range(8)] for t in range(3)]
    acc = [P.sb([128, 512], F32, f"acc{i}") for i in range(2)]
    xcf = [P.sb([128, 512], F32, f"xcf{i}") for i in range(2)]
    zt = [P.sb([128, 1024], F32, f"zt{i}") for i in range(2)]
    gi = P.sb([8, 512], F32, "gi_sb")
    gl = P.sb([8, 512], F32, "gl_sb")
    pr = [P.ps([128, 512], F32, f"pr{i}") for i in range(3)]
    pz = P.ps([128, 1024], F32, "pz")
    pgt = P.ps([128, 512], F32, "pgt")
    P.dma('sp', hr[0][:], hh_d)
    norm_T(C, hr[0][:], 1024, S, xT[:, :, 0:128])
    npr = 0
    for cc in range(8):
        ps = pr[npr % 3]
        npr += 1
        for k in range(8):
            P.mm(ps[:, 0:128], Win.sl(k, cc * 128, (cc + 1) * 128), xT[:, k, 0:128], start=(k == 0), stop=(k == 7))
        P.act(xm[cc][:, 0:3], ps[:, 125:128], AF.Copy)
    nh = 1
    for g in range(TOK // 512):
        c0 = g * 512
        for j in range(4):
            ht = hr[nh % 2]
            nh += 1
            P.dma('sp', ht[:], h[c0 + j * 128:c0 + (j + 1) * 128, :])
            norm_T(C, ht[:], 1024, S, xT[:, :, j * 128:(j + 1) * 128])
        for cc in range(8):
            ps = pr[npr % 3]
            npr += 1
            for k in range(8):
                P.mm(ps[:], Win.sl(k, cc * 128, (cc + 1) * 128), xT[:, k, :], start=(k == 0), stop=(k == 7))
            P.act(xm[cc][:, 3:515], ps[:], AF.Copy)
            P.v('dve', 'tensor_copy', xmb[cc][:], xm[cc][:, 3:515], reads=[xm[cc]], writes=[xmb[cc]])
            a = acc[cc % 2]
            P.v('dve', 'tensor_scalar', a[:], xm[cc][:, 0:512], cw[:, cc * 4:cc * 4 + 1], cb[:, cc:cc + 1],
                ALU.mult, ALU.add, reads=[xm[cc], cw, cb], writes=[a])
            for i in range(1, 4):
                P.v('dve', 'scalar_tensor_tensor', a[:], xm[cc][:, i:i + 512],
                    cw[:, cc * 4 + i:cc * 4 + i + 1], a[:], ALU.mult, ALU.add, reads=[xm[cc], cw, a], writes=[a])
            xf = xcf[cc % 2]
            P.act(xf[:], a[:], AF.Silu)
            P.dma('pool', xc_o[cc * 128:(cc + 1) * 128, c0:c0 + 512], xf[:], W=[("xc_o", g, cc)], is_out=True)
            P.v('pool', 'tensor_copy', xcb[cc][:], xf[:], reads=[xf], writes=[xcb[cc]])
            P.v('pool', 'tensor_copy', xm[cc][:, 0:3], xm[cc][:, 512:515], reads=[xm[cc]], writes=[xm[cc]])
        for j in range(4):
            z = zt[j % 2]
            for n in range(2):
                for k in range(8):
                    P.mm(pz[:, n * 512:(n + 1) * 512], xT[:, k, j * 128:(j + 1) * 128],
                         Win.sl(k, 1024 + n * 512, 1024 + (n + 1) * 512), start=(k == 0), stop=(k == 7))
            P.act(z[:, 0:512], pz[:, 0:512], AF.Copy)
            P.v('dve', 'tensor_copy', z[:, 512:1024], pz[:, 512:1024], reads=[pz], writes=[z])
            P.dma('sp', z_o[c0 + j * 128:c0 + (j + 1) * 128, :], z[:], W=[("z_o", g, j)], is_out=True)
        for t, (W_, src, dst) in enumerate(((Wq, xcb, q_o), (Wk, xcb, k_o), (Wv, xmb, v_o))):
            for hd in range(4):
                for ec in range(2):
                    ps = pr[npr % 3]
                    npr += 1
                    for dc in range(2):
                        P.mm(ps[:], W_.sl(hd * 2 + dc, ec * 128, (ec + 1) * 128), src[hd * 2 + dc][:],
                             start=(dc == 0), stop=(dc == 1))
                    ob = qkvb[t][hd * 2 + ec]
                    if (hd * 2 + ec) % 2 == 0:
                        P.act(ob[:], ps[:], AF.Copy)
                    else:
                        P.v('dve', 'tensor_copy', ob[:], ps[:], reads=[ps], writes=[ob])
                    r0 = hd * 256 + ec * 128
                    P.dma('sp' if t != 1 else 'pool', dst[r0:r0 + 128, c0:c0 + 512], ob[:],
                          W=[("qkv_o", t, g, hd, ec)], is_out=True)
        n = 0
        for hd in range(4):
            for t in range(3):
                for ec in range(2):
                    P.mm(pgt[0:8, :], Wg.sl(hd * 6 + t * 2 + ec, 0, 8), qkvb[t][hd * 2 + ec][:],
                         start=(n == 0), stop=(n == 23))
                    n += 1
        P.act(gi[:], pgt[0:8, :], AF.Identity, bias=bg[:, 0:1])
        P.act(gl[:], gi[:], AF.Exp, scale=-1.0)
        P.act(gl[:], gl[:], AF.Ln, bias=1.0)
        P.v('dve', 'tensor_scalar_mul', gl[:], gl[:], -1.0, reads=[gl], writes=[gl])
        P.dma('sp', gi_o[:, c0:c0 + 512], gi[:], W=[("gi_o", g)], is_out=True)
        P.dma('sp', gl_o[:, c0:c0 + 512], gl[:], W=[("gl_o", g)], is_out=True)
    P.emit()
    return C.nc


def build_m_odd():
    C = Ctx()
    P = C.P
    nch = NCH
    qT_d = C.din("qT", [256, SEQ], BF16)
    kT_d = C.din("kT", [256, SEQ], BF16)
    kt_d = C.din("ktok", [128, nch * 256], BF16)
    v_d = C.din("v", [128, nch * 128], BF16)
    ho_d = C.dout("ho", [128, nch * 128])
    C.ident()
    idf = C.idf
    ii = C.small("ii", [128, nch])
    lf = C.small("lf", [128, nch])
    triu = C.small("triu", [128, 128])
    ones = P.sb([128, 128], F32, "ones")
    P.v('dve', 'memset', ones[:], 1.0, writes=[ones])
    pX = P.ps([128, 512], F32, "pX")
    pN = P.ps([128, 512], F32, "pN")
    pQ = P.ps([128, 512], F32, "pQ")
    pI = P.ps([128, 512], F32, "pI")
    pR = P.ps([128, 512], F32, "pR")
    pU = P.ps([128, 512], F32, "pU")

    def sbt(name, shape=None, dt=F32):
        return P.sb(shape or [128, nch], dt, name)

    def dv(name, *a, reads, writes, eng='dve'):
        P.v(eng, name, *a, reads=reads, writes=writes)

    b_all, u_all, c_all = sbt("b_all"), sbt("u_all"), sbt("c_all")
    P.mm(pX[:, 0:nch], triu[:], lf[:], start=True, stop=True)
    dv('tensor_copy', b_all[:], pX[:, 0:nch], reads=[pX], writes=[b_all])
    dv('tensor_sub', u_all[:], ii[:], b_all[:], reads=[ii, b_all], writes=[u_all])

    def prefix_max(src, dst, rows, n):
        sh = 1
        while sh < n:
            dv('tensor_copy', dst[0:rows, 0:sh], src[0:rows, 0:sh], reads=[src], writes=[dst])
            dv('tensor_max', dst[0:rows, sh:n], src[0:rows, sh:n], src[0:rows, 0:n - sh], reads=[src], writes=[dst])
            src, dst = dst, src
            sh *= 2
        return src

    x0, x1 = sbt("x0", [128, 128]), sbt("x1", [128, 128])
    P.tr(pX[0:nch, 0:128], u_all[:], idf[:])
    dv('tensor_copy', x0[0:nch, :], pX[0:nch, 0:128], reads=[pX], writes=[x0])
    cT = prefix_max(x0, x1, nch, 128)
    P.tr(pX[:, 0:nch], cT[0:nch, :], idf[0:nch, 0:nch])
    dv('tensor_copy', c_all[:], pX[:, 0:nch], reads=[pX], writes=[c_all])
    bT = sbt("bT", [128, 128])
    P.tr(pX[0:nch, 0:128], b_all[:], idf[:])
    dv('tensor_copy', bT[0:nch, :], pX[0:nch, 0:128], reads=[pX], writes=[bT])
    PB = sbt("PB", [128, 1])
    P.mm(pX[0:nch, 0:1], triu[0:nch, 0:nch], bT[0:nch, 127:128], start=True, stop=True)
    dv('tensor_copy', PB[0:nch, :], pX[0:nch, 0:1], reads=[pX], writes=[PB])
    wcol = sbt("wcol", [128, 1])
    dv('tensor_sub', wcol[0:nch, :], cT[0:nch, 127:128], PB[0:nch, :], reads=[cT, PB], writes=[wcol])
    dv('tensor_add', wcol[0:nch, :], wcol[0:nch, :], bT[0:nch, 127:128], reads=[wcol, bT], writes=[wcol])
    r0, r1 = sbt("r0", [1, 128]), sbt("r1", [1, 128])
    pbrow, brow, mrow, mprow = sbt("pbrow", [1, 128]), sbt("brow", [1, 128]), sbt("mrow", [1, 128]), sbt("mprow", [1, 128])
    for col, dst in ((wcol[0:nch, 0:1], r0), (PB[0:nch, 0:1], pbrow), (bT[0:nch, 127:128], brow)):
        P.tr(pX[0:1, 0:nch], col, idf[0:nch, 0:nch])
        dv('tensor_copy', dst[0:1, 0:nch], pX[0:1, 0:nch], reads=[pX], writes=[dst])
    pm = prefix_max(r0, r1, 1, nch)
    dv('tensor_scalar_max', mrow[0:1, 0:nch], pm[0:1, 0:nch], 0.0, reads=[pm], writes=[mrow])
    dv('tensor_add', mrow[0:1, 0:nch], mrow[0:1, 0:nch], pbrow[0:1, 0:nch], reads=[mrow, pbrow], writes=[mrow])
    dv('memset', mprow[:], 0.0, reads=[], writes=[mprow])
    if nch > 1:
        dv('tensor_copy', mprow[0:1, 1:nch], mrow[0:1, 0:nch - 1], reads=[mrow], writes=[mprow])
    mp_all, mn_all, be_all = sbt("mp_all"), sbt("mn_all"), sbt("be_all")
    for row, dst in ((mprow, mp_all), (mrow, mn_all), (brow, be_all)):
        P.mm(pX[:, 0:nch], ones[0:1, :], row[0:1, 0:nch], start=True, stop=True)
        dv('tensor_copy', dst[:], pX[:, 0:nch], reads=[pX], writes=[dst])
    mx_all, m_all, nmx_all, wi_all, em_all = sbt("mx_all"), sbt("m_all"), sbt("nmx_all"), sbt("wi_all"), sbt("em_all")
    d_all, ws_all, dec_all = sbt("d_all"), sbt("ws_all"), sbt("dec_all")
    dv('tensor_max', mx_all[:], c_all[:], mp_all[:], reads=[c_all, mp_all], writes=[mx_all])
    dv('tensor_add', m_all[:], b_all[:], mx_all[:], reads=[b_all, mx_all], writes=[m_all])
    dv('tensor_scalar_mul', nmx_all[:], mx_all[:], -1.0, reads=[mx_all], writes=[nmx_all])
    dv('tensor_sub', wi_all[:], mp_all[:], mx_all[:], reads=[mp_all, mx_all], writes=[wi_all])
    P.act(wi_all[:], wi_all[:], AF.Exp)
    dv('tensor_scalar_mul', wi_all[:], wi_all[:], 0.0625, reads=[wi_all], writes=[wi_all])
    P.act(em_all[:], m_all[:], AF.Exp, scale=-1.0)
    dv('tensor_sub', d_all[:], be_all[:], mn_all[:], reads=[be_all, mn_all], writes=[d_all])
    dv('tensor_add', ws_all[:], u_all[:], d_all[:], reads=[u_all, d_all], writes=[ws_all])
    P.act(ws_all[:], ws_all[:], AF.Exp)
    dv('tensor_add', dec_all[:], mp_all[:], d_all[:], reads=[mp_all, d_all], writes=[dec_all])
    P.act(dec_all[:], dec_all[:], AF.Exp)
    GC = min(8, nch)
    Cn = [P.sb([128, 129], F32, f"Cn{dc}") for dc in range(2)]
    Cnb = [P.sb([128, 129], BF16, f"Cnb{dc}") for dc in range(2)]
    for dc in range(2):
        dv('memset', Cn[dc][:], 0.0, reads=[], writes=[Cn[dc]])
        dv('memset', Cnb[dc][:], 0.0, reads=[], writes=[Cnb[dc]])
    bufs = []
    for i in range(2):
        B = dict(q=P.sb([128, 2, GC * 128], BF16, f"qg{i}"), k=P.sb([128, 2, GC * 128], BF16, f"kg{i}"),
                 kt=P.sb([128, GC * 256], BF16, f"ktg{i}"), v1=P.sb([128, GC, 129], BF16, f"v1g{i}"),
                 ho=P.sb([128, GC, 128], F32, f"hog{i}"))
        dv('memset', B['v1'][:], 1.0, reads=[], writes=[B['v1']], eng='pool')
        bufs.append(B)
    dN = [sbt(f"dN{i}", [128, 128]) for i in range(2)]
    E = [sbt(f"E{i}", [128, 128]) for i in range(2)]
    WT = [sbt(f"WT{i}", [128, 128], BF16) for i in range(2)]
    isb = [sbt(f"isb{i}", [128, 129]) for i in range(2)]
    tot = [sbt(f"tot{i}", [128, 129]) for i in range(2)]
    den = [sbt(f"den{i}", [128, 1]) for i in range(2)]
    kw = [sbt(f"kw{i}", [128, 256], BF16) for i in range(2)]
    pU2 = P.ps([128, 512], F32, "pU2")
    pUs = [pU, pU2]

    def part_a(j):
        g, c = divmod(j, GC)
        B = bufs[g % 2]
        i = j % 2
        sl = slice(c * 128, (c + 1) * 128)
        if c == 0:
            t0 = g * GC * 128
            for dc in range(2):
                P.dma('sp', B['q'][:, dc, :], qT_d[dc * 128:(dc + 1) * 128, t0:t0 + GC * 128])
                P.dma('pool', B['k'][:, dc, :], kT_d[dc * 128:(dc + 1) * 128, t0:t0 + GC * 128])
            P.dma('sp', B['kt'][:], kt_d[:, g * GC * 256:(g + 1) * GC * 256])
            P.dma('pool', B['v1'][:, :, 0:128],
                  v_d[:, g * GC * 128:(g + 1) * GC * 128].rearrange("p (c e) -> p c e", e=128))
        dv('tensor_scalar_mul', dN[i][:], idf[:], nmx_all[:, j:j + 1], reads=[idf, nmx_all], writes=[dN[i]])
        P.mm(pN[:, 0:128], ones[:], dN[i][:], start=True, stop=True)
        P.act(E[i][:], pN[:, 0:128], AF.Exp, bias=u_all[:, j:j + 1])
        dv('tensor_mul', E[i][:], E[i][:], triu[:], reads=[E[i], triu], writes=[E[i]], eng='pool')
        for dc in range(2):
            P.mm(pQ[:, 0:128], B['k'][:, dc, sl], B['q'][:, dc, sl], start=(dc == 0), stop=(dc == 1))
        dv('scalar_tensor_tensor', WT[i][:], pQ[:, 0:128], 0.0625, E[i][:], ALU.mult, ALU.mult,
           reads=[pQ, E[i]], writes=[WT[i]])
        P.mm(pI[:, 0:129], WT[i][:], B['v1'][:, c, :], start=True, stop=True)
        P.act(isb[i][:], pI[:, 0:129], AF.Copy)
        dv('tensor_scalar_mul', kw[i][:], B['kt'][:, c * 256:(c + 1) * 256], ws_all[:, j:j + 1],
           reads=[B['kt'], ws_all], writes=[kw[i]], eng='pool')
        for dc in range(2):
            P.mm(pUs[i][:, dc * 129:(dc + 1) * 129], kw[i][:, dc * 128:(dc + 1) * 128], B['v1'][:, c, :],
                 start=True, stop=True)

    def part_b(j):
        g, c = divmod(j, GC)
        B = bufs[g % 2]
        i = j % 2
        sl = slice(c * 128, (c + 1) * 128)
        for dc in range(2):
            P.mm(pR[:, 0:129], B['q'][:, dc, sl], Cnb[dc][:], start=(dc == 0), stop=(dc == 1))
        dv('scalar_tensor_tensor', tot[i][:], pR[:, 0:129], wi_all[:, j:j + 1], isb[i][:], ALU.mult, ALU.add,
           reads=[pR, wi_all, isb[i]], writes=[tot[i]])
        for dc in range(2):
            dv('scalar_tensor_tensor', Cn[dc][:], Cn[dc][:], dec_all[:, j:j + 1], pUs[i][:, dc * 129:(dc + 1) * 129],
               ALU.mult, ALU.add, reads=[Cn[dc], dec_all, pUs[i]], writes=[Cn[dc]])
            P.act(Cnb[dc][:], Cn[dc][:], AF.Copy)
        dv('tensor_scalar_mul', den[i][:], tot[i][:, 128:129], -1.0, reads=[tot[i]], writes=[den[i]])
        dv('tensor_max', den[i][:], den[i][:], tot[i][:, 128:129], reads=[den[i], tot[i]], writes=[den[i]])
        dv('tensor_max', den[i][:], den[i][:], em_all[:, j:j + 1], reads=[den[i], em_all], writes=[den[i]])
        dv('reciprocal', den[i][:], den[i][:], reads=[den[i]], writes=[den[i]])
        P.act(B['ho'][:, c, :], tot[i][:, 0:128], AF.Copy, scale=den[i][:])
        if c == GC - 1:
            P.dma('sp', ho_d[:, g * GC * 128:(g + 1) * GC * 128], B['ho'][:].rearrange("p c e -> p (c e)"),
                  W=[("ho_d", g)], is_out=True)

    part_a(0)
    for j in range(nch):
        if j + 1 < nch:
            part_a(j + 1)
        part_b(j)
    P.emit()
    return C.nc


def build_cmix_odd():
    C = Ctx()
    P = C.P
    h = C.din("h", [TOK, 1024])
    hc_d = C.din("hc", [TOK, 1024])
    xc_d = C.din("xc", [TOK, 1024])
    z_d = C.din("z", [TOK, 1024])
    ho = C.dout("ho", [TOK, 1024])
    C.ident()
    ghn = C.small("ghn", [128, 1024])
    skp = C.small("skip", [128, 1024], q='pool')
    gpo = C.small("gpo", [128, 1024], q='pool')
    C._stages = [P.sb([128, 1024], F32, f"wstage{i}") for i in range(3)]
    Wout = load_w(C, "w_out", 8, 1024, chunk=1024)
    S = NormScr(C, 1024, "a")
    py = P.ps([128, 1024], F32, "py")
    bufs = []
    for i in range(2):
        bufs.append(dict(h=P.sb([128, 1024], F32, f"ht{i}"), c=P.sb([128, 1024], F32, f"hct{i}"),
                         x=P.sb([128, 1024], F32, f"xct{i}"), z=P.sb([128, 1024], F32, f"zt{i}")))
    cat = P.sb([128, 1024], BF16, "cat")
    catT = P.sb([128, 8, 128], BF16, "catT")
    mu4 = P.sb([128, 4], F32, "mu4")
    ss4 = P.sb([128, 4], F32, "ss4")
    rstd4 = P.sb([128, 4], F32, "rstd4")
    cen = P.sb([128, 1024], F32, "cen")
    hn = P.sb([128, 1024], F32, "hn")
    sz = P.sb([128, 1024], F32, "sz")
    for j in range(NT):
        r0 = j * 128
        B = bufs[j % 2]
        P.dma('sp', B['h'][:], h[r0:r0 + 128, :])
        P.dma('pool', B['c'][:], hc_d[r0:r0 + 128, :])
        P.dma('sp', B['x'][:], xc_d[r0:r0 + 128, :])
        P.dma('pool', B['z'][:], z_d[r0:r0 + 128, :])
        for hh in range(4):
            sl = slice(hh * 256, (hh + 1) * 256)
            P.act(S.junk[:, :256], B['c'][:, sl], AF.Copy, scale=1.0 / 256.0, accum_out=mu4[:, hh:hh + 1])
            P.v('dve', 'tensor_scalar_sub', cen[:, sl], B['c'][:, sl], mu4[:, hh:hh + 1],
                reads=[B['c'], mu4], writes=[cen])
            P.act(S.junk[:, :256], cen[:, sl], AF.Square, scale=1.0 / 16.0, accum_out=ss4[:, hh:hh + 1])
        P.act(rstd4[:], ss4[:], AF.Sqrt, bias=EPS)
        P.v('dve', 'reciprocal', rstd4[:], rstd4[:], reads=[rstd4], writes=[rstd4])
        P.act(sz[:], B['z'][:], AF.Silu)
        for hh in range(4):
            sl = slice(hh * 256, (hh + 1) * 256)
            P.v('dve', 'scalar_tensor_tensor', hn[:, sl], cen[:, sl], rstd4[:, hh:hh + 1], ghn[:, sl],
                ALU.mult, ALU.mult, reads=[cen, rstd4, ghn], writes=[hn])
        P.v('pool', 'tensor_mul', B['x'][:], B['x'][:], skp[:], reads=[B['x'], skp], writes=[B['x']])
        P.v('pool', 'tensor_add', hn[:], hn[:], B['x'][:], reads=[hn, B['x']], writes=[hn])
        P.v('pool', 'tensor_mul', cat[:], hn[:], sz[:], reads=[hn, sz], writes=[cat])
        for c in range(8):
            P.tr(S.pt[:, c, :], cat[:, c * 128:(c + 1) * 128], C.idb[:])
        P.v('dve', 'tensor_copy', catT[:], S.pt[:], reads=[S.pt], writes=[catT])
        for n in range(2):
            for k in range(8):
                P.mm(py[:, n * 512:(n + 1) * 512], catT[:, k, :], Wout.sl(k, n * 512, (n + 1) * 512),
                     start=(k == 0), stop=(k == 7))
        postnorm_res(C, py[:], B['h'][:], gpo, S, S.junk)
        P.dma('sp', ho[r0:r0 + 128, :], B['h'][:], W=[("ho", r0)], is_out=True)
    P.emit()
    return C.nc


def wl(w):
    K, N = w.shape
    KC = K // 128
    return np.ascontiguousarray(w.reshape(KC, 128, N).transpose(1, 0, 2).reshape(128, KC * N))


def vl(g):
    return np.ascontiguousarray(g.reshape(-1, 128).T)


def bc(g, p=128):
    return np.ascontiguousarray(np.broadcast_to(g.reshape(1, -1), (p, g.size)))


_IDN = np.eye(128, dtype=np.float32)
_PROGS = {}


def prog(name, builder):
    if name not in _PROGS:
        _PROGS[name] = builder()
    return _PROGS[name]


def run(nc, maps):
    res = run_bass_kernel_spmd(nc, maps, core_ids=list(range(NCORES)))
    return res.results


def run_ffn(h, w1, w2, gpre, gpost):
    nc = prog("ffn", build_ffn)
    com = {"idn": _IDN, "gpre": vl(gpre), "gpo": bc(gpost), "w1": wl(w1), "w2": wl(w2)}
    maps = [dict(com, h=np.ascontiguousarray(h[c * TOK:(c + 1) * TOK])) for c in range(NCORES)]
    r = run(nc, maps)
    return np.concatenate([r[c]["ho"] for c in range(NCORES)], axis=0)


def run_xa(h, mem, wq, wk, wv, wo, gpre, gmem, gpost):
    nc = prog("xa", build_xa)
    com = {"idn": _IDN, "gpre": vl(gpre), "gmem": vl(gmem), "gpo": bc(gpost), "wq": wl(wq), "wk": wl(wk),
           "wv": wl(wv), "wo": wl(wo), "mem": np.ascontiguousarray(mem)}
    maps = [dict(com, h=np.ascontiguousarray(h[c * TOK:(c + 1) * TOK])) for c in range(NCORES)]
    r = run(nc, maps)
    return np.concatenate([r[c]["ho"] for c in range(NCORES)], axis=0)


def bf(x):
    import ml_dtypes
    return np.ascontiguousarray(x).astype(ml_dtypes.bfloat16)


def inv_freq_tab():
    inv = (np.float32(10000.0) ** (-np.arange(0, 32, 2, dtype=np.float32) / np.float32(32))).astype(np.float32)
    return np.ascontiguousarray(np.broadcast_to(np.tile(inv, 8)[None, :], (128, 128)))


def run_a_even(h, positions, w_in, gpre, g_q, w_uq, g_kv, w_ukv, w_gate, b_gate):
    nc = prog("a_even", build_a_even)
    com = {"idn": _IDN, "gpre": vl(gpre), "gq": vl(g_q), "gkv": vl(g_kv), "w_in": wl(w_in), "w_uq": wl(w_uq),
           "w_ukv": wl(w_ukv), "wga": np.ascontiguousarray(np.concatenate([w_gate, b_gate[None, :]], 0)),
           "invf": inv_freq_tab()}
    maps = []
    for c in range(NCORES):
        pos = positions[c * TOK:(c + 1) * TOK].reshape(NT, 128).T
        maps.append(dict(com, h=np.ascontiguousarray(h[c * TOK:(c + 1) * TOK]), pos=np.ascontiguousarray(pos)))
    r = run(nc, maps)
    return {k: np.concatenate([r[c][k] for c in range(NCORES)], axis=0) for k in ("q", "kv", "kpe", "gqkv", "la", "r")}


def tiles_pm(x):
    T = x.shape[0] // 128
    d = x.shape[1]
    return np.ascontiguousarray(x.reshape(T, 128, d).transpose(1, 0, 2).reshape(128, T * d))


def untiles_pm(y, d):
    T = y.shape[1] // d
    return np.ascontiguousarray(y.reshape(128, T, d).transpose(1, 0, 2).reshape(T * 128, d))


_TRIU = np.triu(np.ones((128, 128), np.float32))
_TRIL = np.tril(np.ones((128, 128), np.float32), -1).T.copy()
_TRIL = np.ascontiguousarray((np.arange(128)[:, None] > np.arange(128)[None, :]).astype(np.float32))


def run_m_even(q, kv, kpe, gqkv, la):
    nc = prog("m_even", build_m_even)
    maps = []
    kpeT = np.ascontiguousarray(kpe.T)
    for c in range(NCORES):
        hg, half = c // 2, c % 2
        qT = np.ascontiguousarray(q[:, c * 96:(c + 1) * 96].T)
        kT = np.ascontiguousarray(np.concatenate([kv[:, c * 128:c * 128 + 64].T, kpeT], axis=0))
        maps.append({
            "qT": qT, "kT": kT, "v": tiles_pm(kv[:, c * 128 + 64:(c + 1) * 128]),
            "trib": bf(_TRIU), "triu": _TRIU, "tril": _TRIL,
            "gqT": np.ascontiguousarray(gqkv[:, hg * 64:(hg + 1) * 64].T),
            "gkT": np.ascontiguousarray(gqkv[:, 256 + hg * 64:256 + (hg + 1) * 64].T),
            "gk": tiles_pm(gqkv[:, 256 + hg * 64:256 + (hg + 1) * 64]),
            "gv": tiles_pm(gqkv[:, 512 + hg * 128 + half * 64:512 + hg * 128 + (half + 1) * 64]),
            "la": tiles_pm(la[:, hg * 64:(hg + 1) * 64]),
        })
    r = run(nc, maps)
    a = np.concatenate([untiles_pm(r[c]["o"], 64) for c in range(NCORES)], axis=1)
    go = np.concatenate([untiles_pm(r[c]["go"], 64) for c in range(NCORES)], axis=1)
    return a, go


def shard(x):
    return [np.ascontiguousarray(x[c * TOK:(c + 1) * TOK]) for c in range(NCORES)]


def run_cmix_even(h, a, go, r, g_gla, w_out, gpost):
    nc = prog("cmix_even", build_cmix_even)
    com = {"idn": _IDN, "ggla": bc(g_gla), "gpo": bc(gpost), "w_out": wl(w_out)}
    hs, as_, gs, rs = shard(h), shard(a), shard(go), shard(r)
    maps = [dict(com, h=hs[c], a=as_[c], go=gs[c], r=rs[c]) for c in range(NCORES)]
    res = run(nc, maps)
    return np.concatenate([res[c]["ho"] for c in range(NCORES)], axis=0)


def run_a_odd(h, w_in, gpre, conv_w, conv_b, w_q, w_k, w_v, w_gates, b_gates):
    nc = prog("a_odd", build_a_odd)

    def hw(w):
        return np.ascontiguousarray(w.reshape(4, 2, 128, 256).transpose(2, 0, 1, 3).reshape(128, 8 * 256))
    com = {"idn": _IDN, "gpre": vl(gpre), "w_in": wl(w_in),
           "convw": np.ascontiguousarray(conv_w.reshape(4, 8, 128).transpose(2, 1, 0).reshape(128, 32)),
           "convb": vl(conv_b), "bg": np.ascontiguousarray(b_gates.reshape(8, 1)),
           "wq": hw(w_q), "wk": hw(w_k), "wv": hw(w_v), "wg": wl(w_gates)}
    maps = []
    for c in range(NCORES):
        hh = h[c * TOK - 128:c * TOK] if c > 0 else np.zeros((128, 1024), np.float32)
        maps.append(dict(com, h=np.ascontiguousarray(h[c * TOK:(c + 1) * TOK]), hh=np.ascontiguousarray(hh)))
    r = run(nc, maps)
    out = {k: np.concatenate([r[c][k] for c in range(NCORES)], axis=1) for k in ("xcT", "qT", "kT", "vT", "gi", "gl")}
    out["z"] = np.concatenate([r[c]["z"] for c in range(NCORES)], axis=0)
    return out


def run_m_odd(qT, kT, vT, gi, gl):
    nc = prog("m_odd", build_m_odd)
    maps = []
    for c in range(NCORES):
        hd, half = c // 2, c % 2
        q = qT[hd * 256:(hd + 1) * 256]
        k = kT[hd * 256:(hd + 1) * 256]
        v = vT[hd * 256 + half * 128:hd * 256 + (half + 1) * 128]
        maps.append({"idn": _IDN, "triu": _TRIU, "qT": np.ascontiguousarray(q), "kT": np.ascontiguousarray(k),
                     "ktok": tiles_pm(np.ascontiguousarray(k.T)), "v": tiles_pm(np.ascontiguousarray(v.T)),
                     "ii": np.ascontiguousarray(gi[hd].reshape(-1, 128).T),
                     "lf": np.ascontiguousarray(gl[4 + hd].reshape(-1, 128).T)})
    r = run(nc, maps)
    return np.concatenate([untiles_pm(r[c]["ho"], 128) for c in range(NCORES)], axis=1)


def run_cmix_odd(h, hc, xc, z, g_hnorm, skip, w_out, gpost):
    nc = prog("cmix_odd", build_cmix_odd)
    com = {"idn": _IDN, "ghn": bc(g_hnorm), "skip": bc(skip), "gpo": bc(gpost), "w_out": wl(w_out)}
    hs, cs, xs, zs = shard(h), shard(hc), shard(xc), shard(z)
    maps = [dict(com, h=hs[c], hc=cs[c], xc=xs[c], z=zs[c]) for c in range(NCORES)]
    res = run(nc, maps)
    return np.concatenate([res[c]["ho"] for c in range(NCORES)], axis=0)


def kernel(x, mem, positions,
           g_mix_pre, g_mix_post, g_xattn_pre, g_xattn_post, g_mem, g_ffn_pre, g_ffn_post,
           ev_w_in, ev_g_q, ev_w_uq, ev_g_kv, ev_w_ukv, ev_w_gate, ev_b_gate, ev_g_gla, ev_w_out,
           od_w_in, od_conv_w, od_conv_b, od_w_q, od_w_k, od_w_v, od_w_gates, od_b_gates,
           od_g_hnorm, od_skip, od_w_out,
           xa_w_q, xa_w_k, xa_w_v, xa_w_o,
           ffn_w1, ffn_w2):
    f = lambda a: np.asarray(a)
    h = np.ascontiguousarray(f(x)[0], dtype=np.float32)
    memv = np.ascontiguousarray(f(mem)[0], dtype=np.float32)
    pos = np.ascontiguousarray(f(positions)[0]).astype(np.int32)
    for layer in range(4):
        j = layer // 2
        if layer % 2 == 0:
            o = run_a_even(h, pos, f(ev_w_in)[j], f(g_mix_pre)[layer], f(ev_g_q)[j], f(ev_w_uq)[j], f(ev_g_kv)[j],
                           f(ev_w_ukv)[j], f(ev_w_gate)[j], f(ev_b_gate)[j])
            a, go = run_m_even(o["q"], o["kv"], o["kpe"], o["gqkv"], o["la"])
            h = run_cmix_even(h, a, go, o["r"], f(ev_g_gla)[j], f(ev_w_out)[j], f(g_mix_post)[layer])
        else:
            o = run_a_odd(h, f(od_w_in)[j], f(g_mix_pre)[layer], f(od_conv_w)[j], f(od_conv_b)[j], f(od_w_q)[j],
                          f(od_w_k)[j], f(od_w_v)[j], f(od_w_gates)[j], f(od_b_gates)[j])
            hc = run_m_odd(o["qT"], o["kT"], o["vT"], o["gi"], o["gl"])
            h = run_cmix_odd(h, hc, np.ascontiguousarray(o["xcT"].T), o["z"], f(od_g_hnorm)[j], f(od_skip)[j],
                             f(od_w_out)[j], f(g_mix_post)[layer])
        h = run_xa(h, memv, f(xa_w_q)[layer], f(xa_w_k)[layer], f(xa_w_v)[layer], f(xa_w_o)[layer],
                   f(g_xattn_pre)[layer], f(g_mem)[layer], f(g_xattn_post)[layer])
        h = run_ffn(h, f(ffn_w1)[layer], f(ffn_w2)[layer], f(g_ffn_pre)[layer], f(g_ffn_post)[layer])
    return h[None].astype(np.float32)
```

```python
import contextlib
import numpy as np
import concourse.bass as bass
import concourse.mybir as mybir
from concourse.bass_utils import run_bass_kernel_spmd

F32 = mybir.dt.float32
BF16 = mybir.dt.bfloat16
I32 = mybir.dt.int32
AF = mybir.ActivationFunctionType
ALU = mybir.AluOpType
AX = mybir.AxisListType

COMPUTE = ('pe', 'act', 'dve', 'pool')
NDSEM = 8


class _Op:
    __slots__ = ('eng', 'fn', 'dma', 'deps', 'has_dep', 'cval', 'dq', 'dslot', 'dval', 'idx')


class Prog:
    def __init__(self, nc, self_sync=True):
        self.nc = nc
        self.ops = []
        self.last_w = {}
        self.readers = {}
        self.stack = contextlib.ExitStack()
        self.self_sync = self_sync
        self.ndma = {}
        self.out_dmas = []
        self._n = 0
        self.psum_names = set()

    def sb(self, shape, dt, name=None):
        self._n += 1
        name = name or f"sb{self._n}"
        return self.stack.enter_context(self.nc.sbuf_tensor(name, list(shape), dt))

    def ps(self, shape, dt=F32, name=None):
        self._n += 1
        name = name or f"ps{self._n}"
        self.psum_names.add(name)
        return self.stack.enter_context(self.nc.psum_tensor(name, list(shape), dt))

    @staticmethod
    def _keys(xs):
        out = []
        for x in xs:
            if x is None or isinstance(x, (int, float)):
                continue
            if isinstance(x, (str, tuple)):
                out.append(x)
            else:
                out.append(x.name)
        return out

    def op(self, eng, fn, reads=(), writes=(), dma=False):
        o = _Op()
        o.eng = eng
        o.fn = fn
        o.dma = dma
        o.has_dep = False
        o.idx = len(self.ops)
        deps = set()
        pn = self.psum_names
        rk = [k[0] if (isinstance(k, tuple) and k[0] in pn) else k for k in self._keys(reads)]
        wk = [k[0] if (isinstance(k, tuple) and k[0] in pn) else k for k in self._keys(writes)]
        for k in rk:
            if k in self.last_w:
                deps.add(self.last_w[k])
            if k in pn:
                r = self.readers.get(k)
                if r:
                    for e2, v in r.items():
                        if e2 != eng and not isinstance(v, list):
                            deps.add(v)
        for k in wk:
            if k in self.last_w:
                deps.add(self.last_w[k])
            r = self.readers.get(k)
            if r:
                for v in r.values():
                    if isinstance(v, list):
                        deps.update(v)
                    else:
                        deps.add(v)
        deps.discard(o.idx)
        o.deps = deps
        if dma:
            j = self.ndma.get(eng, 0)
            self.ndma[eng] = j + 1
            o.dq = eng
            o.dslot = j % NDSEM
            o.dval = 16 * (j // NDSEM + 1)
        self.ops.append(o)
        for k in wk:
            self.last_w[k] = o.idx
            self.readers[k] = {}
        for k in rk:
            if k in wk:
                continue
            r = self.readers.setdefault(k, {})
            if dma:
                r.setdefault('dma', []).append(o.idx)
            else:
                r[eng] = o.idx
        return o

    def dma(self, q, out, in_, R=None, W=None, is_out=False, **kw):
        o = self.op(q, lambda e: e.dma_start(out=out, in_=in_, **kw),
                    reads=[in_] if R is None else R, writes=[out] if W is None else W, dma=True)
        if is_out:
            self.out_dmas.append(o.idx)
        return o

    def mm(self, out, lhsT, rhs, start=True, stop=True, R=None, W=None, **kw):
        return self.op('pe', lambda e: e.matmul(out, lhsT, rhs, start=start, stop=stop, **kw),
                       reads=[lhsT, rhs] if R is None else R, writes=[out] if W is None else W)

    def tr(self, out, in_, ident, R=None, W=None):
        return self.op('pe', lambda e: e.transpose(out, in_, ident),
                       reads=[in_, ident] if R is None else R, writes=[out] if W is None else W)

    def act(self, out, in_, func, bias=None, scale=None, accum_out=None, R=None, W=None, eng='act'):
        kw = {}
        if bias is not None:
            kw['bias'] = bias
        if scale is not None:
            kw['scale'] = scale
        if accum_out is not None:
            kw['accum_out'] = accum_out
        return self.op(eng, lambda e: e.activation(out, in_, func, **kw),
                       reads=[in_, bias, scale] if R is None else R, writes=[out, accum_out] if W is None else W)

    def v(self, eng, name, *args, reads=(), writes=(), **kw):
        return self.op(eng, lambda e: getattr(e, name)(*args, **kw), reads=reads, writes=writes)

    def emit(self):
        nc = self.nc
        ops = self.ops
        for o in ops:
            for d in o.deps:
                ops[d].has_dep = True
        for i in self.out_dmas:
            ops[i].has_dep = True
        st = self.stack
        csem = {e: st.enter_context(nc.semaphore(f"c_{e}")) for e in COMPUTE}
        dsem = {}
        for q in self.ndma:
            dsem[q] = [st.enter_context(nc.semaphore(f"d_{q}_{i}")) for i in range(NDSEM)]
        cnt = {e: 0 for e in COMPUTE}
        for o in ops:
            if not o.dma and o.has_dep:
                cnt[o.eng] += 1
                o.cval = cnt[o.eng]
            elif not o.dma:
                o.cval = None
        per_eng = {}
        for o in ops:
            per_eng.setdefault(o.eng, []).append(o)
        waited = {}
        dma_hist = {q: [] for q in self.ndma}

        def emit_engine(ename, e):
            wl = waited.setdefault(ename, {})
            for o in per_eng.get(ename, []):
                need = {}
                for d in o.deps:
                    p = ops[d]
                    if p.dma:
                        sem = dsem[p.dq][p.dslot]
                        val = p.dval
                    else:
                        if p.eng == ename and (ename == 'pe' or not self.self_sync):
                            continue
                        sem = csem[p.eng]
                        val = p.cval
                    if need.get(id(sem), (None, 0))[1] < val:
                        need[id(sem)] = (sem, val)
                if o.dma and o.dval > 16:
                    sem = dsem[o.dq][o.dslot]
                    k = id(sem)
                    if need.get(k, (None, 0))[1] < o.dval - 16:
                        need[k] = (sem, o.dval - 16)
                for k, (sem, val) in need.items():
                    if wl.get(k, 0) >= val:
                        continue
                    wl[k] = val
                    e.wait_ge(sem, val)
                ins = o.fn(e)
                if o.dma:
                    ins.then_inc(dsem[o.dq][o.dslot], 16)
                elif o.has_dep:
                    ins.then_inc(csem[o.eng], 1)
            if ename == 'sp':
                for i in self.out_dmas:
                    p = ops[i]
                    e.wait_ge(dsem[p.dq][p.dslot], p.dval)

        with nc.Block() as block:
            @block.sync
            def _(e):
                emit_engine('sp', e)

            @block.tensor
            def _(e):
                emit_engine('pe', e)

            @block.scalar
            def _(e):
                emit_engine('act', e)

            @block.vector
            def _(e):
                emit_engine('dve', e)

            @block.gpsimd
            def _(e):
                emit_engine('pool', e)
        self.stack.close()


EPS = 1e-6
NCORES = 8
TOK = 2048
NT = TOK // 128


class Ctx:
    def __init__(self):
        self.nc = bass.Bass("TRN2", target_bir_lowering=False)
        self.P = Prog(self.nc)
        self.rr = 0

    def din(self, name, shape, dt=F32):
        return self.nc.dram_tensor(name, list(shape), dt, kind="ExternalInput").ap()

    def dout(self, name, shape, dt=F32):
        return self.nc.dram_tensor(name, list(shape), dt, kind="ExternalOutput").ap()

    def ident(self):
        P = self.P
        idn = self.din("idn", [128, 128])
        self.idf = P.sb([128, 128], F32, "idf")
        self.idb = P.sb([128, 128], BF16, "idb")
        P.dma('sp', self.idf[:], idn)
        P.v('dve', 'tensor_copy', self.idb[:], self.idf[:], reads=[self.idf], writes=[self.idb])

    def small(self, name, shape, dt=F32, q='sp'):
        d = self.din(name, shape, dt)
        t = self.P.sb(shape, dt, "s_" + name)
        self.P.dma(q, t[:], d)
        return t


class WMat:
    def __init__(self, C, name, KC, N, rows=128):
        self.t = [C.P.sb([rows, N], BF16, f"{name}_{c}") for c in range(KC)]
        self.KC, self.N, self.rows = KC, N, rows

    def sl(self, c, lo, hi):
        return self.t[c][:, lo:hi]


def load_w(C, name, KC, N, g=None, rows=128, chunk=2048, stages=None, q=('sp', 'pool'), engs=('act', 'dve', 'pool')):
    P = C.P
    w = C.din(name, [rows, KC * N])
    W = WMat(C, name, KC, N, rows)
    if stages is None:
        if not hasattr(C, '_stages'):
            C._stages = [P.sb([128, chunk], F32, f"wstage{i}") for i in range(3)]
        stages = C._stages
    i = C.__dict__.get('_wi', 0)
    for c in range(KC):
        eng = engs[c % len(engs)]
        for n0 in range(0, N, chunk):
            n = min(chunk, N - n0)
            st = stages[i % len(stages)]
            P.dma(q[i % len(q)], st[:rows, :n], w[:, c * N + n0:c * N + n0 + n])
            dst = W.t[c][:, n0:n0 + n]
            src = st[:rows, :n]
            if g is None:
                if eng == 'act':
                    P.act(dst, src, AF.Copy)
                else:
                    P.v(eng, 'tensor_copy', dst, src, reads=[st], writes=[W.t[c]])
            else:
                gs = g[:rows, c:c + 1]
                if eng == 'act':
                    P.act(dst, src, AF.Copy, scale=gs)
                else:
                    P.v(eng, 'tensor_scalar_mul', dst, src, gs, reads=[st, g], writes=[W.t[c]])
            i += 1
    C._wi = i
    return W


class NormScr:
    def __init__(self, C, D, tag, pt=None):
        P = C.P
        self.junk = P.sb([128, D], F32, f"junk_{tag}")
        self.ss = P.sb([128, 1], F32, f"ss_{tag}")
        self.rstd = P.sb([128, 1], F32, f"rstd_{tag}")
        self.xb = P.sb([128, D], BF16, f"xb_{tag}")
        self.pt = pt if pt is not None else P.ps([128, D // 128, 128], BF16, f"pt_{tag}")
        self.D = D


def rstd_of(C, src, D, S, ss=None, rstd=None, junk=None):
    P = C.P
    ss = S.ss if ss is None else ss
    rstd = S.rstd if rstd is None else rstd
    junk = S.junk if junk is None else junk
    P.act(junk, src, AF.Square, scale=float(D) ** -0.5, accum_out=ss)
    P.act(rstd, ss, AF.Sqrt, bias=EPS)
    P.v('dve', 'reciprocal', rstd, rstd, reads=[rstd], writes=[rstd])


def norm_T(C, src, D, S, dst, copy_eng='dve'):
    P = C.P
    rstd_of(C, src, D, S, S.ss[:], S.rstd[:], S.junk[:, :D])
    P.act(S.xb[:, :D], src, AF.Copy, scale=S.rstd[:])
    for c in range(D // 128):
        P.tr(S.pt[:, c, :], S.xb[:, c * 128:(c + 1) * 128], C.idb[:])
    if copy_eng == 'act':
        P.act(dst, S.pt[:, :D // 128, :], AF.Copy)
    else:
        P.v('dve', 'tensor_copy', dst, S.pt[:, :D // 128, :], reads=[S.pt], writes=[dst])


def postnorm_res(C, py, ht, gpo, S, tmp):
    P = C.P
    rstd_of(C, py, 1024, S, S.ss[:], S.rstd[:], S.junk[:, :1024])
    P.v('dve', 'scalar_tensor_tensor', tmp[:], py, S.rstd[:], gpo[:], ALU.mult, ALU.mult,
        reads=[py, S.rstd, gpo], writes=[tmp])
    P.v('pool', 'tensor_add', ht, ht, tmp[:], reads=[ht, tmp], writes=[ht])


def build_ffn():
    C = Ctx()
    P = C.P
    h = C.din("h", [TOK, 1024])
    ho = C.dout("ho", [TOK, 1024])
    C.ident()
    gpre = C.small("gpre", [128, 8])
    gpo = C.small("gpo", [128, 1024], q='pool')
    C._stages = [P.sb([128, 1024], F32, f"wstage{i}") for i in range(2)]
    W1 = load_w(C, "w1", 8, 4096, g=gpre, chunk=1024)
    S = NormScr(C, 1024, "a")
    hr = [P.sb([128, 1024], F32, f"hr{j}") for j in range(2)]
    xTs = [P.sb([128, 8, 512], BF16, f"xT{i}") for i in range(2)]
    h1T = [P.sb([128, 512], BF16, f"h1T{f}") for f in range(32)]
    rl = [P.sb([128, 512], BF16, f"rl{i}") for i in range(3)]
    p1 = [P.ps([128, 512], F32, f"p1_{i}") for i in range(3)]
    pys = [P.ps([128, 1024], F32, f"py{i}") for i in range(2)]
    st = dict(n1=0, nh=0)

    def norm_tile(g, j):
        r0 = g * 512 + j * 128
        ht = hr[st['nh'] % 2]
        st['nh'] += 1
        P.dma('sp', ht[:], h[r0:r0 + 128, :])
        norm_T(C, ht[:], 1024, S, xTs[g % 2][:, :, j * 128:(j + 1) * 128])

    for j in range(4):
        norm_tile(0, j)
    W2 = load_w(C, "w2", 32, 1024, chunk=1024, engs=('dve',))
    NG = TOK // 512
    for g in range(NG):
        xT = xTs[g % 2]
        for f in range(32):
            ps = p1[st['n1'] % 3]
            r = rl[st['n1'] % 3]
            st['n1'] += 1
            for k in range(8):
                P.mm(ps[:], W1.sl(k, f * 128, (f + 1) * 128), xT[:, k, :], start=(k == 0), stop=(k == 7))
            P.act(r[:], ps[:], AF.Relu)
            P.v('pool', 'tensor_mul', h1T[f][:], r[:], r[:], reads=[r], writes=[h1T[f]])
        for j in range(4):
            r0 = g * 512 + j * 128
            ht = hr[st['nh'] % 2]
            st['nh'] += 1
            P.dma('pool', ht[:], h[r0:r0 + 128, :])
            py = pys[j % 2]
            for n in range(2):
                for k in range(32):
                    P.mm(py[:, n * 512:(n + 1) * 512], h1T[k][:, j * 128:(j + 1) * 128],
                         W2.sl(k, n * 512, (n + 1) * 512), start=(k == 0), stop=(k == 31))
            if g + 1 < NG:
                norm_tile(g + 1, j)
            postnorm_res(C, py[:], ht[:], gpo, S, S.junk)
            P.dma('sp', ho[r0:r0 + 128, :], ht[:], W=[("ho", r0)], is_out=True)
    P.emit()
    return C.nc


def build_xa():
    C = Ctx()
    P = C.P
    h = C.din("h", [TOK, 1024])
    mem = C.din("mem", [256, 1024])
    ho = C.dout("ho", [TOK, 1024])
    C.ident()
    gpre = C.small("gpre", [128, 8])
    gmem = C.small("gmem", [128, 8])
    gpo = C.small("gpo", [128, 1024], q='pool')
    C._stages = [P.sb([128, 1024], F32, f"wstage{i}") for i in range(3)]
    Wq = load_w(C, "wq", 8, 1024, g=gpre, chunk=1024)
    Wk = load_w(C, "wk", 8, 1024, g=gmem, chunk=1024)
    Wv = load_w(C, "wv", 8, 1024, g=gmem, chunk=1024)
    Wo = load_w(C, "wo", 8, 1024, chunk=1024)
    S = NormScr(C, 1024, "a")
    S2 = NormScr(C, 1024, "b", pt=S.pt)
    hr = [P.sb([128, 1024], F32, f"hr{j}") for j in range(3)]
    pa = [P.ps([128, 512], F32, f"pa{i}") for i in range(2)]
    po = [P.ps([128, 512], F32, f"po{i}") for i in range(2)]
    py = P.ps([128, 1024], F32, "py")
    memT = P.sb([128, 8, 256], BF16, "memT")
    for m in range(2):
        mt = hr[m]
        P.dma('sp', mt[:], mem[m * 128:(m + 1) * 128, :])
        norm_T(C, mt[:], 1024, S, memT[:, :, m * 128:(m + 1) * 128])
    kT = [P.sb([128, 256], BF16, f"kT{oc}") for oc in range(8)]
    for oc in range(8):
        ps = pa[oc % 2]
        for k in range(8):
            P.mm(ps[:, :256], Wk.sl(k, oc * 128, (oc + 1) * 128), memT[:, k, :], start=(k == 0), stop=(k == 7))
        P.v('dve', 'tensor_copy', kT[oc][:], ps[:, :256], reads=[ps], writes=[kT[oc]])
    V1 = [P.sb([128, 4, 257], BF16, f"V1_{m}") for m in range(2)]
    for m in range(2):
        P.v('pool', 'memset', V1[m][:], 1.0, writes=[V1[m]])
        for n in range(2):
            ps = pa[n % 2]
            for k in range(8):
                P.mm(ps[:], memT[:, k, m * 128:(m + 1) * 128], Wv.sl(k, n * 512, (n + 1) * 512),
                     start=(k == 0), stop=(k == 7))
            P.v('dve', 'tensor_copy', V1[m][:, 2 * n:2 * n + 2, 0:256],
                ps[:].rearrange("p (h d) -> p h d", h=2), reads=[ps], writes=[V1[m]])
    xTs = [P.sb([128, 8, 512], BF16, f"xT{i}") for i in range(2)]
    qT = [P.sb([128, 512], BF16, f"qT{oc}") for oc in range(8)]
    PT = [[P.sb([128, 512], BF16, f"PT{hh}_{m}") for m in range(2)] for hh in range(4)]
    oa = P.sb([128, 1024], BF16, "oa")
    oaT = P.sb([128, 8, 128], BF16, "oaT")
    rinv = P.sb([128, 4], F32, "rinv")
    st = dict(nh=0, na=0, no=0)

    def norm_tile(g, j):
        r0 = g * 512 + j * 128
        ht = hr[st['nh'] % 3]
        st['nh'] += 1
        P.dma('sp', ht[:], h[r0:r0 + 128, :])
        norm_T(C, ht[:], 1024, S, xTs[g % 2][:, :, j * 128:(j + 1) * 128])

    for j in range(4):
        norm_tile(0, j)
    NG = TOK // 512
    for g in range(NG):
        xT = xTs[g % 2]
        for oc in range(8):
            ps = pa[st['na'] % 2]
            st['na'] += 1
            for k in range(8):
                P.mm(ps[:], Wq.sl(k, oc * 128, (oc + 1) * 128), xT[:, k, :], start=(k == 0), stop=(k == 7))
            if oc % 2 == 0:
                P.act(qT[oc][:], ps[:], AF.Copy)
            else:
                P.v('dve', 'tensor_copy', qT[oc][:], ps[:], reads=[ps], writes=[qT[oc]])
        for hh in range(4):
            for m in range(2):
                ps = pa[st['na'] % 2]
                st['na'] += 1
                for dc in range(2):
                    P.mm(ps[:], kT[2 * hh + dc][:, m * 128:(m + 1) * 128], qT[2 * hh + dc][:],
                         start=(dc == 0), stop=(dc == 1))
                P.act(PT[hh][m][:], ps[:], AF.Exp, scale=1.0 / 16.0)
        for j in range(4):
            r0 = g * 512 + j * 128
            ht = hr[st['nh'] % 3]
            st['nh'] += 1
            P.dma('pool', ht[:], h[r0:r0 + 128, :])
            for hh in range(4):
                ps = po[st['no'] % 2]
                st['no'] += 1
                for m in range(2):
                    P.mm(ps[:, :257], PT[hh][m][:, j * 128:(j + 1) * 128], V1[m][:, hh, :],
                         start=(m == 0), stop=(m == 1))
                P.v('dve', 'reciprocal', rinv[:, hh:hh + 1], ps[:, 256:257], reads=[ps], writes=[rinv])
                P.act(oa[:, hh * 256:(hh + 1) * 256], ps[:, 0:256], AF.Copy, scale=rinv[:, hh:hh + 1])
            for c in range(8):
                P.tr(S.pt[:, c, :], oa[:, c * 128:(c + 1) * 128], C.idb[:])
            P.v('dve', 'tensor_copy', oaT[:], S.pt[:], reads=[S.pt], writes=[oaT])
            for n in range(2):
                for k in range(8):
                    P.mm(py[:, n * 512:(n + 1) * 512], oaT[:, k, :], Wo.sl(k, n * 512, (n + 1) * 512),
                         start=(k == 0), stop=(k == 7))
            if g + 1 < NG:
                norm_tile(g + 1, j)
            postnorm_res(C, py[:], ht[:], gpo, S2, S2.junk)
            P.dma('sp', ho[r0:r0 + 128, :], ht[:], W=[("ho", r0)], is_out=True)
    P.emit()
    return C.nc


TWO_PI = 6.283185307179586
SKIP = set()


def rope_tables(C, pos_i, invf):
    P = C.P
    posf = P.sb([128, NT], F32, "posf")
    P.v('dve', 'tensor_copy', posf[:], pos_i[:], reads=[pos_i], writes=[posf])
    ang = P.sb([128, NT, 128], F32, "ang")
    for j in range(NT):
        P.v('dve', 'tensor_scalar_mul', ang[:, j, :], invf[:], posf[:, j:j + 1], reads=[invf, posf], writes=[ang])
    tabs = []
    for nm, off in (("sin", 0.0), ("cos", 0.25)):
        t = P.sb([128, NT, 128], F32, "t_" + nm)
        ti = P.sb([128, NT, 128], I32, "ti_" + nm)
        P.v('dve', 'tensor_scalar', t[:], ang[:], 1.0 / TWO_PI, off, ALU.mult, ALU.add, reads=[ang], writes=[t])
        P.v('dve', 'tensor_copy', ti[:], t[:], reads=[t], writes=[ti])
        tf = P.sb([128, NT, 128], F32, "tf_" + nm)
        P.v('dve', 'tensor_copy', tf[:], ti[:], reads=[ti], writes=[tf])
        P.v('dve', 'tensor_sub', t[:], t[:], tf[:], reads=[t, tf], writes=[t])
        P.act(tf[:], t[:], AF.Sin, scale=6.28318)
        tabs.append(tf)
    return tabs[0], tabs[1]


def rope_apply(C, x1, x2, cs, sn, o1, o2, tmps, shape):
    P = C.P
    t1, t2, t3, t4 = tmps
    P.v('dve', 'tensor_mul', t1, x1, cs, reads=[x1, cs], writes=[t1])
    P.v('dve', 'tensor_mul', t2, x2, sn, reads=[x2, sn], writes=[t2])
    P.v('dve', 'tensor_mul', t3, x2, cs, reads=[x2, cs], writes=[t3])
    P.v('dve', 'tensor_mul', t4, x1, sn, reads=[x1, sn], writes=[t4])
    P.v('pool', 'tensor_sub', o1, t1, t2, reads=[t1, t2], writes=[o1])
    P.v('pool', 'tensor_add', o2, t3, t4, reads=[t3, t4], writes=[o2])


def build_a_even():
    C = Ctx()
    P = C.P
    h = C.din("h", [TOK, 1024])
    q_o = C.dout("q", [TOK, 768], BF16)
    kv_o = C.dout("kv", [TOK, 1024], BF16)
    kpe_o = C.dout("kpe", [TOK, 32], BF16)
    g_o = C.dout("gqkv", [TOK, 1024], BF16)
    la_o = C.dout("la", [TOK, 256])
    r_o = C.dout("r", [TOK, 512])
    C.ident()
    gpre = C.small("gpre", [128, 8])
    gq = C.small("gq", [128, 2])
    gkv = C.small("gkv", [128, 1])
    wga = C.small("wga", [17, 256])
    pos_i = C.small("pos", [128, NT], I32)
    invf = C.small("invf", [128, 128])
    C._stages = [P.sb([128, 1968], F32, f"wstage{i}") for i in range(2)]
    Win = load_w(C, "w_in", 8, 1968, g=gpre, chunk=1968)
    Wuq = load_w(C, "w_uq", 2, 768, g=gq, chunk=768)
    Wukv = load_w(C, "w_ukv", 1, 1024, g=gkv, chunk=1024)
    sin_t, cos_t = rope_tables(C, pos_i, invf)
    S = NormScr(C, 1024, "a")
    S2 = NormScr(C, 256, "b", pt=S.pt)
    hr = [P.sb([128, 1024], F32, f"hr{j}") for j in range(2)]
    xTt = [P.sb([128, 8, 128], BF16, f"xTt{j}") for j in range(2)]
    pp = [P.ps([128, 512], F32, f"pp{i}") for i in range(4)]
    pg = P.ps([128, 512], F32, "pg")
    pjs = [P.sb([128, 1968], F32, f"pj{i}") for i in range(2)]
    cqT = P.sb([128, 2, 128], BF16, "cqT")
    ckvT = P.sb([128, 128], BF16, "ckvT")
    aug = P.sb([32, 128], F32, "aug")
    P.v('dve', 'memset', aug[:], 1.0, writes=[aug])
    qos = [P.sb([128, 8, 96], BF16, f"qo{i}") for i in range(2)]
    kvos = [P.sb([128, 1024], BF16, f"kvo{i}") for i in range(2)]
    kpos = [P.sb([128, 32], BF16, f"kpo{i}") for i in range(2)]
    gbs = [P.sb([128, 1024], BF16, f"gb{i}") for i in range(2)]
    las = [P.sb([128, 256], F32, f"la{i}") for i in range(2)]
    ex = P.sb([128, 256], F32, "ex")
    tq = [P.sb([128, 4, 16], F32, f"tq{i}") for i in range(4)]
    tk = [P.sb([128, 16], F32, f"tk{i}") for i in range(4)]
    pq = [P.ps([128, 512], F32, f"pq{i}") for i in range(2)]

    def st1(j):
        r0 = j * 128
        ht = hr[j % 2]
        xT = xTt[j % 2]
        pj = pjs[j % 2]
        P.dma('sp', ht[:], h[r0:r0 + 128, :])
        norm_T(C, ht[:], 1024, S, xT[:])
        for n in range(4):
            w = min(512, 1968 - n * 512)
            for k in range(8):
                P.mm(pp[n][:, :w], xT[:, k, :], Win.sl(k, n * 512, n * 512 + w), start=(k == 0), stop=(k == 7))
        for n in range(4):
            w = min(512, 1968 - n * 512)
            if n % 2 == 0:
                P.act(pj[:, n * 512:n * 512 + w], pp[n][:, :w], AF.Copy)
            else:
                P.v('dve', 'tensor_copy', pj[:, n * 512:n * 512 + w], pp[n][:, :w], reads=[pp[n]], writes=[pj])

    def st2(j):
        r0 = j * 128
        pj = pjs[j % 2]
        P.dma('pool', r_o[r0:r0 + 128, :], pj[:, 1456:1968], W=[("r_o", j)], is_out=True)
        gb = gbs[j % 2]
        P.v('pool', 'tensor_copy', gb[:], pj[:, 416:1440], reads=[pj], writes=[gb])
        P.dma('pool', g_o[r0:r0 + 128, :], gb[:], W=[("g_o", j)], is_out=True)
        rstd_of(C, pj[:, 0:256], 256, S2, S2.ss[:], S2.rstd[:], S2.junk[:, :256])
        P.act(S2.xb[:, :256], pj[:, 0:256], AF.Copy, scale=S2.rstd[:])
        for c in range(2):
            P.tr(S.pt[:, c, :], S2.xb[:, c * 128:(c + 1) * 128], C.idb[:])
        P.v('dve', 'tensor_copy', cqT[:], S.pt[:, 0:2, :], reads=[S.pt], writes=[cqT])
        for hf in range(2):
            for kc in range(2):
                P.mm(pq[hf][:, :384], cqT[:, kc, :], Wuq.sl(kc, hf * 384, (hf + 1) * 384),
                     start=(kc == 0), stop=(kc == 1))
        qo = qos[j % 2]
        cs4 = cos_t[:, j, 0:64].rearrange("p (h f) -> p h f", h=4)
        sn4 = sin_t[:, j, 0:64].rearrange("p (h f) -> p h f", h=4)
        for hf in range(2):
            pqv = pq[hf][:, :384].rearrange("p (h d) -> p h d", h=4)
            P.act(qo[:, 4 * hf:4 * hf + 4, 0:64], pqv[:, :, 0:64], AF.Copy)
            rope_apply(C, pqv[:, :, 64:80], pqv[:, :, 80:96], cs4, sn4,
                       qo[:, 4 * hf:4 * hf + 4, 64:80], qo[:, 4 * hf:4 * hf + 4, 80:96],
                       [t[:] for t in tq], None)
        P.dma('sp', q_o[r0:r0 + 128, :], qo[:].rearrange("p h d -> p (h d)"), W=[("q_o", j)], is_out=True)
        rstd_of(C, pj[:, 256:384], 128, S2, S2.ss[:], S2.rstd[:], S2.junk[:, :128])
        P.act(S2.xb[:, :128], pj[:, 256:384], AF.Copy, scale=S2.rstd[:])
        P.tr(S.pt[:, 0, :], S2.xb[:, 0:128], C.idb[:])
        P.v('dve', 'tensor_copy', ckvT[:], S.pt[:, 0, :], reads=[S.pt], writes=[ckvT])
        for n in range(2):
            P.mm(pq[n][:], ckvT[:], Wukv.sl(0, n * 512, (n + 1) * 512), start=True, stop=True)
        kvo = kvos[j % 2]
        P.act(kvo[:, 0:512], pq[0][:], AF.Copy)
        P.v('dve', 'tensor_copy', kvo[:, 512:1024], pq[1][:], reads=[pq[1]], writes=[kvo])
        P.dma('sp', kv_o[r0:r0 + 128, :], kvo[:], W=[("kv_o", j)], is_out=True)
        kpo = kpos[j % 2]
        rope_apply(C, pj[:, 384:400], pj[:, 400:416], cos_t[:, j, 0:16], sin_t[:, j, 0:16],
                   kpo[:, 0:16], kpo[:, 16:32], [t[:] for t in tk], None)
        P.dma('sp', kpe_o[r0:r0 + 128, :], kpo[:], W=[("kpe_o", j)], is_out=True)
        P.tr(pg[0:16, 256:384], pj[:, 1440:1456], C.idf[:], W=[("pg", "t")])
        P.v('dve', 'tensor_copy', aug[0:16, :], pg[0:16, 256:384], reads=[("pg", "t")], writes=[aug])
        P.mm(pg[:, 0:256], aug[0:17, :], wga[0:17, :], start=True, stop=True, W=[("pg", "m")])
        P.act(ex[:], pg[:, 0:256], AF.Exp, scale=-1.0, R=[("pg", "m")])
        P.act(ex[:], ex[:], AF.Ln, bias=1.0)
        la = las[j % 2]
        P.v('dve', 'tensor_scalar_mul', la[:], ex[:], -1.0 / 16.0, reads=[ex], writes=[la])
        P.dma('sp', la_o[r0:r0 + 128, :], la[:], W=[("la_o", j)], is_out=True)

    st1(0)
    for j in range(NT):
        if j + 1 < NT:
            st1(j + 1)
        st2(j)
    P.emit()
    return C.nc


SEQ = 16384
NCH = SEQ // 128


def build_m_even():
    C = Ctx()
    P = C.P
    qT_d = C.din("qT", [96, SEQ], BF16)
    kT_d = C.din("kT", [96, SEQ], BF16)
    v_d = C.din("v", [128, NCH * 64], BF16)
    tri_d = C.din("trib", [128, 128], BF16)
    o_d = C.dout("o", [128, NCH * 64])
    gqT_d = C.din("gqT", [64, SEQ], BF16)
    gkT_d = C.din("gkT", [64, SEQ], BF16)
    gk_d = C.din("gk", [128, NCH * 64], BF16)
    gv_d = C.din("gv", [128, NCH * 64], BF16)
    la_d = C.din("la", [128, NCH * 64])
    go_d = C.dout("go", [128, NCH * 64])
    triu = C.small("triu", [128, 128])
    tril = C.small("tril", [128, 128])
    QT = P.sb([96, SEQ], BF16, "QT")
    KT = P.sb([96, SEQ], BF16, "KT")
    V1 = P.sb([128, NCH, 65], BF16, "V1")
    trib = P.sb([128, 128], BF16, "trib_sb")
    P.dma('sp', trib[:], tri_d)
    nq = max(1, SEQ // 4096)
    cq = SEQ // nq
    for i in range(nq):
        P.dma('sp', QT[:, i * cq:(i + 1) * cq], qT_d[:, i * cq:(i + 1) * cq])
        P.dma('pool', KT[:, i * cq:(i + 1) * cq], kT_d[:, i * cq:(i + 1) * cq])
    P.v('dve', 'memset', V1[:], 1.0, writes=[V1])
    P.dma('sp', V1[:, :, 0:64], v_d.rearrange("p (t d) -> p t d", d=64))
    pS = [P.ps([128, 512], F32, f"pS{i}") for i in range(3)]
    pO = [P.ps([128, 512], F32, f"pO{i}") for i in range(4)]
    PTs = [P.sb([128, 512], BF16, f"PT{i}") for i in range(3)]
    obs = [P.sb([128, 4, 64], F32, f"ob{i}") for i in range(2)]
    rinv = P.sb([128, 4], F32, "rinv")
    scale = 96.0 ** -0.5
    NR = 3
    blocks = [(qb, kt) for qb in range(0 if 'mla' not in SKIP else SEQ, SEQ // 512) for kt in range(4 * qb + 4)]

    def stage_s(i):
        qb, kt = blocks[i]
        ps, pt = pS[i % NR], PTs[i % NR]
        c0 = max(0, kt - 4 * qb) * 128
        P.mm(ps[:, c0:512], KT[:, kt * 128:(kt + 1) * 128], QT[:, qb * 512 + c0:(qb + 1) * 512],
             start=True, stop=True)
        P.act(pt[:, c0:512], ps[:, c0:512], AF.Exp, scale=scale)
        if kt >= 4 * qb:
            P.v('pool', 'tensor_mul', pt[:, c0:c0 + 128], pt[:, c0:c0 + 128], trib[:],
                reads=[pt, trib], writes=[pt])

    def stage_pv(i):
        qb, kt = blocks[i]
        pt = PTs[i % NR]
        jd = max(0, kt - 4 * qb)
        for j in range(jd, 4):
            P.mm(pO[j][:, 0:65], pt[:, j * 128:(j + 1) * 128], V1[:, kt, :],
                 start=(kt == 0), stop=(kt == 4 * qb + j))
        if kt == 4 * qb + 3:
            ob = obs[qb % 2]
            for j in range(4):
                P.v('dve', 'reciprocal', rinv[:, j:j + 1], pO[j][:, 64:65], reads=[pO[j]], writes=[rinv])
                P.act(ob[:, j, :], pO[j][:, 0:64], AF.Copy, scale=rinv[:, j:j + 1])
            P.dma('sp', o_d[:, qb * 256:(qb + 1) * 256], ob[:].rearrange("p j d -> p (j d)"),
                  W=[("o_d", qb)], is_out=True)

    GC = 8
    S = P.sb([64, 64], F32, "gS")
    Sb = P.sb([64, 64], BF16, "gSb")
    P.v('dve', 'memset', S[:], 0.0, writes=[S])
    P.v('dve', 'memset', Sb[:], 0.0, writes=[Sb])
    bufs = []
    for i in range(2):
        bufs.append(dict(q=P.sb([64, GC * 128], BF16, f"gq{i}"), k=P.sb([64, GC * 128], BF16, f"gkT{i}"),
                         kt=P.sb([128, GC * 64], BF16, f"gk{i}"), v=P.sb([128, GC * 64], BF16, f"gv{i}"),
                         la=P.sb([128, GC * 64], F32, f"gla{i}"), o=P.sb([128, GC * 64], F32, f"go{i}")))
    ND = 3
    eb = [P.sb([64, 128], F32, f"eb{i}") for i in range(ND)]
    enb = [P.sb([64, 128], F32, f"enb{i}") for i in range(ND)]
    ebr = [P.sb([128, 64], F32, f"ebr{i}") for i in range(ND)]
    qs = [P.sb([64, 128], BF16, f"qs{i}") for i in range(ND)]
    ks = [P.sb([64, 128], BF16, f"ks{i}") for i in range(ND)]
    kh = [P.sb([128, 64], BF16, f"kh{i}") for i in range(ND)]
    AT = [P.sb([128, 128], BF16, f"AT{i}") for i in range(ND)]
    pG = P.ps([128, 512], F32, "pG")
    rA, rB, rC, rD, rE = pG[0:64, 0:128], pG[:, 128:192], pG[:, 192:320], pG[:, 320:384], pG[0:64, 384:448]
    gstages = []

    def gla_chunk(g, c):
        B = bufs[g % 2]
        i = (g * GC + c) % ND
        la_c = B['la'][:, c * 64:(c + 1) * 64]
        v_c = B['v'][:, c * 64:(c + 1) * 64]

        def s0():
            if c == 0:
                P.dma('sp', B['q'][:], gqT_d[:, g * GC * 128:(g + 1) * GC * 128])
                P.dma('sp', B['k'][:], gkT_d[:, g * GC * 128:(g + 1) * GC * 128])
                P.dma('pool', B['kt'][:], gk_d[:, g * GC * 64:(g + 1) * GC * 64])
                P.dma('pool', B['v'][:], gv_d[:, g * GC * 64:(g + 1) * GC * 64])
                P.dma('sp', B['la'][:], la_d[:, g * GC * 64:(g + 1) * GC * 64])

        def s1():
            P.mm(rA, la_c, triu[:], start=True, stop=True)
            P.mm(rB, tril[:], la_c, start=True, stop=True)

        def s2():
            P.act(eb[i][:], rA, AF.Exp)
            P.act(enb[i][:], rA, AF.Exp, scale=-1.0)
            P.act(ebr[i][:], rB, AF.Exp)

        def s3():
            P.v('dve', 'scalar_tensor_tensor', qs[i][:], B['q'][:, c * 128:(c + 1) * 128], 0.125, eb[i][:],
                ALU.mult, ALU.mult, reads=[B['q'], eb[i]], writes=[qs[i]])
            P.v('pool', 'tensor_mul', ks[i][:], B['k'][:, c * 128:(c + 1) * 128], enb[i][:],
                reads=[B['k'], enb[i]], writes=[ks[i]])
            P.v('pool', 'tensor_mul', kh[i][:], B['kt'][:, c * 64:(c + 1) * 64], ebr[i][:],
                reads=[B['kt'], ebr[i]], writes=[kh[i]])

        def s4():
            P.mm(rC, ks[i][:], qs[i][:], start=True, stop=True)

        def s5():
            P.v('dve', 'tensor_mul', AT[i][:], rC, triu[:], reads=[pG, triu], writes=[AT[i]])

        def s6():
            P.mm(rD, AT[i][:], v_c, start=True, stop=False)
            P.mm(rD, qs[i][:], Sb[:], start=False, stop=True)
            P.mm(rE, kh[i][:], v_c, start=True, stop=True)

        def s7():
            P.act(B['o'][:, c * 64:(c + 1) * 64], rD, AF.Copy)
            P.v('dve', 'scalar_tensor_tensor', S[:], S[:], eb[i][:, 127:128], rE,
                ALU.mult, ALU.add, reads=[S, eb[i], pG], writes=[S])
            P.v('dve', 'tensor_copy', Sb[:], S[:], reads=[S], writes=[Sb])
            if c == GC - 1:
                P.dma('sp', go_d[:, g * GC * 64:(g + 1) * GC * 64], B['o'][:], W=[("go_d", g)], is_out=True)
        return [s0, s1, s2, s3, s4, s5, s6, s7]

    if 'gla' not in SKIP:
        for g in range(NCH // GC):
            for c in range(GC):
                gstages.extend(gla_chunk(g, c))
    LA = 2
    nb = len(blocks)
    gi_ = 0
    for i in range(nb + LA):
        if i < nb:
            stage_s(i)
        if i - LA >= 0:
            stage_pv(i - LA)
        want = len(gstages) if nb == 0 else min(len(gstages), (max(0, i - 4) * len(gstages)) // max(1, nb - 8) + 0)
        while gi_ < want:
            gstages[gi_]()
            gi_ += 1
    while gi_ < len(gstages):
        gstages[gi_]()
        gi_ += 1
    P.emit()
    return C.nc


def build_cmix_even():
    C = Ctx()
    P = C.P
    h = C.din("h", [TOK, 1024])
    a_d = C.din("a", [TOK, 512])
    go_d = C.din("go", [TOK, 512])
    r_d = C.din("r", [TOK, 512])
    ho = C.dout("ho", [TOK, 1024])
    C.ident()
    ggla = C.small("ggla", [128, 128])
    gpo = C.small("gpo", [128, 1024], q='pool')
    C._stages = [P.sb([128, 1024], F32, f"wstage{i}") for i in range(3)]
    Wout = load_w(C, "w_out", 8, 1024, chunk=1024)
    S = NormScr(C, 1024, "a")
    S2 = NormScr(C, 1024, "b", pt=S.pt)
    pys = [P.ps([128, 1024], F32, f"py{i}") for i in range(2)]
    bufs = []
    for i in range(3):
        bufs.append(dict(h=P.sb([128, 1024], F32, f"ht{i}"), a=P.sb([128, 512], F32, f"at{i}"),
                         g=P.sb([128, 512], F32, f"got{i}"), r=P.sb([128, 512], F32, f"rt{i}")))
    cat = P.sb([128, 1024], BF16, "cat")
    catTs = [P.sb([128, 8, 128], BF16, f"catT{i}") for i in range(2)]
    ss4 = P.sb([128, 4], F32, "ss4")
    rstd4 = P.sb([128, 4], F32, "rstd4")
    sr = P.sb([128, 512], F32, "sr")
    gn = P.sb([128, 512], F32, "gn")

    def st_a(j):
        r0 = j * 128
        B = bufs[j % 3]
        P.dma('sp', B['h'][:], h[r0:r0 + 128, :])
        P.dma('pool', B['a'][:], a_d[r0:r0 + 128, :])
        P.dma('sp', B['g'][:], go_d[r0:r0 + 128, :])
        P.dma('pool', B['r'][:], r_d[r0:r0 + 128, :])
        P.v('pool', 'tensor_copy', cat[:, 0:512], B['a'][:], reads=[B['a']], writes=[cat])
        for hh in range(4):
            P.act(S.junk[:, :128], B['g'][:, hh * 128:(hh + 1) * 128], AF.Square, scale=128.0 ** -0.5,
                  accum_out=ss4[:, hh:hh + 1])
        P.act(rstd4[:], ss4[:], AF.Sqrt, bias=EPS)
        P.v('dve', 'reciprocal', rstd4[:], rstd4[:], reads=[rstd4], writes=[rstd4])
        P.act(sr[:], B['r'][:], AF.Silu)
        for hh in range(4):
            P.v('dve', 'scalar_tensor_tensor', gn[:, hh * 128:(hh + 1) * 128], B['g'][:, hh * 128:(hh + 1) * 128],
                rstd4[:, hh:hh + 1], ggla[:], ALU.mult, ALU.mult, reads=[B['g'], rstd4, ggla], writes=[gn])
        P.v('pool', 'tensor_mul', cat[:, 512:1024], gn[:], sr[:], reads=[gn, sr], writes=[cat])
        for c in range(8):
            P.tr(S.pt[:, c, :], cat[:, c * 128:(c + 1) * 128], C.idb[:])
        P.v('dve', 'tensor_copy', catTs[j % 2][:], S.pt[:], reads=[S.pt], writes=[catTs[j % 2]])

    def st_b(j):
        py = pys[j % 2]
        for n in range(2):
            for k in range(8):
                P.mm(py[:, n * 512:(n + 1) * 512], catTs[j % 2][:, k, :], Wout.sl(k, n * 512, (n + 1) * 512),
                     start=(k == 0), stop=(k == 7))

    def st_c(j):
        r0 = j * 128
        B = bufs[j % 3]
        postnorm_res(C, pys[j % 2][:], B['h'][:], gpo, S2, S2.junk)
        P.dma('sp', ho[r0:r0 + 128, :], B['h'][:], W=[("ho", r0)], is_out=True)

    for t in range(NT + 2):
        if t < NT:
            st_a(t)
        if 0 <= t - 1 < NT:
            st_b(t - 1)
        if 0 <= t - 2 < NT:
            st_c(t - 2)
    P.emit()
    return C.nc


def build_a_odd():
    C = Ctx()
    P = C.P
    h = C.din("h", [TOK, 1024])
    hh_d = C.din("hh", [128, 1024])
    z_o = C.dout("z", [TOK, 1024])
    xc_o = C.dout("xcT", [1024, TOK])
    q_o = C.dout("qT", [1024, TOK], BF16)
    k_o = C.dout("kT", [1024, TOK], BF16)
    v_o = C.dout("vT", [1024, TOK], BF16)
    gi_o = C.dout("gi", [8, TOK])
    gl_o = C.dout("gl", [8, TOK])
    C.ident()
    gpre = C.small("gpre", [128, 8])
    cw = C.small("convw", [128, 32])
    cb = C.small("convb", [128, 8])
    bg = C.small("bg", [8, 1])
    C._stages = [P.sb([128, 2048], F32, f"wstage{i}") for i in range(2)]
    Win = load_w(C, "w_in", 8, 2048, g=gpre, chunk=2048)
    Wq = load_w(C, "wq", 8, 256, chunk=256)
    Wk = load_w(C, "wk", 8, 256, chunk=256)
    Wv = load_w(C, "wv", 8, 256, chunk=256)
    Wg = load_w(C, "wg", 24, 8, chunk=8)
    S = NormScr(C, 1024, "a")
    hr = [P.sb([128, 1024], F32, f"hr{j}") for j in range(2)]
    xTs = [P.sb([128, 8, 512], BF16, f"xT{i}") for i in range(2)]
    xm = [P.sb([128, 515], F32, f"xm{cc}") for cc in range(8)]
    xmb = [P.sb([128, 512], BF16, f"xmb{cc}") for cc in range(8)]
    xcb = [P.sb([128, 512], BF16, f"xcb{cc}") for cc in range(8)]
    qkvb = [[P.sb([128, 512], BF16, f"qkv{t}_{i}") for i in range(8)] for t in range(3)]
    acc = [P.sb([128, 512], F32, f"acc{i}") for i in range(2)]
    xcf = [P.sb([128, 512], F32, f"xcf{i}") for i in range(2)]
    zt = [P.sb([128, 1024], F32, f"zt{i}") for i in range(2)]
    gi = P.sb([8, 512], F32, "gi_sb")
    gl = P.sb([8, 512], F32, "gl_sb")
    pr = [P.ps([128, 512], F32, f"pr{i}") for i in range(3)]
    pz = P.ps([128, 1024], F32, "pz")
    pgt = P.ps([128, 512], F32, "pgt")
    xT = xTs[1]
    P.dma('sp', hr[0][:], hh_d)
    norm_T(C, hr[0][:], 1024, S, xT[:, :, 0:128])
    npr = 0
    for cc in range(8):
        ps = pr[npr % 3]
        npr += 1
        for k in range(8):
            P.mm(ps[:, 0:128], Win.sl(k, cc * 128, (cc + 1) * 128), xT[:, k, 0:128], start=(k == 0), stop=(k == 7))
        P.act(xm[cc][:, 0:3], ps[:, 125:128], AF.Copy)
    st = dict(nh=1)

    def norm_tile(g, j):
        ht = hr[st['nh'] % 2]
        st['nh'] += 1
        P.dma('sp', ht[:], h[g * 512 + j * 128:g * 512 + (j + 1) * 128, :])
        norm_T(C, ht[:], 1024, S, xTs[g % 2][:, :, j * 128:(j + 1) * 128])

    for j in range(4):
        norm_tile(0, j)
    NG = TOK // 512
    for g in range(NG):
        c0 = g * 512
        xT = xTs[g % 2]
        for cc in range(8):
            ps = pr[npr % 3]
            npr += 1
            for k in range(8):
                P.mm(ps[:], Win.sl(k, cc * 128, (cc + 1) * 128), xT[:, k, :], start=(k == 0), stop=(k == 7))
            P.act(xm[cc][:, 3:515], ps[:], AF.Copy)
            P.v('dve', 'tensor_copy', xmb[cc][:], xm[cc][:, 3:515], reads=[xm[cc]], writes=[xmb[cc]])
            a = acc[cc % 2]
            P.v('dve', 'tensor_scalar', a[:], xm[cc][:, 0:512], cw[:, cc * 4:cc * 4 + 1], cb[:, cc:cc + 1],
                ALU.mult, ALU.add, reads=[xm[cc], cw, cb], writes=[a])
            for i in range(1, 4):
                P.v('dve', 'scalar_tensor_tensor', a[:], xm[cc][:, i:i + 512],
                    cw[:, cc * 4 + i:cc * 4 + i + 1], a[:], ALU.mult, ALU.add, reads=[xm[cc], cw, a], writes=[a])
            xf = xcf[cc % 2]
            P.act(xf[:], a[:], AF.Silu)
            P.dma('pool', xc_o[cc * 128:(cc + 1) * 128, c0:c0 + 512], xf[:], W=[("xc_o", g, cc)], is_out=True)
            P.v('pool', 'tensor_copy', xcb[cc][:], xf[:], reads=[xf], writes=[xcb[cc]])
            P.v('pool', 'tensor_copy', xm[cc][:, 0:3], xm[cc][:, 512:515], reads=[xm[cc]], writes=[xm[cc]])
        for j in range(4):
            z = zt[j % 2]
            for n in range(2):
                for k in range(8):
                    P.mm(pz[:, n * 512:(n + 1) * 512], xT[:, k, j * 128:(j + 1) * 128],
                         Win.sl(k, 1024 + n * 512, 1024 + (n + 1) * 512), start=(k == 0), stop=(k == 7))
            P.act(z[:, 0:512], pz[:, 0:512], AF.Copy)
            P.v('dve', 'tensor_copy', z[:, 512:1024], pz[:, 512:1024], reads=[pz], writes=[z])
            P.dma('sp', z_o[c0 + j * 128:c0 + (j + 1) * 128, :], z[:], W=[("z_o", g, j)], is_out=True)
        nq = 0
        for t, (W_, src, dst) in enumerate(((Wq, xcb, q_o), (Wk, xcb, k_o), (Wv, xmb, v_o))):
            for hd in range(4):
                for ec in range(2):
                    if g + 1 < NG and nq % 6 == 0:
                        norm_tile(g + 1, nq // 6)
                    nq += 1
                    ps = pr[npr % 3]
                    npr += 1
                    for dc in range(2):
                        P.mm(ps[:], W_.sl(hd * 2 + dc, ec * 128, (ec + 1) * 128), src[hd * 2 + dc][:],
                             start=(dc == 0), stop=(dc == 1))
                    ob = qkvb[t][hd * 2 + ec]
                    if (hd * 2 + ec) % 2 == 0:
                        P.act(ob[:], ps[:], AF.Copy)
                    else:
                        P.v('dve', 'tensor_copy', ob[:], ps[:], reads=[ps], writes=[ob])
                    r0 = hd * 256 + ec * 128
                    P.dma('sp' if t != 1 else 'pool', dst[r0:r0 + 128, c0:c0 + 512], ob[:],
                          W=[("qkv_o", t, g, hd, ec)], is_out=True)
        n = 0
        for hd in range(4):
            for t in range(3):
                for ec in range(2):
                    P.mm(pgt[0:8, :], Wg.sl(hd * 6 + t * 2 + ec, 0, 8), qkvb[t][hd * 2 + ec][:],
                         start=(n == 0), stop=(n == 23))
                    n += 1
        P.act(gi[:], pgt[0:8, :], AF.Identity, bias=bg[:, 0:1])
        P.act(gl[:], gi[:], AF.Exp, scale=-1.0)
        P.act(gl[:], gl[:], AF.Ln, bias=1.0)
        P.v('dve', 'tensor_scalar_mul', gl[:], gl[:], -1.0, reads=[gl], writes=[gl])
        P.dma('sp', gi_o[:, c0:c0 + 512], gi[:], W=[("gi_o", g)], is_out=True)
        P.dma('sp', gl_o[:, c0:c0 + 512], gl[:], W=[("gl_o", g)], is_out=True)
    P.emit()
    return C.nc


def build_m_odd():
    C = Ctx()
    P = C.P
    nch = NCH
    qT_d = C.din("qT", [256, SEQ], BF16)
    kT_d = C.din("kT", [256, SEQ], BF16)
    kt_d = C.din("ktok", [128, nch * 256], BF16)
    v_d = C.din("v", [128, nch * 128], BF16)
    ho_d = C.dout("ho", [128, nch * 128])
    C.ident()
    idf = C.idf
    ii = C.small("ii", [128, nch])
    lf = C.small("lf", [128, nch])
    triu = C.small("triu", [128, 128])
    ones = P.sb([128, 128], F32, "ones")
    P.v('dve', 'memset', ones[:], 1.0, writes=[ones])
    pX = P.ps([128, 512], F32, "pX")
    pN = P.ps([128, 512], F32, "pN")
    pQ = P.ps([128, 512], F32, "pQ")
    pI = P.ps([128, 512], F32, "pI")
    pR = P.ps([128, 512], F32, "pR")
    pU = P.ps([128, 512], F32, "pU")

    def sbt(name, shape=None, dt=F32):
        return P.sb(shape or [128, nch], dt, name)

    def dv(name, *a, reads, writes, eng='dve'):
        P.v(eng, name, *a, reads=reads, writes=writes)

    b_all, u_all, c_all = sbt("b_all"), sbt("u_all"), sbt("c_all")
    P.mm(pX[:, 0:nch], triu[:], lf[:], start=True, stop=True)
    dv('tensor_copy', b_all[:], pX[:, 0:nch], reads=[pX], writes=[b_all])
    dv('tensor_sub', u_all[:], ii[:], b_all[:], reads=[ii, b_all], writes=[u_all])

    def prefix_max(src, dst, rows, n):
        sh = 1
        while sh < n:
            dv('tensor_copy', dst[0:rows, 0:sh], src[0:rows, 0:sh], reads=[src], writes=[dst])
            dv('tensor_max', dst[0:rows, sh:n], src[0:rows, sh:n], src[0:rows, 0:n - sh], reads=[src], writes=[dst])
            src, dst = dst, src
            sh *= 2
        return src

    x0, x1 = sbt("x0", [128, 128]), sbt("x1", [128, 128])
    P.tr(pX[0:nch, 0:128], u_all[:], idf[:])
    dv('tensor_copy', x0[0:nch, :], pX[0:nch, 0:128], reads=[pX], writes=[x0])
    cT = prefix_max(x0, x1, nch, 128)
    P.tr(pX[:, 0:nch], cT[0:nch, :], idf[0:nch, 0:nch])
    dv('tensor_copy', c_all[:], pX[:, 0:nch], reads=[pX], writes=[c_all])
    bT = sbt("bT", [128, 128])
    P.tr(pX[0:nch, 0:128], b_all[:], idf[:])
    dv('tensor_copy', bT[0:nch, :], pX[0:nch, 0:128], reads=[pX], writes=[bT])
    PB = sbt("PB", [128, 1])
    P.mm(pX[0:nch, 0:1], triu[0:nch, 0:nch], bT[0:nch, 127:128], start=True, stop=True)
    dv('tensor_copy', PB[0:nch, :], pX[0:nch, 0:1], reads=[pX], writes=[PB])
    wcol = sbt("wcol", [128, 1])
    dv('tensor_sub', wcol[0:nch, :], cT[0:nch, 127:128], PB[0:nch, :], reads=[cT, PB], writes=[wcol])
    dv('tensor_add', wcol[0:nch, :], wcol[0:nch, :], bT[0:nch, 127:128], reads=[wcol, bT], writes=[wcol])
    r0, r1 = sbt("r0", [1, 128]), sbt("r1", [1, 128])
    pbrow, brow, mrow, mprow = sbt("pbrow", [1, 128]), sbt("brow", [1, 128]), sbt("mrow", [1, 128]), sbt("mprow", [1, 128])
    for col, dst in ((wcol[0:nch, 0:1], r0), (PB[0:nch, 0:1], pbrow), (bT[0:nch, 127:128], brow)):
        P.tr(pX[0:1, 0:nch], col, idf[0:nch, 0:nch])
        dv('tensor_copy', dst[0:1, 0:nch], pX[0:1, 0:nch], reads=[pX], writes=[dst])
    pm = prefix_max(r0, r1, 1, nch)
    dv('tensor_scalar_max', mrow[0:1, 0:nch], pm[0:1, 0:nch], 0.0, reads=[pm], writes=[mrow])
    dv('tensor_add', mrow[0:1, 0:nch], mrow[0:1, 0:nch], pbrow[0:1, 0:nch], reads=[mrow, pbrow], writes=[mrow])
    dv('memset', mprow[:], 0.0, reads=[], writes=[mprow])
    if nch > 1:
        dv('tensor_copy', mprow[0:1, 1:nch], mrow[0:1, 0:nch - 1], reads=[mrow], writes=[mprow])
    mp_all, mn_all, be_all = sbt("mp_all"), sbt("mn_all"), sbt("be_all")
    for row, dst in ((mprow, mp_all), (mrow, mn_all), (brow, be_all)):
        P.mm(pX[:, 0:nch], ones[0:1, :], row[0:1, 0:nch], start=True, stop=True)
        dv('tensor_copy', dst[:], pX[:, 0:nch], reads=[pX], writes=[dst])
    mx_all, m_all, nmx_all, wi_all, em_all = sbt("mx_all"), sbt("m_all"), sbt("nmx_all"), sbt("wi_all"), sbt("em_all")
    d_all, ws_all, dec_all = sbt("d_all"), sbt("ws_all"), sbt("dec_all")
    dv('tensor_max', mx_all[:], c_all[:], mp_all[:], reads=[c_all, mp_all], writes=[mx_all])
    dv('tensor_add', m_all[:], b_all[:], mx_all[:], reads=[b_all, mx_all], writes=[m_all])
    dv('tensor_scalar_mul', nmx_all[:], mx_all[:], -1.0, reads=[mx_all], writes=[nmx_all])
    dv('tensor_sub', wi_all[:], mp_all[:], mx_all[:], reads=[mp_all, mx_all], writes=[wi_all])
    P.act(wi_all[:], wi_all[:], AF.Exp)
    dv('tensor_scalar_mul', wi_all[:], wi_all[:], 0.0625, reads=[wi_all], writes=[wi_all])
    P.act(em_all[:], m_all[:], AF.Exp, scale=-1.0)
    dv('tensor_sub', d_all[:], be_all[:], mn_all[:], reads=[be_all, mn_all], writes=[d_all])
    dv('tensor_add', ws_all[:], u_all[:], d_all[:], reads=[u_all, d_all], writes=[ws_all])
    P.act(ws_all[:], ws_all[:], AF.Exp)
    dv('tensor_add', dec_all[:], mp_all[:], d_all[:], reads=[mp_all, d_all], writes=[dec_all])
    P.act(dec_all[:], dec_all[:], AF.Exp)
    GC = min(8, nch)
    Cn = [P.sb([128, 129], F32, f"Cn{dc}") for dc in range(2)]
    Cnb = [P.sb([128, 129], BF16, f"Cnb{dc}") for dc in range(2)]
    for dc in range(2):
        dv('memset', Cn[dc][:], 0.0, reads=[], writes=[Cn[dc]])
        dv('memset', Cnb[dc][:], 0.0, reads=[], writes=[Cnb[dc]])
    bufs = []
    for i in range(2):
        B = dict(q=P.sb([128, 2, GC * 128], BF16, f"qg{i}"), k=P.sb([128, 2, GC * 128], BF16, f"kg{i}"),
                 kt=P.sb([128, GC * 256], BF16, f"ktg{i}"), v1=P.sb([128, GC, 129], BF16, f"v1g{i}"),
                 ho=P.sb([128, GC, 128], F32, f"hog{i}"))
        dv('memset', B['v1'][:], 1.0, reads=[], writes=[B['v1']], eng='pool')
        bufs.append(B)
    dN = [sbt(f"dN{i}", [128, 128]) for i in range(2)]
    E = [sbt(f"E{i}", [128, 128]) for i in range(2)]
    WT = [sbt(f"WT{i}", [128, 128], BF16) for i in range(2)]
    isb = [sbt(f"isb{i}", [128, 129]) for i in range(2)]
    tot = [sbt(f"tot{i}", [128, 129]) for i in range(2)]
    den = [sbt(f"den{i}", [128, 1]) for i in range(2)]
    kw = [sbt(f"kw{i}", [128, 256], BF16) for i in range(2)]
    pU2 = P.ps([128, 512], F32, "pU2")
    pUs = [pU, pU2]

    def part_a(j):
        g, c = divmod(j, GC)
        B = bufs[g % 2]
        i = j % 2
        sl = slice(c * 128, (c + 1) * 128)
        if c == 0:
            t0 = g * GC * 128
            for dc in range(2):
                P.dma('sp', B['q'][:, dc, :], qT_d[dc * 128:(dc + 1) * 128, t0:t0 + GC * 128])
                P.dma('pool', B['k'][:, dc, :], kT_d[dc * 128:(dc + 1) * 128, t0:t0 + GC * 128])
            P.dma('sp', B['kt'][:], kt_d[:, g * GC * 256:(g + 1) * GC * 256])
            P.dma('pool', B['v1'][:, :, 0:128],
                  v_d[:, g * GC * 128:(g + 1) * GC * 128].rearrange("p (c e) -> p c e", e=128))
        dv('tensor_scalar_mul', dN[i][:], idf[:], nmx_all[:, j:j + 1], reads=[idf, nmx_all], writes=[dN[i]])
        P.mm(pN[:, 0:128], ones[:], dN[i][:], start=True, stop=True)
        P.act(E[i][:], pN[:, 0:128], AF.Exp, bias=u_all[:, j:j + 1])
        dv('tensor_mul', E[i][:], E[i][:], triu[:], reads=[E[i], triu], writes=[E[i]], eng='pool')
        for dc in range(2):
            P.mm(pQ[:, 0:128], B['k'][:, dc, sl], B['q'][:, dc, sl], start=(dc == 0), stop=(dc == 1))
        dv('scalar_tensor_tensor', WT[i][:], pQ[:, 0:128], 0.0625, E[i][:], ALU.mult, ALU.mult,
           reads=[pQ, E[i]], writes=[WT[i]])
        P.mm(pI[:, 0:129], WT[i][:], B['v1'][:, c, :], start=True, stop=True)
        P.act(isb[i][:], pI[:, 0:129], AF.Copy)
        dv('tensor_scalar_mul', kw[i][:], B['kt'][:, c * 256:(c + 1) * 256], ws_all[:, j:j + 1],
           reads=[B['kt'], ws_all], writes=[kw[i]], eng='pool')
        for dc in range(2):
            P.mm(pUs[i][:, dc * 129:(dc + 1) * 129], kw[i][:, dc * 128:(dc + 1) * 128], B['v1'][:, c, :],
                 start=True, stop=True)

    def part_b(j):
        g, c = divmod(j, GC)
        B = bufs[g % 2]
        i = j % 2
        sl = slice(c * 128, (c + 1) * 128)
        for dc in range(2):
            P.mm(pR[:, 0:129], B['q'][:, dc, sl], Cnb[dc][:], start=(dc == 0), stop=(dc == 1))
        dv('scalar_tensor_tensor', tot[i][:], pR[:, 0:129], wi_all[:, j:j + 1], isb[i][:], ALU.mult, ALU.add,
           reads=[pR, wi_all, isb[i]], writes=[tot[i]])
        for dc in range(2):
            dv('scalar_tensor_tensor', Cn[dc][:], Cn[dc][:], dec_all[:, j:j + 1], pUs[i][:, dc * 129:(dc + 1) * 129],
               ALU.mult, ALU.add, reads=[Cn[dc], dec_all, pUs[i]], writes=[Cn[dc]])
            P.act(Cnb[dc][:], Cn[dc][:], AF.Copy)
        dv('tensor_scalar_mul', den[i][:], tot[i][:, 128:129], -1.0, reads=[tot[i]], writes=[den[i]])
        dv('tensor_max', den[i][:], den[i][:], tot[i][:, 128:129], reads=[den[i], tot[i]], writes=[den[i]])
        dv('tensor_max', den[i][:], den[i][:], em_all[:, j:j + 1], reads=[den[i], em_all], writes=[den[i]])
        dv('reciprocal', den[i][:], den[i][:], reads=[den[i]], writes=[den[i]])
        P.act(B['ho'][:, c, :], tot[i][:, 0:128], AF.Copy, scale=den[i][:])
        if c == GC - 1:
            P.dma('sp', ho_d[:, g * GC * 128:(g + 1) * GC * 128], B['ho'][:].rearrange("p c e -> p (c e)"),
                  W=[("ho_d", g)], is_out=True)

    part_a(0)
    for j in range(nch):
        if j + 1 < nch:
            part_a(j + 1)
        part_b(j)
    P.emit()
    return C.nc


def build_cmix_odd():
    C = Ctx()
    P = C.P
    h = C.din("h", [TOK, 1024])
    hc_d = C.din("hc", [TOK, 1024])
    xc_d = C.din("xc", [TOK, 1024])
    z_d = C.din("z", [TOK, 1024])
    ho = C.dout("ho", [TOK, 1024])
    C.ident()
    ghn = C.small("ghn", [128, 1024])
    skp = C.small("skip", [128, 1024], q='pool')
    gpo = C.small("gpo", [128, 1024], q='pool')
    C._stages = [P.sb([128, 1024], F32, f"wstage{i}") for i in range(3)]
    Wout = load_w(C, "w_out", 8, 1024, chunk=1024)
    S = NormScr(C, 1024, "a")
    S2 = NormScr(C, 1024, "b", pt=S.pt)
    pys = [P.ps([128, 1024], F32, f"py{i}") for i in range(2)]
    bufs = []
    for i in range(3):
        bufs.append(dict(h=P.sb([128, 1024], F32, f"ht{i}"), c=P.sb([128, 1024], F32, f"hct{i}"),
                         x=P.sb([128, 1024], F32, f"xct{i}"), z=P.sb([128, 1024], F32, f"zt{i}")))
    cat = P.sb([128, 1024], BF16, "cat")
    catTs = [P.sb([128, 8, 128], BF16, f"catT{i}") for i in range(2)]
    mu4 = P.sb([128, 4], F32, "mu4")
    ss4 = P.sb([128, 4], F32, "ss4")
    rstd4 = P.sb([128, 4], F32, "rstd4")
    cen = P.sb([128, 1024], F32, "cen")
    hn = P.sb([128, 1024], F32, "hn")
    sz = P.sb([128, 1024], F32, "sz")

    def st_a(j):
        r0 = j * 128
        B = bufs[j % 3]
        P.dma('sp', B['h'][:], h[r0:r0 + 128, :])
        P.dma('pool', B['c'][:], hc_d[r0:r0 + 128, :])
        P.dma('sp', B['x'][:], xc_d[r0:r0 + 128, :])
        P.dma('pool', B['z'][:], z_d[r0:r0 + 128, :])
        for hh in range(4):
            sl = slice(hh * 256, (hh + 1) * 256)
            P.act(S.junk[:, :256], B['c'][:, sl], AF.Copy, scale=1.0 / 256.0, accum_out=mu4[:, hh:hh + 1])
            P.v('dve', 'tensor_scalar_sub', cen[:, sl], B['c'][:, sl], mu4[:, hh:hh + 1],
                reads=[B['c'], mu4], writes=[cen])
            P.act(S.junk[:, :256], cen[:, sl], AF.Square, scale=1.0 / 16.0, accum_out=ss4[:, hh:hh + 1])
        P.act(rstd4[:], ss4[:], AF.Sqrt, bias=EPS)
        P.v('dve', 'reciprocal', rstd4[:], rstd4[:], reads=[rstd4], writes=[rstd4])
        P.act(sz[:], B['z'][:], AF.Silu)
        for hh in range(4):
            sl = slice(hh * 256, (hh + 1) * 256)
            P.v('dve', 'scalar_tensor_tensor', hn[:, sl], cen[:, sl], rstd4[:, hh:hh + 1], ghn[:, sl],
                ALU.mult, ALU.mult, reads=[cen, rstd4, ghn], writes=[hn])
        P.v('pool', 'tensor_mul', B['x'][:], B['x'][:], skp[:], reads=[B['x'], skp], writes=[B['x']])
        P.v('pool', 'tensor_add', hn[:], hn[:], B['x'][:], reads=[hn, B['x']], writes=[hn])
        P.v('pool', 'tensor_mul', cat[:], hn[:], sz[:], reads=[hn, sz], writes=[cat])
        for c in range(8):
            P.tr(S.pt[:, c, :], cat[:, c * 128:(c + 1) * 128], C.idb[:])
        P.v('dve', 'tensor_copy', catTs[j % 2][:], S.pt[:], reads=[S.pt], writes=[catTs[j % 2]])

    def st_b(j):
        py = pys[j % 2]
        for n in range(2):
            for k in range(8):
                P.mm(py[:, n * 512:(n + 1) * 512], catTs[j % 2][:, k, :], Wout.sl(k, n * 512, (n + 1) * 512),
                     start=(k == 0), stop=(k == 7))

    def st_c(j):
        r0 = j * 128
        B = bufs[j % 3]
        postnorm_res(C, pys[j % 2][:], B['h'][:], gpo, S2, S2.junk)
        P.dma('sp', ho[r0:r0 + 128, :], B['h'][:], W=[("ho", r0)], is_out=True)

    for t in range(NT + 2):
        if t < NT:
            st_a(t)
        if 0 <= t - 1 < NT:
            st_b(t - 1)
        if 0 <= t - 2 < NT:
            st_c(t - 2)
    P.emit()
    return C.nc


def wl(w):
    K, N = w.shape
    KC = K // 128
    return np.ascontiguousarray(w.reshape(KC, 128, N).transpose(1, 0, 2).reshape(128, KC * N))


def vl(g):
    return np.ascontiguousarray(g.reshape(-1, 128).T)


def bc(g, p=128):
    return np.ascontiguousarray(np.broadcast_to(g.reshape(1, -1), (p, g.size)))


_IDN = np.eye(128, dtype=np.float32)
_PROGS = {}


def prog(name, builder):
    if name not in _PROGS:
        _PROGS[name] = builder()
    return _PROGS[name]


def run(nc, maps):
    res = run_bass_kernel_spmd(nc, maps, core_ids=list(range(NCORES)))
    return res.results


def run_ffn(h, w1, w2, gpre, gpost):
    nc = prog("ffn", build_ffn)
    com = {"idn": _IDN, "gpre": vl(gpre), "gpo": bc(gpost), "w1": wl(w1), "w2": wl(w2)}
    maps = [dict(com, h=np.ascontiguousarray(h[c * TOK:(c + 1) * TOK])) for c in range(NCORES)]
    r = run(nc, maps)
    return np.concatenate([r[c]["ho"] for c in range(NCORES)], axis=0)


def run_xa(h, mem, wq, wk, wv, wo, gpre, gmem, gpost):
    nc = prog("xa", build_xa)
    com = {"idn": _IDN, "gpre": vl(gpre), "gmem": vl(gmem), "gpo": bc(gpost), "wq": wl(wq), "wk": wl(wk),
           "wv": wl(wv), "wo": wl(wo), "mem": np.ascontiguousarray(mem)}
    maps = [dict(com, h=np.ascontiguousarray(h[c * TOK:(c + 1) * TOK])) for c in range(NCORES)]
    r = run(nc, maps)
    return np.concatenate([r[c]["ho"] for c in range(NCORES)], axis=0)


def bf(x):
    import ml_dtypes
    return np.ascontiguousarray(x).astype(ml_dtypes.bfloat16)


def inv_freq_tab():
    inv = (np.float32(10000.0) ** (-np.arange(0, 32, 2, dtype=np.float32) / np.float32(32))).astype(np.float32)
    return np.ascontiguousarray(np.broadcast_to(np.tile(inv, 8)[None, :], (128, 128)))


def run_a_even(h, positions, w_in, gpre, g_q, w_uq, g_kv, w_ukv, w_gate, b_gate):
    nc = prog("a_even", build_a_even)
    com = {"idn": _IDN, "gpre": vl(gpre), "gq": vl(g_q), "gkv": vl(g_kv), "w_in": wl(w_in), "w_uq": wl(w_uq),
           "w_ukv": wl(w_ukv), "wga": np.ascontiguousarray(np.concatenate([w_gate, b_gate[None, :]], 0)),
           "invf": inv_freq_tab()}
    maps = []
    for c in range(NCORES):
        pos = positions[c * TOK:(c + 1) * TOK].reshape(NT, 128).T
        maps.append(dict(com, h=np.ascontiguousarray(h[c * TOK:(c + 1) * TOK]), pos=np.ascontiguousarray(pos)))
    r = run(nc, maps)
    return {k: np.concatenate([r[c][k] for c in range(NCORES)], axis=0) for k in ("q", "kv", "kpe", "gqkv", "la", "r")}


def tiles_pm(x):
    T = x.shape[0] // 128
    d = x.shape[1]
    return np.ascontiguousarray(x.reshape(T, 128, d).transpose(1, 0, 2).reshape(128, T * d))


def untiles_pm(y, d):
    T = y.shape[1] // d
    return np.ascontiguousarray(y.reshape(128, T, d).transpose(1, 0, 2).reshape(T * 128, d))


_TRIU = np.triu(np.ones((128, 128), np.float32))
_TRIL = np.tril(np.ones((128, 128), np.float32), -1).T.copy()
_TRIL = np.ascontiguousarray((np.arange(128)[:, None] > np.arange(128)[None, :]).astype(np.float32))


def run_m_even(q, kv, kpe, gqkv, la):
    nc = prog("m_even", build_m_even)
    maps = []
    kpeT = np.ascontiguousarray(kpe.T)
    for c in range(NCORES):
        hg, half = c // 2, c % 2
        qT = np.ascontiguousarray(q[:, c * 96:(c + 1) * 96].T)
        kT = np.ascontiguousarray(np.concatenate([kv[:, c * 128:c * 128 + 64].T, kpeT], axis=0))
        maps.append({
            "qT": qT, "kT": kT, "v": tiles_pm(kv[:, c * 128 + 64:(c + 1) * 128]),
            "trib": bf(_TRIU), "triu": _TRIU, "tril": _TRIL,
            "gqT": np.ascontiguousarray(gqkv[:, hg * 64:(hg + 1) * 64].T),
            "gkT": np.ascontiguousarray(gqkv[:, 256 + hg * 64:256 + (hg + 1) * 64].T),
            "gk": tiles_pm(gqkv[:, 256 + hg * 64:256 + (hg + 1) * 64]),
            "gv": tiles_pm(gqkv[:, 512 + hg * 128 + half * 64:512 + hg * 128 + (half + 1) * 64]),
            "la": tiles_pm(la[:, hg * 64:(hg + 1) * 64]),
        })
    r = run(nc, maps)
    a = np.concatenate([untiles_pm(r[c]["o"], 64) for c in range(NCORES)], axis=1)
    go = np.concatenate([untiles_pm(r[c]["go"], 64) for c in range(NCORES)], axis=1)
    return a, go


def shard(x):
    return [np.ascontiguousarray(x[c * TOK:(c + 1) * TOK]) for c in range(NCORES)]


def run_cmix_even(h, a, go, r, g_gla, w_out, gpost):
    nc = prog("cmix_even", build_cmix_even)
    com = {"idn": _IDN, "ggla": bc(g_gla), "gpo": bc(gpost), "w_out": wl(w_out)}
    hs, as_, gs, rs = shard(h), shard(a), shard(go), shard(r)
    maps = [dict(com, h=hs[c], a=as_[c], go=gs[c], r=rs[c]) for c in range(NCORES)]
    res = run(nc, maps)
    return np.concatenate([res[c]["ho"] for c in range(NCORES)], axis=0)


def run_a_odd(h, w_in, gpre, conv_w, conv_b, w_q, w_k, w_v, w_gates, b_gates):
    nc = prog("a_odd", build_a_odd)

    def hw(w):
        return np.ascontiguousarray(w.reshape(4, 2, 128, 256).transpose(2, 0, 1, 3).reshape(128, 8 * 256))
    com = {"idn": _IDN, "gpre": vl(gpre), "w_in": wl(w_in),
           "convw": np.ascontiguousarray(conv_w.reshape(4, 8, 128).transpose(2, 1, 0).reshape(128, 32)),
           "convb": vl(conv_b), "bg": np.ascontiguousarray(b_gates.reshape(8, 1)),
           "wq": hw(w_q), "wk": hw(w_k), "wv": hw(w_v), "wg": wl(w_gates)}
    maps = []
    for c in range(NCORES):
        hh = h[c * TOK - 128:c * TOK] if c > 0 else np.zeros((128, 1024), np.float32)
        maps.append(dict(com, h=np.ascontiguousarray(h[c * TOK:(c + 1) * TOK]), hh=np.ascontiguousarray(hh)))
    r = run(nc, maps)
    out = {k: np.concatenate([r[c][k] for c in range(NCORES)], axis=1) for k in ("xcT", "qT", "kT", "vT", "gi", "gl")}
    out["z"] = np.concatenate([r[c]["z"] for c in range(NCORES)], axis=0)
    return out


def run_m_odd(qT, kT, vT, gi, gl):
    nc = prog("m_odd", build_m_odd)
    maps = []
    for c in range(NCORES):
        hd, half = c // 2, c % 2
        q = qT[hd * 256:(hd + 1) * 256]
        k = kT[hd * 256:(hd + 1) * 256]
        v = vT[hd * 256 + half * 128:hd * 256 + (half + 1) * 128]
        maps.append({"idn": _IDN, "triu": _TRIU, "qT": np.ascontiguousarray(q), "kT": np.ascontiguousarray(k),
                     "ktok": tiles_pm(np.ascontiguousarray(k.T)), "v": tiles_pm(np.ascontiguousarray(v.T)),
                     "ii": np.ascontiguousarray(gi[hd].reshape(-1, 128).T),
                     "lf": np.ascontiguousarray(gl[4 + hd].reshape(-1, 128).T)})
    r = run(nc, maps)
    return np.concatenate([untiles_pm(r[c]["ho"], 128) for c in range(NCORES)], axis=1)


def run_cmix_odd(h, hc, xc, z, g_hnorm, skip, w_out, gpost):
    nc = prog("cmix_odd", build_cmix_odd)
    com = {"idn": _IDN, "ghn": bc(g_hnorm), "skip": bc(skip), "gpo": bc(gpost), "w_out": wl(w_out)}
    hs, cs, xs, zs = shard(h), shard(hc), shard(xc), shard(z)
    maps = [dict(com, h=hs[c], hc=cs[c], xc=xs[c], z=zs[c]) for c in range(NCORES)]
    res = run(nc, maps)
    return np.concatenate([res[c]["ho"] for c in range(NCORES)], axis=0)


def kernel(x, mem, positions,
           g_mix_pre, g_mix_post, g_xattn_pre, g_xattn_post, g_mem, g_ffn_pre, g_ffn_post,
           ev_w_in, ev_g_q, ev_w_uq, ev_g_kv, ev_w_ukv, ev_w_gate, ev_b_gate, ev_g_gla, ev_w_out,
           od_w_in, od_conv_w, od_conv_b, od_w_q, od_w_k, od_w_v, od_w_gates, od_b_gates,
           od_g_hnorm, od_skip, od_w_out,
           xa_w_q, xa_w_k, xa_w_v, xa_w_o,
           ffn_w1, ffn_w2):
    f = lambda a: np.asarray(a)
    h = np.ascontiguousarray(f(x)[0], dtype=np.float32)
    memv = np.ascontiguousarray(f(mem)[0], dtype=np.float32)
    pos = np.ascontiguousarray(f(positions)[0]).astype(np.int32)
    for layer in range(4):
        j = layer // 2
        if layer % 2 == 0:
            o = run_a_even(h, pos, f(ev_w_in)[j], f(g_mix_pre)[layer], f(ev_g_q)[j], f(ev_w_uq)[j], f(ev_g_kv)[j],
                           f(ev_w_ukv)[j], f(ev_w_gate)[j], f(ev_b_gate)[j])
            a, go = run_m_even(o["q"], o["kv"], o["kpe"], o["gqkv"], o["la"])
            h = run_cmix_even(h, a, go, o["r"], f(ev_g_gla)[j], f(ev_w_out)[j], f(g_mix_post)[layer])
        else:
            o = run_a_odd(h, f(od_w_in)[j], f(g_mix_pre)[layer], f(od_conv_w)[j], f(od_conv_b)[j], f(od_w_q)[j],
                          f(od_w_k)[j], f(od_w_v)[j], f(od_w_gates)[j], f(od_b_gates)[j])
            hc = run_m_odd(o["qT"], o["kT"], o["vT"], o["gi"], o["gl"])
            h = run_cmix_odd(h, hc, np.ascontiguousarray(o["xcT"].T), o["z"], f(od_g_hnorm)[j], f(od_skip)[j],
                             f(od_w_out)[j], f(g_mix_post)[layer])
        h = run_xa(h, memv, f(xa_w_q)[layer], f(xa_w_k)[layer], f(xa_w_v)[layer], f(xa_w_o)[layer],
                   f(g_xattn_pre)[layer], f(g_mem)[layer], f(g_xattn_post)[layer])
        h = run_ffn(h, f(ffn_w1)[layer], f(ffn_w2)[layer], f(g_ffn_pre)[layer], f(g_ffn_post)[layer])
    return h[None].astype(np.float32)
```
